# Optimizing a Trainium2 kernel written in Bass

```python
import math
import jax, jax.numpy as jnp
from jax import lax
import numpy as np

D_MODEL = 1024
BATCH = 4
SEQ = 4096
DEPTH = 2

M_HEADS = 4
M_DQK = 64
M_DV = 128
M_CHUNK = 64
M_CONV = 4
M_QK_W = M_HEADS * M_DQK
M_V_W = M_HEADS * M_DV

A_HEADS = 8
A_KV_HEADS = 2
A_HEAD_DIM = 64
A_GROUP = A_HEADS // A_KV_HEADS
A_Q_W = A_HEADS * A_HEAD_DIM
A_KV_W = A_KV_HEADS * A_HEAD_DIM
WINDOW = 128

N_BUCKETS = 32
MAX_DISTANCE = 128

N_EXPERTS = 16
N_GROUPS = 4
EXPERTS_PER_GROUP = N_EXPERTS // N_GROUPS
GROUP_SCORE_TOPK = 2
TOP_K = 2
D_EXPERT = 512

IN_SIZES = (M_QK_W, M_QK_W, M_V_W, M_V_W, M_HEADS, M_HEADS,
            A_Q_W, A_KV_W, A_KV_W, D_MODEL, D_MODEL)
D_IN = M_QK_W * 2 + M_V_W * 2 + M_HEADS * 2 + A_Q_W + A_KV_W * 2 + D_MODEL * 2

EPS = 1e-6
NEG_INF = -1e30

kernel_name = 'hybrid_mlstm_swa_groupmoe_block'


def _rmsnorm(x, w):
    xf = x.astype(jnp.float32)
    y = xf * lax.rsqrt(jnp.mean(xf * xf, axis=-1, keepdims=True) + EPS)
    return (y * w.astype(jnp.float32)).astype(x.dtype)


def _modulate(h, shift, scale):
    return h * (1 + scale[:, None, :]) + shift[:, None, :]


def _split_points(sizes):
    pts, acc = [], 0
    for sz in sizes[:-1]:
        acc += sz
        pts.append(acc)
    return pts


def _causal_conv(x, w, b):
    s = x.shape[1]
    xp = jnp.pad(x, ((0, 0), (M_CONV - 1, 0), (0, 0)))
    y = b
    for j in range(M_CONV):
        y = y + w[j] * xp[:, j:j + s, :]
    return y


def _t5_bucket(n):
    max_exact = N_BUCKETS // 2
    large = max_exact + (np.log(np.maximum(n, 1) / max_exact)
                         / math.log(MAX_DISTANCE / max_exact)
                         * (N_BUCKETS - max_exact)).astype(np.int32)
    large = np.minimum(large, N_BUCKETS - 1)
    return np.where(n < max_exact, n, large).astype(np.int32)


def _chunk_step(carry, inp):
    C, n, m = carry
    C_loc, n_loc, m_loc, b_last = inp
    m_new = jnp.maximum(b_last + m, m_loc)
    decay = jnp.exp(b_last + m - m_new)
    fresh = jnp.exp(m_loc - m_new)
    C_new = decay[..., None, None] * C + fresh[..., None, None] * C_loc
    n_new = decay[..., None] * n + fresh[..., None] * n_loc
    return (C_new, n_new, m_new), (C, n, m)


def _mlstm(q, k, v, i_pre, f_pre):
    b, s, _ = q.shape
    nc = s // M_CHUNK

    def heads(t, d):
        t = t.astype(jnp.float32).reshape(b, s, M_HEADS, d).transpose(0, 2, 1, 3)
        return t.reshape(b, M_HEADS, nc, M_CHUNK, d)

    def gates(t):
        return t.astype(jnp.float32).transpose(0, 2, 1).reshape(b, M_HEADS, nc, M_CHUNK)

    q = heads(q, M_DQK) * (M_DQK ** -0.5)
    k = heads(k, M_DQK)
    v = heads(v, M_DV)
    ig = gates(i_pre)
    logf = jax.nn.log_sigmoid(gates(f_pre))
    bcum = jnp.cumsum(logf, axis=-1)
    b_last = bcum[..., -1]

    w_state = b_last[..., None] - bcum + ig
    m_loc = jnp.max(w_state, axis=-1)
    a = jnp.exp(w_state - m_loc[..., None])
    C_loc = jnp.einsum('bhcl,bhcld,bhcle->bhcde', a, v, k)
    n_loc = jnp.einsum('bhcl,bhcle->bhce', a, k)

    init = (jnp.zeros((b, M_HEADS, M_DV, M_DQK), jnp.float32),
            jnp.zeros((b, M_HEADS, M_DQK), jnp.float32),
            jnp.zeros((b, M_HEADS), jnp.float32))
    xs = (jnp.moveaxis(C_loc, 2, 0), jnp.moveaxis(n_loc, 2, 0),
          jnp.moveaxis(m_loc, 2, 0), jnp.moveaxis(b_last, 2, 0))
    _, (C_prev, n_prev, m_prev) = lax.scan(_chunk_step, init, xs)
    C_prev = jnp.moveaxis(C_prev, 0, 2)
    n_prev = jnp.moveaxis(n_prev, 0, 2)
    m_prev = jnp.moveaxis(m_prev, 0, 2)

    causal = jnp.tril(jnp.ones((M_CHUNK, M_CHUNK), dtype=bool))
    d_log = jnp.where(causal, bcum[..., :, None] - bcum[..., None, :] + ig[..., None, :], -jnp.inf)
    m_inter = bcum + m_prev[..., None]
    m_row = jnp.maximum(m_inter, jnp.max(d_log, axis=-1))
    scores = jnp.einsum('bhcld,bhcsd->bhcls', q, k) * jnp.exp(d_log - m_row[..., None])
    inter = jnp.exp(m_inter - m_row)
    num = (jnp.einsum('bhcls,bhcsd->bhcld', scores, v)
           + inter[..., None] * jnp.einsum('bhcle,bhcde->bhcld', q, C_prev))
    den = scores.sum(-1) + inter * jnp.einsum('bhcle,bhce->bhcl', q, n_prev)
    h = num / jnp.maximum(jnp.abs(den), jnp.exp(-m_row))[..., None]
    return h.reshape(b, M_HEADS, s, M_DV).transpose(0, 2, 1, 3).reshape(b, s, M_V_W)


def _sliding_window_attention(q, k, v, sinks, rel_bias):
    b, s, _ = q.shape
    nb = s // WINDOW
    qb = q.reshape(b, nb, WINDOW, A_KV_HEADS, A_GROUP, A_HEAD_DIM).astype(jnp.float32)

    def band(t):
        t = t.reshape(b, s, A_KV_HEADS, A_HEAD_DIM)
        tp = jnp.pad(t, ((0, 0), (WINDOW, 0), (0, 0), (0, 0)))
        tp = tp.reshape(b, nb + 1, WINDOW, A_KV_HEADS, A_HEAD_DIM)
        return jnp.concatenate([tp[:, :-1], tp[:, 1:]], axis=2)

    kb = band(k).astype(jnp.float32)
    vb = band(v)

    q_pos = np.arange(WINDOW)[:, None]
    k_pos = np.arange(2 * WINDOW)[None, :]
    dist = q_pos + WINDOW - k_pos
    in_window = (dist >= 0) & (dist < WINDOW)
    key_valid = (np.arange(nb)[:, None] * WINDOW + np.arange(2 * WINDOW)[None, :] - WINDOW) >= 0
    mask = jnp.asarray(in_window[None] & key_valid[:, None, :])
    bucket = jnp.asarray(_t5_bucket(np.maximum(dist, 0)))
    bias = jnp.take(rel_bias.astype(jnp.float32), bucket, axis=0)
    bias = bias.transpose(2, 0, 1).reshape(A_KV_HEADS, A_GROUP, WINDOW, 2 * WINDOW)

    scores = jnp.einsum('bnqkgd,bnskd->bnkgqs', qb, kb) * (A_HEAD_DIM ** -0.5)
    scores = jnp.where(mask[None, :, None, None], scores + bias, NEG_INF)
    sink = jnp.broadcast_to(sinks.astype(jnp.float32).reshape(A_KV_HEADS, A_GROUP, 1, 1),
                            scores.shape[:-1] + (1,))
    probs = jax.nn.softmax(jnp.concatenate([scores, sink], axis=-1), axis=-1)[..., :-1]
    out = jnp.einsum('bnkgqs,bnskd->bnqkgd', probs.astype(v.dtype), vb)
    return out.reshape(b, s, A_Q_W)


def _hybrid_mixer(h, w_in, conv_w, conv_b, b_igate, b_fgate, w_mnorm, sinks, rel_bias,
                  w_br_m, w_br_a, w_out):
    z = h @ w_in
    (q_m, k_m, v_m, o_m, i_m, f_m, q_a, k_a, v_a, g_m, g_a) = jnp.split(
        z, _split_points(IN_SIZES), axis=-1)
    qk = jax.nn.silu(_causal_conv(jnp.concatenate([q_m, k_m], axis=-1), conv_w, conv_b))
    q_m, k_m = jnp.split(qk, 2, axis=-1)
    h_m = _mlstm(q_m, k_m, v_m, i_m + b_igate, f_m + b_fgate).astype(h.dtype)
    bsz, s = h_m.shape[0], h_m.shape[1]
    h_m = _rmsnorm(h_m.reshape(bsz, s, M_HEADS, M_DV), w_mnorm.reshape(M_HEADS, M_DV))
    h_m = h_m.reshape(bsz, s, M_V_W) * jax.nn.sigmoid(o_m)
    h_a = _sliding_window_attention(q_a, k_a, v_a, sinks, rel_bias)
    merged = jax.nn.sigmoid(g_m) * (h_m @ w_br_m) + jax.nn.sigmoid(g_a) * (h_a @ w_br_a)
    return merged @ w_out


def _grouped_moe(h, w_router, router_bias, w_gate, w_up, w_down):
    b, s, d = h.shape
    t = h.reshape(b * s, d)
    scores = jax.nn.sigmoid((t @ w_router).astype(jnp.float32))
    sel = scores + router_bias.astype(jnp.float32)
    grp = lax.top_k(sel.reshape(-1, N_GROUPS, EXPERTS_PER_GROUP), GROUP_SCORE_TOPK)[0].sum(-1)
    gsel = jnp.argmax(grp, axis=-1)
    gmask = jnp.repeat(gsel[:, None] == jnp.arange(N_GROUPS)[None, :], EXPERTS_PER_GROUP, axis=-1)
    _, idx = lax.top_k(jnp.where(gmask, sel, -jnp.inf), TOP_K)
    wts = jnp.take_along_axis(scores, idx, axis=-1)
    wts = wts / jnp.sum(wts, axis=-1, keepdims=True)
    gates = jnp.sum(jax.nn.one_hot(idx, N_EXPERTS, dtype=jnp.float32) * wts[..., None], axis=1)
    gates = gates.astype(h.dtype)
    y = jnp.zeros_like(t)
    for e in range(N_EXPERTS):
        act = jax.nn.silu(t @ w_gate[e]) * (t @ w_up[e])
        y = y + gates[:, e:e + 1] * (act @ w_down[e])
    return y.reshape(b, s, d)


def setup_inputs(seed: int = 0) -> dict:
    key = jax.random.key(seed)
    ks = jax.random.split(key, 23)

    def nrm(k, shape, scale):
        return jax.random.normal(k, shape, jnp.float32) * scale

    return {
        'x': nrm(ks[0], (BATCH, SEQ, D_MODEL), 1.0),
        'c': nrm(ks[1], (BATCH, D_MODEL), 1.0),
        'w_ada': nrm(ks[2], (DEPTH, D_MODEL, 6 * D_MODEL), 0.5 * D_MODEL ** -0.5),
        'b_ada': nrm(ks[3], (DEPTH, 6 * D_MODEL), 0.02),
        'w_norm1': 1.0 + nrm(ks[4], (DEPTH, D_MODEL), 0.02),
        'w_in': nrm(ks[5], (DEPTH, D_MODEL, D_IN), D_MODEL ** -0.5),
        'conv_w': nrm(ks[6], (DEPTH, M_CONV, 2 * M_QK_W), M_CONV ** -0.5),
        'conv_b': nrm(ks[7], (DEPTH, 2 * M_QK_W), 0.02),
        'b_igate': nrm(ks[8], (DEPTH, M_HEADS), 0.1),
        'b_fgate': jnp.linspace(3.0, 6.0, M_HEADS, dtype=jnp.float32)[None, :]
                   + nrm(ks[9], (DEPTH, M_HEADS), 0.1),
        'w_mnorm': 1.0 + nrm(ks[10], (DEPTH, M_V_W), 0.02),
        'sinks': nrm(ks[11], (DEPTH, A_HEADS), 0.5),
        'rel_bias': nrm(ks[12], (N_BUCKETS, A_HEADS), 0.3),
        'w_br_m': nrm(ks[13], (DEPTH, M_V_W, D_MODEL), M_V_W ** -0.5),
        'w_br_a': nrm(ks[14], (DEPTH, A_Q_W, D_MODEL), A_Q_W ** -0.5),
        'w_out': nrm(ks[15], (DEPTH, D_MODEL, D_MODEL), D_MODEL ** -0.5),
        'w_norm2': 1.0 + nrm(ks[16], (DEPTH, D_MODEL), 0.02),
        'w_router': nrm(ks[17], (D_MODEL, N_EXPERTS), D_MODEL ** -0.5),
        'router_bias': nrm(ks[18], (N_EXPERTS,), 0.01),
        'w_gate_e': nrm(ks[19], (DEPTH, N_EXPERTS, D_MODEL, D_EXPERT), D_MODEL ** -0.5),
        'w_up_e': nrm(ks[20], (DEPTH, N_EXPERTS, D_MODEL, D_EXPERT), D_MODEL ** -0.5),
        'w_down_e': nrm(ks[21], (DEPTH, N_EXPERTS, D_EXPERT, D_MODEL), D_EXPERT ** -0.5),
        'w_final': 1.0 + nrm(ks[22], (D_MODEL,), 0.02),
    }


def reference(x, c, w_ada, b_ada, w_norm1, w_in, conv_w, conv_b, b_igate, b_fgate, w_mnorm,
              sinks, rel_bias, w_br_m, w_br_a, w_out, w_norm2, w_router, router_bias,
              w_gate_e, w_up_e, w_down_e, w_final):
    cond = jax.nn.silu(c)
    for l in range(DEPTH):
        mod = cond @ w_ada[l] + b_ada[l]
        sh1, sc1, g1, sh2, sc2, g2 = jnp.split(mod, 6, axis=-1)
        h = _modulate(_rmsnorm(x, w_norm1[l]), sh1, sc1)
        x = x + g1[:, None, :] * _hybrid_mixer(
            h, w_in[l], conv_w[l], conv_b[l], b_igate[l], b_fgate[l], w_mnorm[l], sinks[l],
            rel_bias, w_br_m[l], w_br_a[l], w_out[l])
        h = _modulate(_rmsnorm(x, w_norm2[l]), sh2, sc2)
        x = x + g2[:, None, :] * _grouped_moe(
            h, w_router, router_bias, w_gate_e[l], w_up_e[l], w_down_e[l])
    return _rmsnorm(x, w_final)
```

```python
import math
import os
from contextlib import ExitStack

import numpy as np
import concourse.bass as bass
import concourse.mybir as mybir
from concourse.bass_utils import run_bass_kernel_spmd

F32 = mybir.dt.float32
BF16 = mybir.dt.bfloat16
AF = mybir.ActivationFunctionType
ALU = mybir.AluOpType
AX = mybir.AxisListType

D = 1024
NTOK = 2048
NT = 16
G = 256
TPG = G // 128
NG = NTOK // G
EPS = 1e-6
NE = 16
DE = 512
NWIN = 4488
LN8 = math.log(0.125)
NCORES = 8

PL = {}
_o = 0
for _n, _w in [("wn1", 8), ("wn2", 8), ("bada", 48), ("convw", 16),
               ("convb", 4), ("bif", 8), ("wmn", 512), ("sinks", 8)]:
    PL[_n] = (_o, _o + _w)
    _o += _w
NPL = _o
PG = {}
_o = 0
for _n, _w in [("cT", 8), ("flag", 1), ("rb", 16), ("wr", 128), ("wfin", 1024)]:
    PG[_n] = (_o, _o + _w)
    _o += _w
NPG = _o


class Res:
    __slots__ = ("name", "w", "r")

    def __init__(self, name):
        self.name = name
        self.w = None
        self.r = {}


class Sync:
    def __init__(self, nc, es):
        self.nc = nc
        self.es = es
        self.engs = {"pe": nc.tensor, "act": nc.scalar, "dve": nc.vector, "pool": nc.gpsimd, "sp": nc.sync}
        self.sems = {}
        self.cnt = {}
        for k in self.engs:
            self.sems[k] = es.enter_context(nc.semaphore("s_" + k))
            self.cnt[k] = 0
        self.seen = {k: {} for k in self.engs}

    def dma_sem(self, key):
        if key not in self.sems:
            self.sems[key] = self.es.enter_context(self.nc.semaphore("d_" + key))
            self.cnt[key] = 0
        return key

    def need(self, eng, ev, same_ok=False):
        if ev is None:
            return
        k, v = ev
        if k == eng and not same_ok:
            return
        if self.seen[eng].get(k, 0) >= v:
            return
        self.engs[eng].wait_ge(self.sems[k], v)
        self.seen[eng][k] = v

    def deps(self, eng, reads, writes, force_same=False):
        for r in reads:
            self.need(eng, r.w, same_ok=True)
        for w in writes:
            self.need(eng, w.w, same_ok=force_same)
            for k, v in w.r.items():
                self.need(eng, (k, v), same_ok=force_same)

    def emit(self, eng, fn, reads=(), writes=(), inc=True):
        self.deps(eng, reads, writes)
        inst = fn(self.engs[eng])
        if inc:
            inst.then_inc(self.sems[eng], 1)
            self.cnt[eng] += 1
            ev = (eng, self.cnt[eng])
        else:
            ev = (eng, self.cnt[eng] + 1)
        for r in reads:
            r.r[eng] = max(r.r.get(eng, 0), ev[1])
        for w in writes:
            w.w = ev
            w.r = {}
        return inst

    def dma(self, queue, out, in_, reads=(), writes=(), semkey=None):
        self.deps(queue, reads, writes, force_same=True)
        semkey = self.dma_sem(semkey)
        inst = self.engs[queue].dma_start(out=out, in_=in_)
        inst.then_inc(self.sems[semkey], 16)
        self.cnt[semkey] += 16
        ev = (semkey, self.cnt[semkey])
        for r in reads:
            r.r[semkey] = ev[1]
        for w in writes:
            w.w = ev
            w.r = {}
        return inst

    def emit_cc(self, fn, reads, writes, key):
        self.deps("pool", reads, writes, force_same=True)
        key = self.dma_sem(key)
        inst = fn(self.engs["pool"])
        inst.then_inc(self.sems[key])
        self.cnt[key] = 1
        ev = (key, 1)
        for r in reads:
            r.r[key] = 1
        for w in writes:
            w.w = ev
            w.r = {}
        return inst

    def barrier(self, skip=()):
        for e in self.engs:
            for k in self.sems:
                if k != e and self.cnt[k] > 0 and k not in skip:
                    self.need(e, (k, self.cnt[k]))


class T:
    def __init__(self, es, nc, name, shape, dtype, psum=False):
        if psum:
            self.t = es.enter_context(nc.psum_tensor("p_" + name, shape, dtype))
        else:
            self.t = es.enter_context(nc.sbuf_tensor("t_" + name, shape, dtype))
        self.r = Res(name)
        self.name = name
        self.psum = psum

    def __getitem__(self, k):
        return self.t[k]


def build(nl, final, moe=True, dbg=99):
    nc = bass.Bass("TRN2", target_bir_lowering=False)

    def din(name, shape, dt=F32):
        return nc.dram_tensor(name, shape, dt, kind="ExternalInput").ap()

    def dout(name, shape, dt=F32):
        return nc.dram_tensor(name, shape, dt, kind="ExternalOutput").ap()

    x_d = din("x", [NTOK, D])
    prmg_d = din("prmg", [128, NPG])
    prml_d = din("prml", [nl, 128, NPL])
    prmb_d = din("prmb", [nl, 128, 2048])
    cst_d = din("cst", [128, 384])
    biasT_d = din("biasT", [128, 2048])
    wada_d = din("w_ada", [nl, D, 6 * D])
    win_d = din("w_in", [nl, D, NWIN])
    wbr_d = din("w_br", [nl, 2, 512, D])
    wout_d = din("w_out", [nl, D, D])
    wg_d = din("wg", [nl, NE, D, DE])
    wu_d = din("wu", [nl, NE, D, DE])
    wd_d = din("wd", [nl, NE, DE, D])
    y_d = dout("y", [NTOK, D])

    with ExitStack() as es:
        sy = Sync(nc, es)

        def E(eng, fn, r=(), w=(), inc=True):
            w = list(w) + [t for t in r if getattr(t, "psum", False) and t not in w]
            return sy.emit(eng, fn, reads=[t.r for t in r], writes=[t.r for t in w], inc=inc)

        def DMA(q, out, in_, r=(), w=(), key=None):
            return sy.dma(q, out, in_, reads=[t.r for t in r], writes=[t.r for t in w], semkey=key)

        def mk(name, shape, dt, stack=es):
            return T(stack, nc, name, shape, dt)

        x = mk("x", [128, NT, D], F32)
        xr = [Res(f"x{t}") for t in range(NT)]

        class XT:
            def __init__(self, t):
                self.r = xr[t]
        xt = [XT(t) for t in range(NT)]
        prmg = mk("prmg", [128, NPG], F32)
        prml = mk("prml", [128, NPL], F32)
        cst = mk("cst", [128, 384], F32)
        ident_b = mk("ident_b", [128, 128], BF16)
        tri_b = mk("tri_b", [128, 128], BF16)
        modc = mk("modc", [128, 48], F32)
        a1 = mk("a1", [128, 8], F32)
        a2 = mk("a2", [128, 8], F32)
        g1bc = mk("g1bc", [128, D], F32)
        g2bc = mk("g2bc", [128, D], F32)
        cond_bf = mk("cond_bf", [128, 8], BF16)
        cond_f = mk("cond_f", [128, 8], F32)
        banks = [T(es, nc, f"bank{i}", [128, 512], F32, psum=True) for i in range(8)]
        bstate = {"i": 0}

        def bank():
            b = banks[bstate["i"] % 8]
            bstate["i"] += 1
            return b

        ident_f = cst[:, 0:128]
        tri_f = cst[:, 128:256]
        ones_f = cst[:, 256:384]

        def pl(name, a=None, b=None):
            lo, hi = PL[name]
            if a is None:
                return prml[:, lo:hi]
            return prml[:, lo + a:lo + b]

        def pg(name, a=None, b=None):
            lo, hi = PG[name]
            if a is None:
                return prmg[:, lo:hi]
            return prmg[:, lo + a:lo + b]

        xv = x_d.rearrange("(t p) d -> p t d", p=128)
        for t in range(NT):
            DMA("sp", x[:, t, :], xv[:, t, :], w=[xt[t]], key=f"x{t}")
        DMA("sp", prmg[:], prmg_d, w=[prmg], key="prmg")
        DMA("sp", cst[:], cst_d, w=[cst], key="cst")
        E("dve", lambda e: e.tensor_copy(ident_b[:], ident_f), r=[cst], w=[ident_b])
        E("dve", lambda e: e.tensor_copy(tri_b[:], tri_f), r=[cst], w=[tri_b])
        ones_b = mk("ones_b", [128, 128], BF16)
        E("dve", lambda e: e.tensor_copy(ones_b[:], ones_f), r=[cst], w=[ones_b])
        E("act", lambda e: e.activation(cond_f[:], pg("cT"), AF.Silu), r=[prmg], w=[cond_f])
        E("dve", lambda e: e.tensor_copy(cond_bf[:], cond_f[:]), r=[cond_f], w=[cond_bf])

        def mm_group(out_ap, pairs, r, w):
            n = len(pairs)
            for i, (lt, rh) in enumerate(pairs):
                E("pe", lambda e, lt=lt, rh=rh, i=i: e.matmul(out_ap, lt, rh, start=(i == 0), stop=(i == n - 1)),
                  r=r, w=w, inc=(i == n - 1))

        sm = {}

        def small(name, shape, dt=F32, stack=es):
            if name not in sm:
                sm[name] = mk(name, shape, dt, stack)
            return sm[name]

        def norm_gen(t, acol, shcol, dst, dcol, sc, router_dst=None, alloc=None, rel=None):
            junk, ssum, tmp1, tmp2, rstd, xn = sc
            junk = xn
            E("dve", lambda e: e.memset(ssum[:], 0.0), w=[ssum])
            E("act", lambda e: e.activation(junk[:], x[:, t, :], AF.Square, accum_out=ssum[:, 0:1]),
              r=[xt[t], ssum], w=[junk, ssum])
            yield
            E("dve", lambda e: e.tensor_scalar(tmp1[:], ssum[:], 1.0 / D, EPS, ALU.mult, ALU.add), r=[ssum], w=[tmp1])
            E("act", lambda e: e.sqrt(tmp2[:], tmp1[:]), r=[tmp1], w=[tmp2])
            yield
            E("dve", lambda e: e.reciprocal(rstd[:], tmp2[:]), r=[tmp2], w=[rstd])
            E("act", lambda e: e.mul(xn[:], x[:, t, :], rstd[:, 0:1]), r=[xt[t], rstd], w=[xn])
            yield
            for half in range(2):
                if alloc is None:
                    b = bank()
                else:
                    b = yield from alloc()
                for j in range(4):
                    kk = half * 4 + j
                    E("pe", lambda e, j=j, kk=kk, b=b: e.transpose(b[:, j * 128:(j + 1) * 128],
                                                                    xn[:, kk * 128:(kk + 1) * 128], ident_f),
                      r=[xn, cst], w=[b], inc=(j == 3))
                yield
                for j in range(4):
                    kk = half * 4 + j
                    if half == 0:
                        E("dve", lambda e, j=j, kk=kk, b=b: e.tensor_scalar(
                            dst[:, kk, dcol:dcol + 128], b[:, j * 128:(j + 1) * 128],
                            acol[:, kk:kk + 1], shcol[:, kk:kk + 1], ALU.mult, ALU.add), r=[b, modc, a1, a2], w=[dst])
                    else:
                        E("act", lambda e, j=j, kk=kk, b=b: e.activation(
                            dst[:, kk, dcol:dcol + 128], b[:, j * 128:(j + 1) * 128], AF.Identity,
                            bias=shcol[:, kk:kk + 1], scale=acol[:, kk:kk + 1]), r=[b, modc, a1, a2], w=[dst])
                if router_dst is not None:
                    E("act", lambda e, half=half, b=b: e.copy(router_dst[:, half * 512:(half + 1) * 512], b[:]),
                      r=[b], w=[router_dst])
                if rel is not None:
                    rel(b)
                yield

        def norm_tile(t, acol, shcol, dst, dcol, sc, router_dst=None):
            for _ in norm_gen(t, acol, shcol, dst, dcol, sc, router_dst):
                pass

        for l in range(nl):
            if dbg <= 1:
                break
            sy.barrier()
            DMA("sp", prml[:], prml_d[l], w=[prml], key="prml")
            with ExitStack() as ms:
                biasT = mk(f"biasT_{l}", [128, 2048], BF16, ms)
                DMA("pool", biasT[:], biasT_d, w=[biasT], key="biasT")
                ring = [mk(f"ring{i}_{l}", [128, 8, 512], BF16, ms) for i in range(3)]
                wbr = mk(f"wbr_{l}", [128, 2, 4, D], BF16, ms)
                wout = mk(f"wout_{l}", [128, 8, D], BF16, ms)
                hT2 = [mk(f"hT{i}_{l}", [128, 8, G], BF16, ms) for i in range(2)]
                qkpre = [mk(f"qkpre{i}_{l}", [128, 3 + G], F32, ms) for i in range(4)]
                qkT = mk(f"qkT_{l}", [128, 4, G], BF16, ms)
                qaT = None
                kaT = mk(f"kaT_{l}", [128, 2, 128 + G], BF16, ms)
                vaug = [mk(f"vaug{i}_{l}", [128, 2, 72], BF16, ms) for i in range(3)]
                vhalo = mk(f"vhalo_{l}", [128, 2, 72], BF16, ms)
                state_f = mk(f"state_f_{l}", [128, 2, 129], F32, ms)
                state_b = mk(f"state_b_{l}", [128, 2, 129], BF16, ms)
                hmT = None
                haT = None
                mT = None
                esink = mk(f"esink_{l}", [128, 8], F32, ms)
                nsc = (None, mk(f"ssum_{l}", [128, 1], F32, ms),
                       mk(f"tmp1_{l}", [128, 1], F32, ms), mk(f"tmp2_{l}", [128, 1], F32, ms),
                       mk(f"rstd_{l}", [128, 1], F32, ms), mk(f"xn_{l}", [128, D], F32, ms))
                gsb = [mk(f"gsb{i}_{l}", [128, 8], F32, ms) for i in range(TPG)]
                ef4 = [mk(f"ef4{i}_{l}", [128, 4], F32, ms) for i in range(TPG)]
                logf = [mk(f"logf{i}_{l}", [128, 4], F32, ms) for i in range(TPG)]
                lhi = [mk(f"lhi{i}_{l}", [128, 4], BF16, ms) for i in range(TPG)]
                llo = [mk(f"llo{i}_{l}", [128, 4], BF16, ms) for i in range(TPG)]
                w4 = [mk(f"w4{i}_{l}", [128, 4], F32, ms) for i in range(TPG)]
                sc4 = [mk(f"sc4{i}_{l}", [128, 4], F32, ms) for i in range(TPG)]
                eb4 = [mk(f"eb4{i}_{l}", [128, 4], F32, ms) for i in range(TPG)]
                vpr = [mk(f"vpr{i}_{l}", [128, 4, 136], BF16, ms) for i in range(TPG)]
                som = None
                ktok = [mk(f"ktok{i}_{l}", [128, 2, 2, 128], BF16, ms) for i in range(TPG)]
                ebsel = [mk(f"ebsel{i}_{l}", [128, 2], F32, ms) for i in range(TPG)]
                sd = None
                den4 = None
                r4 = None
                hm = None
                sqj = None
                ss4 = None
                rn4 = None
                hmo = None
                tmpS = None
                pT = None
                dn4 = None
                hao = None
                sg1 = mk(f"sg1_{l}", [128, G], F32, ms)
                acc = sg1
                for i in range(TPG):
                    E("dve", lambda e, i=i: e.memset(ktok[i][:], 0.0), w=[ktok[i]])
                sg2 = None

                for i in range(2):
                    DMA("pool", wbr[:, i, :, :], wbr_d[l, i].rearrange("(k p) n -> p k n", p=128), w=[wbr], key="wbr")
                for k in range(8):
                    DMA("pool", wout[:, k, :], wout_d[l, k * 128:(k + 1) * 128, :], w=[wout], key="wout")
                flag = pg("flag")
                E("act", lambda e: e.activation(esink[:], pl("sinks"), AF.Exp), r=[prml], w=[esink])
                for i in range(3):
                    E("dve", lambda e, i=i: e.memset(vaug[i][:], 1.0), w=[vaug[i]])
                ib_d = nc.dram_tensor(f"ib{l}", [128, 656], F32)
                ob_d = nc.dram_tensor(f"ob{l}", [256, 656], F32)
                ibv = ib_d.ap()
                obv = ob_d.ap()

                class RO:
                    def __init__(self, n):
                        self.r = Res(n)
                ib_parts = [RO(f"ibp{i}_{l}") for i in range(7)]
                obR = RO(f"ob_{l}")
                pieces = [(0, 512), (512, 512), (1024, 392), (1416, 512), (1928, 512),
                          (2440, 512), (2952, 512), (3464, 512), (3976, 512)]

                class _Stop(Exception):
                    pass

                def chk(k):
                    if dbg == k:
                        raise _Stop()
                CUR = {"free": list(range(8))}
                ada_es = ExitStack()
                aring = [mk(f"aring{i}_{l}", [128, 8, 512], BF16, ada_es) for i in range(3)]
                prmb = mk(f"prmb_{l}", [128, 2048], F32, ada_es)
                DMA("sp", prmb[:], prmb_d[l], w=[prmb], key="prmb")
                cond_bc = mk(f"cond_bc_{l}", [128, 8, 128], BF16, ada_es)
                E("dve", lambda e: e.tensor_copy(cond_bc[:], cond_f[:].unsqueeze(2).to_broadcast([128, 8, 128])),
                  r=[cond_f], w=[cond_bc])

                def ada_issue(j, l=l):
                    rb_ = aring[j % 3]
                    DMA("pool", rb_[:], wada_d[l, :, j * 512:(j + 1) * 512].rearrange("(k p) n -> p k n", p=128),
                        w=[rb_], key=f"aring{j % 3}")

                def ada_steps(js, l=l):
                    ada_issue(js[0])
                    for ji, j in enumerate(js):
                        if ji + 1 < len(js):
                            ada_issue(js[ji + 1])
                        yield
                        while not CUR["free"]:
                            yield
                        bi_ = CUR["free"].pop(0)
                        b = banks[bi_]
                        rb_ = aring[j % 3]
                        if j in (4, 5, 10, 11):
                            gb = g1bc if j < 6 else g2bc
                            boff_ = 0 if j < 6 else 1024
                            c0 = (j % 2) * 512 if j < 6 else (j - 10) * 512
                            mm_group(b[:], [(cond_bc[:, k, :], rb_[:, k, :]) for k in range(8)], r=[cond_bc, rb_], w=[b])
                            E("dve", lambda e, b=b, gb=gb, boff_=boff_, c0=c0: e.tensor_tensor(
                                gb[:, c0:c0 + 512], b[:], prmb[:, boff_ + c0:boff_ + c0 + 512], ALU.add), r=[b, prmb], w=[gb])
                        else:
                            for jt in range(4):
                                mm_group(b[:, jt:jt + 1],
                                         [(rb_[:, k, jt * 128:(jt + 1) * 128], cond_bf[:, k:k + 1]) for k in range(8)],
                                         r=[cond_bf, rb_], w=[b])
                            E("dve", lambda e, b=b, j=j: e.tensor_tensor(modc[:, 4 * j:4 * j + 4], b[:, 0:4],
                                                                         pl("bada", 4 * j, 4 * j + 4), ALU.add),
                              r=[b, prml], w=[modc])
                        CUR["free"].append(bi_)
                        yield

                for _ in ada_steps([0, 1, 2, 3]):
                    pass
                E("dve", lambda e: e.scalar_tensor_tensor(a1[:], modc[:, 8:16], 1.0, pl("wn1"), ALU.add, ALU.mult),
                  r=[modc, prml], w=[a1])

                def ada_rest():
                    yield from ada_steps([4, 5, 6, 7, 8, 9, 10, 11])
                    E("dve", lambda e: e.scalar_tensor_tensor(a2[:], modc[:, 32:40], 1.0, pl("wn2"), ALU.add, ALU.mult),
                      r=[modc, prml], w=[a2])
                background = [ada_rest()]
                sh1 = modc[:, 0:8]
                sh2 = modc[:, 24:32]


                p2s = ExitStack()
                for pss in (1, 2):
                    if pss == 1:
                        E("dve", lambda e: e.memset(state_f[:], 0.0), w=[state_f])
                        E("dve", lambda e: e.memset(state_b[:], 0.0), w=[state_b])
                        for i in range(4):
                            E("dve", lambda e, i=i: e.memset(qkpre[i][:, 0:3], 0.0), w=[qkpre[i]])
                        E("dve", lambda e: e.memset(kaT[:, :, 0:128], 0.0), w=[kaT])
                    else:
                        for gb_ in background:
                            for _ in gb_:
                                pass
                        background = []
                        sy.barrier(skip=(f"cc{l}",))
                        ada_es.close()
                        qaT = mk(f"qaT_{l}", [128, 4, G], BF16, p2s)
                        hmT = mk(f"hmT_{l}", [128, 4, G], BF16, p2s)
                        haT = mk(f"haT_{l}", [128, 4, G], BF16, p2s)
                        mT = mk(f"mT_{l}", [128, 8, G], BF16, p2s)
                        som = [mk(f"som{i}_{l}", [128, 512], BF16, p2s) for i in range(TPG)]
                        sd = [mk(f"sd{i}_{l}", [128, 4, 128], BF16, p2s) for i in range(TPG)]
                        den4 = [mk(f"den4{i}_{l}", [128, 4], F32, p2s) for i in range(TPG)]
                        r4 = [mk(f"r4{i}_{l}", [128, 4], F32, p2s) for i in range(TPG)]
                        hm = [mk(f"hm{i}_{l}", [128, 4, 128], F32, p2s) for i in range(TPG)]
                        sqj = [mk(f"sqj{i}_{l}", [128, 128], F32, p2s) for i in range(TPG)]
                        ss4 = [mk(f"ss4{i}_{l}", [128, 4], F32, p2s) for i in range(TPG)]
                        rn4 = [mk(f"rn4{i}_{l}", [128, 4], F32, p2s) for i in range(TPG)]
                        hmo = [mk(f"hmo{i}_{l}", [128, 512], BF16, p2s) for i in range(TPG)]
                        tmpS = [mk(f"tmpS{i}_{l}", [128, 2, 512], F32, p2s) for i in range(TPG)]
                        pT = [mk(f"pT{i}_{l}", [128, 2, 512], BF16, p2s) for i in range(TPG)]
                        dn4 = [mk(f"dn4{i}_{l}", [128, 4], F32, p2s) for i in range(TPG)]
                        hao = [mk(f"hao{i}_{l}", [128, 512], BF16, p2s) for i in range(TPG)]
                        sg2 = mk(f"sg2_{l}", [128, G], F32, p2s)
                        for k in range(8):
                            E("dve", lambda e, k=k: e.tensor_tensor(wout[:, k, :], wout[:, k, :], g1bc[:], ALU.mult),
                              r=[wout, g1bc], w=[wout])
                        DMA("sp", state_f[:], obv[0:128, 0:258].rearrange("p (a b) -> p a b", a=2), r=[obR], w=[state_f], key="h_st")
                        E("dve", lambda e: e.tensor_scalar(state_f[:], state_f[:], flag[:, 0:1], None, ALU.mult),
                          r=[state_f, prmg], w=[state_f])
                        E("act", lambda e: e.copy(state_b[:], state_f[:]), r=[state_f], w=[state_b])
                        for i in range(4):
                            DMA("sp", qkpre[i][:, 0:3], obv[0:128, 258 + 3 * i:261 + 3 * i], r=[obR], w=[qkpre[i]], key=f"h_cv{i}")
                            E("dve", lambda e, i=i: e.tensor_scalar(qkpre[i][:, 0:3], qkpre[i][:, 0:3], flag[:, 0:1], None,
                                                                      ALU.mult), r=[qkpre[i], prmg], w=[qkpre[i]])
                        DMA("pool", kaT[:, :, 0:128], obv[0:128, 270:526].rearrange("p (a b) -> p a b", a=2), r=[obR], w=[kaT], key="h_ka")
                        DMA("pool", vhalo[:, :, 0:65], obv[0:128, 526:656].rearrange("p (a b) -> p a b", a=2), r=[obR], w=[vhalo], key="h_va")
                        E("dve", lambda e: e.tensor_scalar(vhalo[:, :, 0:65], vhalo[:, :, 0:65], flag[:, 0:1], None, ALU.mult),
                          r=[vhalo, prmg], w=[vhalo])
                    pcs = (2, 0, 3) if pss == 1 else (1, 2, 4, 0, 3, 5, 6, 7, 8)
                    seq = [(g, p) for g in range(NG) for p in pcs]
                    rstate = {"issued": 0}

                    def ring_issue(seq=seq, rstate=rstate):
                        i = rstate["issued"]
                        if i >= len(seq):
                            return
                        _, p = seq[i]
                        c0, wd_ = pieces[p]
                        rb_ = ring[i % 3]
                        DMA("pool", rb_[:, :, 0:wd_], win_d[l, :, c0:c0 + wd_].rearrange("(k p) n -> p k n", p=128),
                            w=[rb_], key=f"ring{i % 3}")
                        rstate["issued"] += 1

                    ring_issue()
                    ring_issue()
                    uidx = {"i": 0}

                    def ring_get(uidx=uidx):
                        i = uidx["i"]
                        uidx["i"] += 1
                        return ring[i % 3]

                    try:
                        for g in range(NG):
                            if dbg <= 3 or (dbg <= 30 and g >= 1):
                                break
                            hT = hT2[g % 2]
                            if g == 0:
                                for tt in range(TPG):
                                    norm_tile(g * TPG + tt, a1, sh1, hT, tt * 128, nsc)
                            chk(11)
                            if pss == 2:
                                rb_ = ring_get()
                                for mt in range(4):
                                    b = bank()
                                    mm_group(b[:, 0:G], [(rb_[:, k, mt * 128:(mt + 1) * 128], hT[:, k, :]) for k in range(8)],
                                             r=[rb_, hT], w=[b])
                                    E("act", lambda e, b=b, mt=mt: e.copy(qaT[:, mt, :], b[:, 0:G]), r=[b], w=[qaT])
                                ring_issue()
                            chk(13)
                            rb_ = ring_get()
                            for j in range(2):
                                b = bank()
                                mm_group(b[:, 0:G], [(rb_[:, k, j * 128:(j + 1) * 128], hT[:, k, :]) for k in range(8)],
                                         r=[rb_, hT], w=[b])
                                E("act", lambda e, b=b, j=j: e.copy(kaT[:, j, 128:128 + G], b[:, 0:G]), r=[b], w=[kaT])
                            chk(131)
                            dfree = list(range(8))

                            def dalloc():
                                while not dfree:
                                    yield
                                return banks[dfree.pop(0)]

                            def drel(b_):
                                dfree.append(banks.index(b_))

                            def gate_gen(tt, g=g):
                                t = g * TPG + tt
                                va = vaug[t % 3]
                                b = yield from dalloc()
                                yield
                                mm_group(b[:, 0:136], [(hT[:, k, tt * 128:(tt + 1) * 128], rb_[:, k, 256:392]) for k in range(8)],
                                         r=[rb_, hT], w=[b])
                                yield
                                E("dve", lambda e, b=b, tt=tt: e.tensor_tensor(gsb[tt][:], b[:, 0:8], pl("bif"), ALU.add),
                                  r=[b, prml], w=[gsb[tt]])
                                yield
                                E("act", lambda e, b=b, va=va: e.copy(va[:, :, 0:64],
                                                                        b[:, 8:136].rearrange("p (a b) -> p a b", a=2)),
                                  r=[b], w=[va])
                                drel(b)
                                yield
                                E("act", lambda e, tt=tt: e.activation(ef4[tt][:], gsb[tt][:, 4:8], AF.Exp, scale=-1.0),
                                  r=[gsb[tt]], w=[ef4[tt]])
                                yield
                                E("act", lambda e, tt=tt: e.activation(ef4[tt][:], ef4[tt][:], AF.Ln, bias=1.0),
                                  r=[ef4[tt]], w=[ef4[tt]])
                                yield
                                E("dve", lambda e, tt=tt: e.tensor_scalar(logf[tt][:], ef4[tt][:], -1.0, None, ALU.mult),
                                  r=[ef4[tt]], w=[logf[tt]])
                                yield
                                b2 = yield from dalloc()
                                yield
                                E("dve", lambda e, tt=tt: e.tensor_copy(lhi[tt][:], logf[tt][:]), r=[logf[tt]], w=[lhi[tt]])
                                yield
                                E("dve", lambda e, tt=tt: e.tensor_tensor(llo[tt][:], logf[tt][:], lhi[tt][:], ALU.subtract),
                                  r=[logf[tt], lhi[tt]], w=[llo[tt]])
                                yield
                                mm_group(b2[:, 0:4], [(tri_b[:], lhi[tt][:]), (tri_b[:], llo[tt][:])],
                                         r=[tri_b, lhi[tt], llo[tt]], w=[b2])
                                yield
                                mm_group(b2[:, 4:8], [(ones_b[:], lhi[tt][:]), (ones_b[:], llo[tt][:])],
                                         r=[ones_b, lhi[tt], llo[tt]], w=[b2])
                                yield
                                E("dve", lambda e, b2=b2, tt=tt: e.tensor_tensor(w4[tt][:], b2[:, 0:4], gsb[tt][:, 0:4],
                                                                                   ALU.subtract), r=[gsb[tt], b2], w=[w4[tt]])
                                yield
                                E("act", lambda e, tt=tt: e.activation(w4[tt][:], w4[tt][:], AF.Exp, scale=-1.0), r=[w4[tt]], w=[w4[tt]])
                                yield
                                E("act", lambda e, b2=b2, tt=tt: e.activation(sc4[tt][:], b2[:, 0:4], AF.Exp),
                                  r=[b2], w=[sc4[tt]])
                                yield
                                E("act", lambda e, b2=b2, tt=tt: e.activation(eb4[tt][:], b2[:, 4:8], AF.Exp),
                                  r=[b2], w=[eb4[tt]])
                                drel(b2)
                                eb2 = eb4[tt][:, 0:4].rearrange("p (j r) -> p j r", r=2)
                                yield
                                E("act", lambda e, tt=tt, eb2=eb2: e.copy(ebsel[tt][0:64, :], eb2[0:64, :, 0]),
                                  r=[eb4[tt]], w=[ebsel[tt]])
                                E("act", lambda e, tt=tt, eb2=eb2: e.copy(ebsel[tt][64:128, :], eb2[64:128, :, 1]),
                                  r=[eb4[tt]], w=[ebsel[tt]])

                            def pe_gen(g=g):
                                if pss == 2:
                                    rb_ = ring_get()
                                    for tt in range(TPG):
                                        b = yield from dalloc()
                                        mm_group(b[:], [(hT[:, k, tt * 128:(tt + 1) * 128], rb_[:, k, :]) for k in range(8)],
                                                 r=[rb_, hT], w=[b])
                                        E("act", lambda e, b=b, tt=tt: e.activation(som[tt][:], b[:], AF.Sigmoid), r=[b], w=[som[tt]])
                                        E("dve", lambda e, tt=tt: e.tensor_tensor(som[tt][:], som[tt][:], pl("wmn"), ALU.mult),
                                          r=[som[tt], prml], w=[som[tt]])
                                        drel(b)
                                        yield
                                    ring_issue()
                                rb_ = ring_get()
                                for mt in range(4):
                                    if pss == 1 and mt < 2 and g < NG - 1:
                                        continue
                                    b = yield from dalloc()
                                    mm_group(b[:, 0:G], [(rb_[:, k, mt * 128:(mt + 1) * 128], hT[:, k, :]) for k in range(8)],
                                             r=[rb_, hT], w=[b])
                                    E("act", lambda e, b=b, mt=mt: e.copy(qkpre[mt][:, 3:3 + G], b[:, 0:G]), r=[b], w=[qkpre[mt]])
                                    drel(b)
                                    yield
                                    if pss == 2 or mt >= 2:
                                        cw = lambda j, mt=mt: pl("convw", mt * 4 + j, mt * 4 + j + 1)
                                        E("dve", lambda e, mt=mt, cw=cw: e.tensor_scalar(acc[:], qkpre[mt][:, 0:G], cw(0),
                                                                                           pl("convb", mt, mt + 1), ALU.mult, ALU.add),
                                          r=[qkpre[mt], prml], w=[acc])
                                        for j in range(1, 4):
                                            E("dve", lambda e, mt=mt, j=j, cw=cw: e.scalar_tensor_tensor(
                                                acc[:], qkpre[mt][:, j:j + G], cw(j), acc[:], ALU.mult, ALU.add),
                                              r=[qkpre[mt], prml, acc], w=[acc])
                                        E("act", lambda e, mt=mt: e.activation(qkT[:, mt, :], acc[:], AF.Silu), r=[acc], w=[qkT])
                                    E("act", lambda e, mt=mt: e.copy(qkpre[mt][:, 0:3], qkpre[mt][:, G:G + 3]),
                                      r=[qkpre[mt]], w=[qkpre[mt]])
                                    yield
                                ring_issue()
                                chk(12)
                            ggs = [gate_gen(tt) for tt in range(TPG)] + [pe_gen()]
                            while ggs:
                                for gg_ in list(ggs):
                                    try:
                                        next(gg_)
                                    except StopIteration:
                                        ggs.remove(gg_)
                            chk(138)
                            ring_issue()
                            chk(14)
                            rb_ = ring_get()
                            for tt in range(TPG):
                                b = bank()
                                mm_group(b[:], [(hT[:, k, tt * 128:(tt + 1) * 128], rb_[:, k, :]) for k in range(8)],
                                         r=[rb_, hT], w=[b])
                                E("dve", lambda e, b=b, tt=tt: e.tensor_tensor(
                                    vpr[tt][:, :, 0:128], b[:].rearrange("p (a b) -> p a b", a=4),
                                    w4[tt][:].unsqueeze(2).to_broadcast([128, 4, 128]), ALU.mult), r=[b, w4[tt]], w=[vpr[tt]])
                                E("dve", lambda e, tt=tt: e.tensor_copy(vpr[tt][:, :, 128:129], w4[tt][:].unsqueeze(2)),
                                  r=[w4[tt]], w=[vpr[tt]])
                            ring_issue()
                            chk(15)
                            chk(16)
                            free_banks = list(range(8))
                            CUR["free"] = free_banks

                            def galloc():
                                while not free_banks:
                                    yield
                                return banks[free_banks.pop(0)]

                            def grel(b_):
                                free_banks.append(banks.index(b_))

                            def mlstm_gen(tt, g=g):
                                t = g * TPG + tt
                                cs = slice(tt * 128, (tt + 1) * 128)
                                ktok_ = ktok[tt]
                                if pss == 2:
                                    sd_, den4_, r4_, hm_, ss4_, rn4_, hmo_, sqj_ = (
                                        sd[tt], den4[tt], r4[tt], hm[tt], ss4[tt], rn4[tt], hmo[tt], sqj[tt])
                                b = yield from galloc()
                                bb = b[:].bitcast(BF16)
                                for j in range(2):
                                    E("pe", lambda e, j=j, bb=bb: e.transpose(bb[:, j * 128:(j + 1) * 128], qkT[:, 2 + j, cs],
                                                                               ident_b[:]),
                                      r=[qkT, ident_b], w=[b], inc=(j == 1))
                                bb3 = bb[:, 0:256].rearrange("p (j c) -> p j c", j=2)
                                for w_ in range(2):
                                    E("act", lambda e, w_=w_: e.copy(ktok_[:, :, w_, w_ * 64:(w_ + 1) * 64],
                                                                     bb3[:, :, w_ * 64:(w_ + 1) * 64]), r=[b], w=[ktok_])
                                grel(b)
                                if pss == 2:
                                    bsr = [(yield from galloc()), (yield from galloc())]
                                    for h in range(4):
                                        p0 = 64 * (h % 2)
                                        rg, jj = h % 2, h // 2
                                        E("pe", lambda e, h=h, p0=p0, rg=rg, jj=jj: e.matmul(
                                            bsr[rg][:, jj * 128:(jj + 1) * 128], qkT[p0:p0 + 64, 2 + h // 2, cs],
                                            qkT[p0:p0 + 64, h // 2, cs], start=True, stop=True), r=[qkT], w=[bsr[rg]], inc=True)
                                    sd4 = sd_[:].rearrange("p (j r) l -> p j r l", r=2)
                                    for rg in range(2):
                                        E("dve", lambda e, rg=rg: e.tensor_tensor(
                                            sd4[:, :, rg, :], bsr[rg][:, 0:256].rearrange("p (a b) -> p a b", a=2),
                                            tri_f.unsqueeze(1).to_broadcast([128, 2, 128]), ALU.mult), r=[bsr[rg], cst], w=[sd_])
                                    grel(bsr[0])
                                    grel(bsr[1])
                                    bn = [(yield from galloc()), (yield from galloc())]
                                    for h in range(4):
                                        p0 = 64 * (h % 2)
                                        o = bn[h // 2][:, (h % 2) * 256:(h % 2) * 256 + 129]
                                        E("pe", lambda e, h=h, o=o: e.matmul(o, sd_[:, h, :], vpr[tt][:, h, 0:129], start=True,
                                                                             stop=False),
                                          r=[sd_, vpr[tt]], w=[bn[h // 2]], inc=False)
                                        E("pe", lambda e, h=h, o=o, p0=p0: e.matmul(o, qkT[p0:p0 + 64, h // 2, cs],
                                                                                    state_b[p0:p0 + 64, h // 2, :],
                                                                                    start=False, stop=True),
                                          r=[qkT, state_b], w=[bn[h // 2]], inc=True)
                                bd = yield from galloc()
                                for j in range(2):
                                    o = bd[:, j * 256:j * 256 + 129]
                                    E("pe", lambda e, j=j, o=o: e.matmul(o, ktok_[:, j, 0, :], vpr[tt][:, 2 * j, 0:129],
                                                                         start=True, stop=False),
                                      r=[ktok_, vpr[tt]], w=[bd], inc=False)
                                    E("pe", lambda e, j=j, o=o: e.matmul(o, ktok_[:, j, 1, :], vpr[tt][:, 2 * j + 1, 0:129],
                                                                         start=False, stop=True),
                                      r=[ktok_, vpr[tt]], w=[bd], inc=True)
                                E("dve", lambda e: e.tensor_tensor(
                                    state_f[:], bd[:, 0:512].rearrange("p (j c) -> p j c", j=2)[:, :, 0:129], state_f[:],
                                    ALU.add), r=[state_f, bd], w=[state_f])
                                E("dve", lambda e: e.tensor_tensor(
                                    state_f[:], state_f[:], ebsel[tt][:].unsqueeze(2).to_broadcast([128, 2, 129]), ALU.mult),
                                  r=[state_f, ebsel[tt]], w=[state_f])
                                E("act", lambda e: e.copy(state_b[:], state_f[:]), r=[state_f], w=[state_b])
                                grel(bd)
                                yield "state_done"
                                if pss != 2:
                                    return
                                for j in range(2):
                                    v3 = bn[j][:].rearrange("p (a b) -> p a b", a=2)
                                    E("dve", lambda e, j=j, v3=v3: e.scalar_tensor_tensor(
                                        den4_[:, 2 * j:2 * j + 2], v3[:, :, 128], 0.125, sc4[tt][:, 2 * j:2 * j + 2],
                                        ALU.mult, ALU.mult), r=[bn[j], sc4[tt]], w=[den4_])
                                E("act", lambda e: e.activation(den4_[:], den4_[:], AF.Abs), r=[den4_], w=[den4_])
                                yield
                                E("dve", lambda e: e.tensor_scalar(den4_[:], den4_[:], 1.0, None, ALU.max), r=[den4_], w=[den4_])
                                E("dve", lambda e: e.reciprocal(den4_[:], den4_[:]), r=[den4_], w=[den4_])
                                E("dve", lambda e: e.scalar_tensor_tensor(r4_[:], sc4[tt][:], 0.125, den4_[:], ALU.mult, ALU.mult),
                                  r=[sc4[tt], den4_], w=[r4_])
                                yield
                                for j in range(2):
                                    v3 = bn[j][:].rearrange("p (a b) -> p a b", a=2)
                                    E("dve", lambda e, j=j, v3=v3: e.tensor_tensor(
                                        hm_[:, 2 * j:2 * j + 2, :], v3[:, :, 0:128],
                                        r4_[:, 2 * j:2 * j + 2].unsqueeze(2).to_broadcast([128, 2, 128]), ALU.mult),
                                      r=[bn[j], r4_], w=[hm_])
                                E("dve", lambda e: e.memset(ss4_[:], 0.0), w=[ss4_])
                                grel(bn[0])
                                grel(bn[1])
                                yield
                                for h in range(4):
                                    E("act", lambda e, h=h: e.activation(sqj_[:], hm_[:, h, :], AF.Square,
                                                                         accum_out=ss4_[:, h:h + 1]),
                                      r=[hm_, ss4_], w=[sqj_, ss4_])
                                yield
                                E("dve", lambda e: e.tensor_scalar(ss4_[:], ss4_[:], 1.0 / 128, EPS, ALU.mult, ALU.add),
                                  r=[ss4_], w=[ss4_])
                                E("act", lambda e: e.sqrt(ss4_[:], ss4_[:]), r=[ss4_], w=[ss4_])
                                yield
                                E("dve", lambda e: e.reciprocal(rn4_[:], ss4_[:]), r=[ss4_], w=[rn4_])
                                E("dve", lambda e: e.tensor_tensor(hm_[:], hm_[:], rn4_[:].unsqueeze(2).to_broadcast([128, 4, 128]),
                                                                   ALU.mult), r=[hm_, rn4_], w=[hm_])
                                yield
                                hm2 = hm_[:].rearrange("p a b -> p (a b)")
                                E("dve", lambda e: e.tensor_tensor(hmo_[:], hm2, som[tt][:], ALU.mult), r=[hm_, som[tt]], w=[hmo_])
                                yield
                                b = yield from galloc()
                                bb = b[:].bitcast(BF16)
                                for f in range(4):
                                    E("pe", lambda e, f=f, bb=bb: e.transpose(bb[:, f * 128:(f + 1) * 128],
                                                                               hmo_[:, f * 128:(f + 1) * 128], ident_b[:]),
                                      r=[hmo_, ident_b], w=[b], inc=(f == 3))
                                E("act", lambda e, bb=bb: e.copy(hmT[:, :, cs], bb[:, 0:512].rearrange("p (a b) -> p a b", a=4)),
                                  r=[b], w=[hmT])
                                grel(b)

                            def swa_gen(tt, g=g):
                                t = g * TPG + tt
                                cs = slice(tt * 128, (tt + 1) * 128)
                                tmpS_, pT_, dn4_, hao_ = tmpS[tt], pT[tt], dn4[tt], hao[tt]
                                vprev = vaug[(t - 1) % 3] if t > 0 else vhalo
                                vcur = vaug[t % 3]
                                pc = slice(tt * 128, tt * 128 + 128)
                                cc = slice(128 + tt * 128, 256 + tt * 128)
                                for kv in range(2):
                                    bS = [(yield from galloc()), (yield from galloc())]
                                    for rg in range(2):
                                        for bi, ks in enumerate((pc, cc)):
                                            for gg in range(2):
                                                g4 = gg * 2 + rg
                                                h = kv * 4 + g4
                                                p0 = 64 * rg
                                                sl_ = (bi * 2 + gg) * 128
                                                E("pe", lambda e, rg=rg, ks=ks, sl_=sl_, h=h, p0=p0: e.matmul(
                                                    bS[rg][:, sl_:sl_ + 128], kaT[p0:p0 + 64, kv, ks],
                                                    qaT[p0:p0 + 64, h // 2, cs], start=True, stop=True),
                                                  r=[kaT, qaT], w=[bS[rg]], inc=True)
                                        boff = (kv * 2 + rg) * 512
                                        E("dve", lambda e, rg=rg, boff=boff: e.scalar_tensor_tensor(
                                            tmpS_[:, rg, :], bS[rg][:], 0.125, biasT[:, boff:boff + 512], ALU.mult, ALU.add),
                                          r=[bS[rg], biasT], w=[tmpS_])
                                    grel(bS[0])
                                    grel(bS[1])
                                    yield
                                    E("act", lambda e: e.activation(pT_[:], tmpS_[:], AF.Exp), r=[tmpS_], w=[pT_])
                                    yield
                                    bp = yield from galloc()
                                    for g4 in range(4):
                                        o = bp[:, g4 * 65:(g4 + 1) * 65]
                                        rg, gg = g4 % 2, g4 // 2
                                        E("pe", lambda e, rg=rg, gg=gg, o=o: e.matmul(
                                            o, pT_[:, rg, gg * 128:(gg + 1) * 128], vprev[:, kv, 0:65], start=True, stop=False),
                                          r=[pT_, vprev], w=[bp], inc=False)
                                        E("pe", lambda e, rg=rg, gg=gg, o=o: e.matmul(
                                            o, pT_[:, rg, (2 + gg) * 128:(3 + gg) * 128], vcur[:, kv, 0:65], start=False,
                                            stop=True), r=[pT_, vcur], w=[bp], inc=True)
                                    yield
                                    pv3 = bp[:, 0:260].rearrange("p (a b) -> p a b", a=4)
                                    E("dve", lambda e, pv3=pv3: e.tensor_tensor(dn4_[:], pv3[:, :, 64],
                                                                                esink[:, kv * 4:(kv + 1) * 4], ALU.add),
                                      r=[bp, esink], w=[dn4_])
                                    E("dve", lambda e: e.reciprocal(dn4_[:], dn4_[:]), r=[dn4_], w=[dn4_])
                                    E("dve", lambda e, pv3=pv3: e.tensor_tensor(
                                        hao_[:, kv * 256:(kv + 1) * 256].rearrange("p (a b) -> p a b", a=4), pv3[:, :, 0:64],
                                        dn4_[:].unsqueeze(2).to_broadcast([128, 4, 64]), ALU.mult), r=[bp, dn4_], w=[hao_])
                                    grel(bp)
                                    yield
                                b = yield from galloc()
                                bb = b[:].bitcast(BF16)
                                for f in range(4):
                                    E("pe", lambda e, f=f, bb=bb: e.transpose(bb[:, f * 128:(f + 1) * 128],
                                                                               hao_[:, f * 128:(f + 1) * 128], ident_b[:]),
                                      r=[hao_, ident_b], w=[b], inc=(f == 3))
                                E("act", lambda e, bb=bb: e.copy(haT[:, :, cs], bb[:, 0:512].rearrange("p (a b) -> p a b", a=4)),
                                  r=[b], w=[haT])
                                grel(b)

                            pending_m = [mlstm_gen(tt) for tt in range(TPG)]
                            if os.environ.get("KSEQ") == "1":
                                for gm in pending_m:
                                    for _ in gm:
                                        pass
                                pending_m = []
                                if pss == 2:
                                    for tt in range(TPG):
                                        for _ in swa_gen(tt):
                                            pass
                                pss_swa = False
                            elif os.environ.get("KSEQ") == "2":
                                pss_swa = False
                            else:
                                pss_swa = (pss == 2)
                            chains = []
                            if pss_swa:
                                chains += [swa_gen(tt) for tt in range(TPG)]
                            if g + 1 < NG and os.environ.get("KSEQ") is None:
                                def nnext_gen(g=g):
                                    for tt in range(TPG):
                                        yield from norm_gen((g + 1) * TPG + tt, a1, sh1, hT2[(g + 1) % 2], tt * 128, nsc,
                                                            alloc=galloc, rel=grel)
                                chains.append(nnext_gen())
                            elif g + 1 < NG:
                                for tt in range(TPG):
                                    norm_tile((g + 1) * TPG + tt, a1, sh1, hT2[(g + 1) % 2], tt * 128, nsc)
                            started = 0
                            mchains = []
                            next_ok = True
                            while pending_m or chains or mchains:
                                if pending_m and next_ok:
                                    gm = pending_m.pop(0)
                                    mchains.append(gm)
                                    next_ok = False
                                    fresh = gm
                                else:
                                    fresh = None
                                for gm in list(mchains):
                                    try:
                                        v = next(gm)
                                        if v == "state_done":
                                            next_ok = True
                                    except StopIteration:
                                        mchains.remove(gm)
                                        if gm is fresh or True:
                                            pass
                                for gs in list(chains):
                                    try:
                                        next(gs)
                                    except StopIteration:
                                        chains.remove(gs)
                                for gb_ in list(background):
                                    try:
                                        next(gb_)
                                    except StopIteration:
                                        background.remove(gb_)
                            if os.environ.get("KSEQ") == "2" and pss == 2:
                                for tt in range(TPG):
                                    for _ in swa_gen(tt):
                                        pass
                            chk(23)
                            E("dve", lambda e: e.tensor_copy(kaT[:, :, 0:128], kaT[:, :, G:G + 128]), r=[kaT], w=[kaT])
                            chk(24)
                            if pss == 2:
                                for pi in range(4):
                                    rb_ = ring_get()
                                    for oi in range(2):
                                        ot = 2 * pi + oi
                                        bgm, bga, bbm, bba = bank(), bank(), bank(), bank()
                                        mm_group(bgm[:, 0:G], [(rb_[:, k, (2 * oi) * 128:(2 * oi + 1) * 128], hT[:, k, :])
                                                               for k in range(8)], r=[rb_, hT], w=[bgm])
                                        mm_group(bga[:, 0:G], [(rb_[:, k, (2 * oi + 1) * 128:(2 * oi + 2) * 128], hT[:, k, :])
                                                               for k in range(8)], r=[rb_, hT], w=[bga])
                                        mm_group(bbm[:, 0:G], [(wbr[:, 0, k, ot * 128:(ot + 1) * 128], hmT[:, k, :]) for k in range(4)],
                                                 r=[wbr, hmT], w=[bbm])
                                        mm_group(bba[:, 0:G], [(wbr[:, 1, k, ot * 128:(ot + 1) * 128], haT[:, k, :]) for k in range(4)],
                                                 r=[wbr, haT], w=[bba])
                                        E("act", lambda e, bgm=bgm: e.activation(sg1[:], bgm[:, 0:G], AF.Sigmoid), r=[bgm], w=[sg1])
                                        E("act", lambda e, bga=bga: e.activation(sg2[:], bga[:, 0:G], AF.Sigmoid), r=[bga], w=[sg2])
                                        E("dve", lambda e, bbm=bbm: e.tensor_tensor(sg1[:], bbm[:, 0:G], sg1[:], ALU.mult),
                                          r=[sg1, bbm], w=[sg1])
                                        E("dve", lambda e, bba=bba: e.tensor_tensor(sg2[:], bba[:, 0:G], sg2[:], ALU.mult),
                                          r=[sg2, bba], w=[sg2])
                                        E("dve", lambda e, ot=ot: e.tensor_tensor(mT[:, ot, :], sg1[:], sg2[:], ALU.add),
                                          r=[sg1, sg2], w=[mT])
                                    ring_issue()
                            chk(25)
                            for tt in (range(TPG) if pss == 2 else ()):
                                t = g * TPG + tt
                                for oc in range(2):
                                    b = bank()
                                    mm_group(b[:], [(mT[:, k, tt * 128:(tt + 1) * 128], wout[:, k, oc * 512:(oc + 1) * 512])
                                                    for k in range(8)], r=[mT, wout], w=[b])
                                    E("dve", lambda e, b=b, t=t, oc=oc: e.tensor_tensor(
                                        x[:, t, oc * 512:(oc + 1) * 512], b[:], x[:, t, oc * 512:(oc + 1) * 512], ALU.add),
                                      r=[b, xt[t]], w=[xt[t]])
                    except _Stop:
                        pass
                    if pss == 1:
                        lastc = slice(128 + (TPG - 1) * 128, 256 + (TPG - 1) * 128)
                        vlast = vaug[(NT - 1) % 3]
                        DMA("sp", ibv[:, 0:258].rearrange("p (a b) -> p a b", a=2), state_f[:], r=[state_f], w=[ib_parts[0]], key="x_st")
                        for i in range(4):
                            DMA("sp", ibv[:, 258 + 3 * i:261 + 3 * i], qkpre[i][:, 0:3], r=[qkpre[i]], w=[ib_parts[1 + i]], key=f"x_cv{i}")
                        DMA("pool", ibv[:, 270:526].rearrange("p (a b) -> p a b", a=2), kaT[:, :, 0:128], r=[kaT], w=[ib_parts[5]], key="x_ka")
                        DMA("pool", ibv[:, 526:656].rearrange("p (a b) -> p a b", a=2), vlast[:, :, 0:65], r=[vlast], w=[ib_parts[6]], key="x_va")
                        sy.emit_cc(lambda e: e.collective_compute("AllGather", ALU.bypass,
                                                                  replica_groups=[[2 * i, 2 * i + 1] for i in range(NCORES // 2)],
                                                                  ins=[ibv.opt()], outs=[obv.opt()]),
                                   reads=[t.r for t in ib_parts], writes=[obR.r], key=f"cc{l}")
                sy.barrier()
                p2s.close()

            if not moe:
                continue
            with ExitStack() as ms:
                h2T = mk(f"h2T_{l}", [128, 8, NTOK], BF16, ms)
                gates = mk(f"gates_{l}", [128, NT, NE], F32, ms)
                xnT2 = [mk(f"xnT{i}_{l}", [128, D], F32, ms) for i in range(2)]
                xnT = xnT2[0]
                wr2 = mk(f"wr2_{l}", [128, 8, NE], F32, ms)
                cbc = mk(f"cbc_{l}", [128, NE], F32, ms)
                nsc2 = [(None, mk(f"ssumb{i}_{l}", [128, 1], F32, ms),
                         mk(f"tmp1b{i}_{l}", [128, 1], F32, ms), mk(f"tmp2b{i}_{l}", [128, 1], F32, ms),
                         mk(f"rstdb{i}_{l}", [128, 1], F32, ms), mk(f"xnb{i}_{l}", [128, D], F32, ms)) for i in range(2)]
                wgb = [mk(f"wgb{i}_{l}", [128, 8, DE], BF16, ms) for i in range(2)]
                wub = [mk(f"wub{i}_{l}", [128, 8, DE], BF16, ms) for i in range(2)]
                wdb = [mk(f"wdb{i}_{l}", [128, 4, D], BF16, ms) for i in range(2)]
                actT = [mk(f"actT{i}_{l}", [128, 4, 512], BF16, ms) for i in range(2)]
                sil = [mk(f"sil{i}_{l}", [128, 512], F32, ms) for i in range(2)]
                R16s = [{n: mk(f"r_{n}{i}_{l}", [128, 16], F32, ms) for n in
                         ["sc", "sel", "selm", "m1", "selm2", "m2", "wts"]} for i in range(2)]
                R4s = [{n: mk(f"q_{n}{i}_{l}", [128, 4], F32, ms) for n in
                        ["hi1", "lo1", "hi2", "lo2", "top1", "mm2", "mm3", "grp", "gmask"]} for i in range(2)]
                R1s = [{n: mk(f"s_{n}{i}_{l}", [128, 1], F32, ms) for n in ["gmax", "t1", "t2", "den"]} for i in range(2)]

                def load_expert(e_):
                    i = e_ % 2
                    DMA("pool", wgb[i][:], wg_d[l, e_].rearrange("(k p) n -> p k n", p=128), w=[wgb[i]], key=f"wg{i}")
                    DMA("pool", wub[i][:], wu_d[l, e_].rearrange("(k p) n -> p k n", p=128), w=[wub[i]], key=f"wu{i}")
                    DMA("pool", wdb[i][:], wd_d[l, e_].rearrange("(k p) n -> p k n", p=128), w=[wdb[i]], key=f"wd{i}")

                load_expert(0)
                load_expert(1)
                E("dve", lambda e: e.tensor_tensor(wr2[:], pg("wr").rearrange("p (a b) -> p a b", a=8),
                                                   a2[:].unsqueeze(2).to_broadcast([128, 8, NE]), ALU.mult),
                  r=[prmg, a2], w=[wr2])
                sh2bc = sil[0]
                sh2bc = xnT
                sh2v = sh2bc[:].rearrange("p (a b) -> p a b", a=8)
                E("dve", lambda e: e.tensor_copy(sh2v, sh2.unsqueeze(2).to_broadcast([128, 8, 128])),
                  r=[modc], w=[sh2bc])
                b = bank()
                mm_group(b[:, 0:NE], [(sh2v[:, k, :], pg("wr", k * NE, (k + 1) * NE)) for k in range(8)],
                         r=[sh2bc, prmg], w=[b])
                E("dve", lambda e, b=b: e.tensor_copy(cbc[:], b[:, 0:NE]), r=[b], w=[cbc])

                mfree = list(range(8))

                def malloc_():
                    while not mfree:
                        yield
                    return banks[mfree.pop(0)]

                def mrel(b_):
                    mfree.append(banks.index(b_))

                def tile_chain(t):
                    par = t % 2
                    xnT = xnT2[par]
                    R16, R4, R1 = R16s[par], R4s[par], R1s[par]
                    yield from norm_gen(t, a2, sh2, h2T, t * 128, nsc2[par], router_dst=xnT, alloc=malloc_, rel=mrel)
                    yield "norm_done"
                    b = yield from malloc_()
                    mm_group(b[:, 0:NE], [(xnT[:, k * 128:(k + 1) * 128], wr2[:, k, :]) for k in range(8)],
                             r=[xnT, wr2], w=[b])
                    r16, r4_, r1 = R16, R4, R1
                    E("dve", lambda e, b=b: e.tensor_tensor(r16["sel"][:], b[:, 0:NE], cbc[:], ALU.add),
                      r=[b, cbc], w=[r16["sel"]])
                    mrel(b)
                    yield
                    E("act", lambda e: e.activation(r16["sc"][:], r16["sel"][:], AF.Sigmoid), r=[r16["sel"]], w=[r16["sc"]])
                    E("dve", lambda e: e.tensor_tensor(r16["sel"][:], r16["sc"][:], pg("rb"), ALU.add),
                      r=[r16["sc"], prmg], w=[r16["sel"]])
                    s3 = r16["sel"][:].rearrange("p (a b) -> p a b", a=4)

                    def tt_(o, i0, i1, op, rr, ww):
                        E("dve", lambda e: e.tensor_tensor(o, i0, i1, op), r=rr, w=ww)
                    tt_(r4_["hi1"][:], s3[:, :, 0], s3[:, :, 1], ALU.max, [r16["sel"]], [r4_["hi1"]])
                    tt_(r4_["lo1"][:], s3[:, :, 0], s3[:, :, 1], ALU.min, [r16["sel"]], [r4_["lo1"]])
                    tt_(r4_["hi2"][:], s3[:, :, 2], s3[:, :, 3], ALU.max, [r16["sel"]], [r4_["hi2"]])
                    tt_(r4_["lo2"][:], s3[:, :, 2], s3[:, :, 3], ALU.min, [r16["sel"]], [r4_["lo2"]])
                    tt_(r4_["top1"][:], r4_["hi1"][:], r4_["hi2"][:], ALU.max, [r4_["hi1"], r4_["hi2"]], [r4_["top1"]])
                    tt_(r4_["mm2"][:], r4_["hi1"][:], r4_["hi2"][:], ALU.min, [r4_["hi1"], r4_["hi2"]], [r4_["mm2"]])
                    tt_(r4_["mm3"][:], r4_["lo1"][:], r4_["lo2"][:], ALU.max, [r4_["lo1"], r4_["lo2"]], [r4_["mm3"]])
                    tt_(r4_["mm2"][:], r4_["mm2"][:], r4_["mm3"][:], ALU.max, [r4_["mm2"], r4_["mm3"]], [r4_["mm2"]])
                    tt_(r4_["grp"][:], r4_["top1"][:], r4_["mm2"][:], ALU.add, [r4_["top1"], r4_["mm2"]], [r4_["grp"]])
                    yield
                    E("dve", lambda e: e.tensor_reduce(r1["gmax"][:], r4_["grp"][:], AX.X, ALU.max),
                      r=[r4_["grp"]], w=[r1["gmax"]])
                    E("dve", lambda e: e.tensor_scalar(r4_["gmask"][:], r4_["grp"][:], r1["gmax"][:, 0:1], None, ALU.is_ge),
                      r=[r4_["grp"], r1["gmax"]], w=[r4_["gmask"]])
                    E("dve", lambda e: e.tensor_scalar(r4_["gmask"][:], r4_["gmask"][:], 1e9, -1e9, ALU.mult, ALU.add),
                      r=[r4_["gmask"]], w=[r4_["gmask"]])
                    E("dve", lambda e: e.tensor_tensor(r16["selm"][:].rearrange("p (a b) -> p a b", a=4), s3,
                                                       r4_["gmask"][:].unsqueeze(2).to_broadcast([128, 4, 4]), ALU.add),
                      r=[r16["sel"], r4_["gmask"]], w=[r16["selm"]])
                    yield
                    E("dve", lambda e: e.tensor_reduce(r1["t1"][:], r16["selm"][:], AX.X, ALU.max),
                      r=[r16["selm"]], w=[r1["t1"]])
                    E("dve", lambda e: e.tensor_scalar(r16["m1"][:], r16["selm"][:], r1["t1"][:, 0:1], None, ALU.is_ge),
                      r=[r16["selm"], r1["t1"]], w=[r16["m1"]])
                    E("dve", lambda e: e.scalar_tensor_tensor(r16["selm2"][:], r16["m1"][:], -1e9, r16["selm"][:],
                                                              ALU.mult, ALU.add),
                      r=[r16["m1"], r16["selm"]], w=[r16["selm2"]])
                    yield
                    E("dve", lambda e: e.tensor_reduce(r1["t2"][:], r16["selm2"][:], AX.X, ALU.max),
                      r=[r16["selm2"]], w=[r1["t2"]])
                    E("dve", lambda e: e.tensor_scalar(r16["m2"][:], r16["selm2"][:], r1["t2"][:, 0:1], None, ALU.is_ge),
                      r=[r16["selm2"], r1["t2"]], w=[r16["m2"]])
                    tt_(r16["m1"][:], r16["m1"][:], r16["m2"][:], ALU.add, [r16["m1"], r16["m2"]], [r16["m1"]])
                    tt_(r16["wts"][:], r16["sc"][:], r16["m1"][:], ALU.mult, [r16["sc"], r16["m1"]], [r16["wts"]])
                    yield
                    E("dve", lambda e: e.tensor_reduce(r1["den"][:], r16["wts"][:], AX.X, ALU.add),
                      r=[r16["wts"]], w=[r1["den"]])
                    E("dve", lambda e: e.reciprocal(r1["den"][:], r1["den"][:]), r=[r1["den"]], w=[r1["den"]])
                    E("dve", lambda e, t=t: e.tensor_scalar(gates[:, t, :], r16["wts"][:], r1["den"][:, 0:1], None, ALU.mult),
                      r=[r16["wts"], r1["den"]], w=[gates])


                tchains = []
                nxt = 0
                ok = True
                while nxt < NT or tchains:
                    if ok and nxt < NT and len(tchains) < 2:
                        tchains.append(tile_chain(nxt))
                        nxt += 1
                        ok = False
                    for gtc in list(tchains):
                        try:
                            v = next(gtc)
                            if v == "norm_done":
                                ok = True
                        except StopIteration:
                            tchains.remove(gtc)
                            ok = True
                for e_ in range(NE):
                    i = e_ % 2
                    for k in range(4):
                        E("dve", lambda e, k=k, i=i: e.tensor_tensor(wdb[i][:, k, :], wdb[i][:, k, :], g2bc[:], ALU.mult),
                          r=[wdb[i], g2bc], w=[wdb[i]])
                    for gq in range(NTOK // 512):
                        at = actT[gq % 2]
                        ts = slice(gq * 512, (gq + 1) * 512)
                        for dt_ in range(4):
                            bg, bu = bank(), bank()
                            mm_group(bg[:], [(wgb[i][:, k, dt_ * 128:(dt_ + 1) * 128], h2T[:, k, ts]) for k in range(8)],
                                     r=[wgb[i], h2T], w=[bg])
                            mm_group(bu[:], [(wub[i][:, k, dt_ * 128:(dt_ + 1) * 128], h2T[:, k, ts]) for k in range(8)],
                                     r=[wub[i], h2T], w=[bu])
                            sl = sil[dt_ % 2]
                            E("act", lambda e, bg=bg, sl=sl: e.activation(sl[:], bg[:], AF.Silu), r=[bg], w=[sl])
                            E("dve", lambda e, bu=bu, sl=sl, at=at, dt_=dt_: e.tensor_tensor(at[:, dt_, :], bu[:], sl[:],
                                                                                           ALU.mult),
                              r=[sl, bu], w=[at])
                        for tt in range(4):
                            t = gq * 4 + tt
                            for oc in range(2):
                                b = bank()
                                mm_group(b[:], [(at[:, k, tt * 128:(tt + 1) * 128], wdb[i][:, k, oc * 512:(oc + 1) * 512])
                                                for k in range(4)], r=[at, wdb[i]], w=[b])
                                E("dve", lambda e, b=b, t=t, oc=oc, e_=e_: e.scalar_tensor_tensor(
                                    x[:, t, oc * 512:(oc + 1) * 512], b[:], gates[:, t, e_:e_ + 1],
                                    x[:, t, oc * 512:(oc + 1) * 512], ALU.mult, ALU.add),
                                  r=[b, gates, xt[t]], w=[xt[t]])
                    if e_ + 2 < NE:
                        load_expert(e_ + 2)
                sy.barrier()

        yv = y_d.rearrange("(t p) d -> p t d", p=128)
        if final:
            with ExitStack() as ms:
                fs = (mk("fjunk", [128, D], BF16, ms), mk("fssum", [128, 1], F32, ms), mk("ftmp1", [128, 1], F32, ms),
                      mk("ftmp2", [128, 1], F32, ms), mk("frstd", [128, 1], F32, ms))
                yb = [mk(f"yb{i}", [128, D], F32, ms) for i in range(2)]
                junk, ssum, tmp1, tmp2, rstd = fs
                for t in range(NT):
                    E("dve", lambda e: e.memset(ssum[:], 0.0), w=[ssum])
                    E("act", lambda e, t=t: e.activation(junk[:], x[:, t, :], AF.Square, accum_out=ssum[:, 0:1]),
                      r=[xt[t], ssum], w=[junk, ssum])
                    E("dve", lambda e: e.tensor_scalar(tmp1[:], ssum[:], 1.0 / D, EPS, ALU.mult, ALU.add), r=[ssum], w=[tmp1])
                    E("act", lambda e: e.sqrt(tmp2[:], tmp1[:]), r=[tmp1], w=[tmp2])
                    E("dve", lambda e: e.reciprocal(rstd[:], tmp2[:]), r=[tmp2], w=[rstd])
                    o = yb[t % 2]
                    E("dve", lambda e, t=t, o=o: e.scalar_tensor_tensor(o[:], x[:, t, :], rstd[:, 0:1], pg("wfin"),
                                                                        ALU.mult, ALU.mult),
                      r=[xt[t], rstd, prmg], w=[o])
                    DMA("sp", yv[:, t, :], o[:], r=[o], key=f"out{t % 2}")
                sy.barrier()
        else:
            for t in range(NT):
                DMA("sp", yv[:, t, :], x[:, t, :], r=[xt[t]], key="out")
            sy.barrier()
    return nc


def _t5_bucket(n):
    max_exact = 16
    large = max_exact + (np.log(np.maximum(n, 1) / max_exact) / math.log(128 / max_exact) * (32 - max_exact)).astype(np.int32)
    large = np.minimum(large, 31)
    return np.where(n < max_exact, n, large).astype(np.int32)


def _colmajor(v, ncol):
    return np.ascontiguousarray(np.asarray(v, np.float32).reshape(ncol, 128).T)


def _bc(v):
    v = np.asarray(v, np.float32).reshape(1, -1)
    return np.ascontiguousarray(np.broadcast_to(v, (128, v.shape[1])))


_WIN_PERM = None


def _win_perm():
    q_m = list(range(0, 256)); k_m = list(range(256, 512)); v_m = list(range(512, 1024)); o_m = list(range(1024, 1536))
    i_g = list(range(1536, 1540)); f_g = list(range(1540, 1544)); q_a = list(range(1544, 2056))
    k_a = list(range(2056, 2184)); v_a = list(range(2184, 2312)); g_m = list(range(2312, 3336)); g_a = list(range(3336, 4360))
    cols = []
    cols += q_m + k_m
    cols += q_a
    cols += k_a[0:64] + k_a[0:64] + k_a[64:128] + k_a[64:128] + i_g + f_g + v_a
    cols += v_m
    cols += o_m
    for pi in range(4):
        for oi in range(2):
            ot = 2 * pi + oi
            cols += g_m[ot * 128:(ot + 1) * 128] + g_a[ot * 128:(ot + 1) * 128]
    assert len(cols) == NWIN
    return np.asarray(cols, np.int64)


def _consts(rel_bias):
    cst = np.zeros((128, 384), np.float32)
    cst[:, 0:128] = np.eye(128, dtype=np.float32)
    s = np.arange(128)[:, None]
    l_ = np.arange(128)[None, :]
    cst[:, 128:256] = (s <= l_).astype(np.float32)
    cst[:, 256:384] = 1.0
    q = np.arange(128)[None, :]
    sk = np.arange(128)[:, None]
    bT = np.zeros((128, 2, 2, 2, 2, 128), np.float32)
    for blk in range(2):
        dist = q + 128 - (sk + blk * 128)
        ok = (dist >= 0) & (dist < 128)
        bucket = _t5_bucket(np.maximum(dist, 0))
        vals = np.asarray(rel_bias, np.float32)[bucket]
        vals = np.where(ok[:, :, None], vals, np.float32(-30000.0))
        for h in range(8):
            kv, g4 = h // 4, h % 4
            bT[:, kv, g4 % 2, blk, g4 // 2, :] = vals[:, :, h]
    return cst, np.ascontiguousarray(bT.reshape(128, 2048))


_PROGS = {}


def _prog(nl, final):
    key = (nl, final)
    if key not in _PROGS:
        _PROGS[key] = build(nl, final)
    return _PROGS[key]


def _prep_common(inp):
    perm = _win_perm()
    cst, biasT = _consts(inp["rel_bias"])
    return perm, cst, biasT


def _layer_arrays(inp, ls, perm):
    f = lambda a: np.ascontiguousarray(np.asarray(a, np.float32))
    d = {}
    d["w_ada"] = f(inp["w_ada"][ls])
    d["w_in"] = f(np.asarray(inp["w_in"])[ls][:, :, perm])
    d["w_br"] = f(np.stack([np.asarray(inp["w_br_m"])[ls], np.asarray(inp["w_br_a"])[ls]], axis=1))
    d["w_out"] = f(inp["w_out"][ls])
    d["wg"] = f(inp["w_gate_e"][ls])
    d["wu"] = f(inp["w_up_e"][ls])
    d["wd"] = f(inp["w_down_e"][ls])
    prml = []
    for l in ls:
        ba = np.asarray(inp["b_ada"][l], np.float32)
        parts = [
            _colmajor(inp["w_norm1"][l], 8), _colmajor(inp["w_norm2"][l], 8), _colmajor(ba, 48),
            np.ascontiguousarray(np.asarray(inp["conv_w"][l], np.float32).reshape(4, 4, 128).transpose(2, 1, 0).reshape(128, 16)),
            _colmajor(inp["conv_b"][l], 4),
            _bc(np.concatenate([np.asarray(inp["b_igate"][l]), np.asarray(inp["b_fgate"][l])])),
            _bc(inp["w_mnorm"][l]), _bc(inp["sinks"][l]),
        ]
        p = np.concatenate(parts, axis=1)
        assert p.shape == (128, NPL), p.shape
        prml.append(p)
    d["prml"] = np.ascontiguousarray(np.stack(prml, 0))
    d["prmb"] = np.ascontiguousarray(np.stack([np.concatenate(
        [_bc(np.asarray(inp["b_ada"][l], np.float32)[2048:3072]), _bc(np.asarray(inp["b_ada"][l], np.float32)[5120:6144])],
        axis=1) for l in ls], 0))
    return d


def _prmg(inp, b, half):
    wr = np.asarray(inp["w_router"], np.float32).reshape(8, 128, NE).transpose(1, 0, 2).reshape(128, 8 * NE)
    parts = [_colmajor(np.asarray(inp["c"])[b], 8), np.full((128, 1), float(half), np.float32),
             _bc(inp["router_bias"]), np.ascontiguousarray(wr), _bc(inp["w_final"])]
    p = np.concatenate(parts, axis=1)
    assert p.shape == (128, NPG)
    return np.ascontiguousarray(p)


def kernel(**inp):
    inp = {k: np.asarray(v) for k, v in inp.items()}
    perm, cst, biasT = _prep_common(inp)
    la = _layer_arrays(inp, [0, 1], perm)
    nc = _prog(2, True)
    in_maps = []
    for c in range(8):
        m = {"x": np.ascontiguousarray(inp["x"][c // 2, (c % 2) * NTOK:(c % 2 + 1) * NTOK, :], dtype=np.float32),
             "prmg": _prmg(inp, c // 2, c % 2), "cst": cst, "biasT": biasT}
        m.update(la)
        in_maps.append(m)
    res = run_bass_kernel_spmd(nc, in_maps, core_ids=list(range(8)))
    out = np.zeros((4, 4096, D), np.float32)
    for c in range(8):
        out[c // 2, (c % 2) * NTOK:(c % 2 + 1) * NTOK, :] = np.asarray(res.results[c]["y"], dtype=np.float32)
    return out
```

```python
import math
import os
from contextlib import ExitStack

import numpy as np
import concourse.bass as bass
import concourse.mybir as mybir
from concourse.bass_utils import run_bass_kernel_spmd

F32 = mybir.dt.float32
BF16 = mybir.dt.bfloat16
AF = mybir.ActivationFunctionType
ALU = mybir.AluOpType
AX = mybir.AxisListType

D = 1024
NTOK = 2048
NT = 16
G = 256
TPG = G // 128
NG = NTOK // G
EPS = 1e-6
NE = 16
DE = 512
NWIN = 4488
LN8 = math.log(0.125)
NCORES = 8

PL = {}
_o = 0
for _n, _w in [("wn1", 8), ("wn2", 8), ("bada", 48), ("convw", 16),
               ("convb", 4), ("bif", 8), ("wmn", 512), ("sinks", 8)]:
    PL[_n] = (_o, _o + _w)
    _o += _w
NPL = _o
PG = {}
_o = 0
for _n, _w in [("cT", 8), ("flag", 1), ("rb", 16), ("wr", 128), ("wfin", 1024)]:
    PG[_n] = (_o, _o + _w)
    _o += _w
NPG = _o


class Res:
    __slots__ = ("name", "w", "r")

    def __init__(self, name):
        self.name = name
        self.w = None
        self.r = {}


class Sync:
    def __init__(self, nc, es):
        self.nc = nc
        self.es = es
        self.engs = {"pe": nc.tensor, "act": nc.scalar, "dve": nc.vector, "pool": nc.gpsimd, "sp": nc.sync}
        self.sems = {}
        self.cnt = {}
        for k in self.engs:
            self.sems[k] = es.enter_context(nc.semaphore("s_" + k))
            self.cnt[k] = 0
        self.seen = {k: {} for k in self.engs}

    def dma_sem(self, key):
        if key not in self.sems:
            self.sems[key] = self.es.enter_context(self.nc.semaphore("d_" + key))
            self.cnt[key] = 0
        return key

    def need(self, eng, ev, same_ok=False):
        if ev is None:
            return
        k, v = ev
        if k == eng and not same_ok:
            return
        if self.seen[eng].get(k, 0) >= v:
            return
        self.engs[eng].wait_ge(self.sems[k], v)
        self.seen[eng][k] = v

    def deps(self, eng, reads, writes, force_same=False):
        for r in reads:
            self.need(eng, r.w, same_ok=True)
        for w in writes:
            self.need(eng, w.w, same_ok=force_same)
            for k, v in w.r.items():
                self.need(eng, (k, v), same_ok=force_same)

    def emit(self, eng, fn, reads=(), writes=(), inc=True):
        self.deps(eng, reads, writes)
        inst = fn(self.engs[eng])
        if inc:
            inst.then_inc(self.sems[eng], 1)
            self.cnt[eng] += 1
            ev = (eng, self.cnt[eng])
        else:
            ev = (eng, self.cnt[eng] + 1)
        for r in reads:
            r.r[eng] = max(r.r.get(eng, 0), ev[1])
        for w in writes:
            w.w = ev
            w.r = {}
        return inst

    def dma(self, queue, out, in_, reads=(), writes=(), semkey=None):
        self.deps(queue, reads, writes, force_same=True)
        semkey = self.dma_sem(semkey)
        inst = self.engs[queue].dma_start(out=out, in_=in_)
        inst.then_inc(self.sems[semkey], 16)
        self.cnt[semkey] += 16
        ev = (semkey, self.cnt[semkey])
        for r in reads:
            r.r[semkey] = ev[1]
        for w in writes:
            w.w = ev
            w.r = {}
        return inst

    def emit_cc(self, fn, reads, writes, key):
        self.deps("pool", reads, writes, force_same=True)
        key = self.dma_sem(key)
        inst = fn(self.engs["pool"])
        inst.then_inc(self.sems[key])
        self.cnt[key] = 1
        ev = (key, 1)
        for r in reads:
            r.r[key] = 1
        for w in writes:
            w.w = ev
            w.r = {}
        return inst

    def barrier(self, skip=()):
        for e in self.engs:
            for k in self.sems:
                if k != e and self.cnt[k] > 0 and k not in skip:
                    self.need(e, (k, self.cnt[k]))


class T:
    def __init__(self, es, nc, name, shape, dtype, psum=False):
        if psum:
            self.t = es.enter_context(nc.psum_tensor("p_" + name, shape, dtype))
        else:
            self.t = es.enter_context(nc.sbuf_tensor("t_" + name, shape, dtype))
        self.r = Res(name)
        self.name = name
        self.psum = psum

    def __getitem__(self, k):
        return self.t[k]


def build(nl, final, moe=True, dbg=99):
    nc = bass.Bass("TRN2", target_bir_lowering=False)

    def din(name, shape, dt=F32):
        return nc.dram_tensor(name, shape, dt, kind="ExternalInput").ap()

    def dout(name, shape, dt=F32):
        return nc.dram_tensor(name, shape, dt, kind="ExternalOutput").ap()

    x_d = din("x", [NTOK, D])
    prmg_d = din("prmg", [128, NPG])
    prml_d = din("prml", [nl, 128, NPL])
    prmb_d = din("prmb", [nl, 128, 2048])
    cst_d = din("cst", [128, 384])
    biasT_d = din("biasT", [128, 2048])
    wada_d = din("w_ada", [nl, D, 6 * D])
    win_d = din("w_in", [nl, D, NWIN])
    wbr_d = din("w_br", [nl, 2, 512, D])
    wout_d = din("w_out", [nl, D, D])
    wg_d = din("wg", [nl, NE, D, DE])
    wu_d = din("wu", [nl, NE, D, DE])
    wd_d = din("wd", [nl, NE, DE, D])
    y_d = dout("y", [NTOK, D])

    with ExitStack() as es:
        sy = Sync(nc, es)

        def E(eng, fn, r=(), w=(), inc=True):
            w = list(w) + [t for t in r if getattr(t, "psum", False) and t not in w]
            return sy.emit(eng, fn, reads=[t.r for t in r], writes=[t.r for t in w], inc=inc)

        def DMA(q, out, in_, r=(), w=(), key=None):
            return sy.dma(q, out, in_, reads=[t.r for t in r], writes=[t.r for t in w], semkey=key)

        def mk(name, shape, dt, stack=es):
            return T(stack, nc, name, shape, dt)

        x = mk("x", [128, NT, D], F32)
        xr = [Res(f"x{t}") for t in range(NT)]

        class XT:
            def __init__(self, t):
                self.r = xr[t]
        xt = [XT(t) for t in range(NT)]
        prmg = mk("prmg", [128, NPG], F32)
        prml = mk("prml", [128, NPL], F32)
        cst = mk("cst", [128, 384], F32)
        ident_b = mk("ident_b", [128, 128], BF16)
        tri_b = mk("tri_b", [128, 128], BF16)
        modc = mk("modc", [128, 48], F32)
        a1 = mk("a1", [128, 8], F32)
        a2 = mk("a2", [128, 8], F32)
        g1bc = mk("g1bc", [128, D], F32)
        g2bc = mk("g2bc", [128, D], F32)
        cond_bf = mk("cond_bf", [128, 8], BF16)
        cond_f = mk("cond_f", [128, 8], F32)
        banks = [T(es, nc, f"bank{i}", [128, 512], F32, psum=True) for i in range(8)]
        bstate = {"i": 0}

        def bank():
            b = banks[bstate["i"] % 8]
            bstate["i"] += 1
            return b

        ident_f = cst[:, 0:128]
        tri_f = cst[:, 128:256]
        ones_f = cst[:, 256:384]

        def pl(name, a=None, b=None):
            lo, hi = PL[name]
            if a is None:
                return prml[:, lo:hi]
            return prml[:, lo + a:lo + b]

        def pg(name, a=None, b=None):
            lo, hi = PG[name]
            if a is None:
                return prmg[:, lo:hi]
            return prmg[:, lo + a:lo + b]

        xv = x_d.rearrange("(t p) d -> p t d", p=128)
        for t in range(NT):
            DMA("sp", x[:, t, :], xv[:, t, :], w=[xt[t]], key=f"x{t}")
        DMA("sp", prmg[:], prmg_d, w=[prmg], key="prmg")
        DMA("sp", cst[:], cst_d, w=[cst], key="cst")
        E("dve", lambda e: e.tensor_copy(ident_b[:], ident_f), r=[cst], w=[ident_b])
        E("dve", lambda e: e.tensor_copy(tri_b[:], tri_f), r=[cst], w=[tri_b])
        ones_b = mk("ones_b", [128, 128], BF16)
        E("dve", lambda e: e.tensor_copy(ones_b[:], ones_f), r=[cst], w=[ones_b])
        E("act", lambda e: e.activation(cond_f[:], pg("cT"), AF.Silu), r=[prmg], w=[cond_f])
        E("dve", lambda e: e.tensor_copy(cond_bf[:], cond_f[:]), r=[cond_f], w=[cond_bf])

        def mm_group(out_ap, pairs, r, w):
            n = len(pairs)
            for i, (lt, rh) in enumerate(pairs):
                E("pe", lambda e, lt=lt, rh=rh, i=i: e.matmul(out_ap, lt, rh, start=(i == 0), stop=(i == n - 1)),
                  r=r, w=w, inc=(i == n - 1))

        sm = {}

        def small(name, shape, dt=F32, stack=es):
            if name not in sm:
                sm[name] = mk(name, shape, dt, stack)
            return sm[name]

        def norm_gen(t, acol, shcol, dst, dcol, sc, router_dst=None, alloc=None, rel=None):
            junk, ssum, tmp1, tmp2, rstd, xn = sc
            junk = xn
            E("dve", lambda e: e.memset(ssum[:], 0.0), w=[ssum])
            E("act", lambda e: e.activation(junk[:], x[:, t, :], AF.Square, accum_out=ssum[:, 0:1]),
              r=[xt[t], ssum], w=[junk, ssum])
            yield
            E("dve", lambda e: e.tensor_scalar(tmp1[:], ssum[:], 1.0 / D, EPS, ALU.mult, ALU.add), r=[ssum], w=[tmp1])
            E("act", lambda e: e.sqrt(tmp2[:], tmp1[:]), r=[tmp1], w=[tmp2])
            yield
            E("dve", lambda e: e.reciprocal(rstd[:], tmp2[:]), r=[tmp2], w=[rstd])
            E("act", lambda e: e.mul(xn[:], x[:, t, :], rstd[:, 0:1]), r=[xt[t], rstd], w=[xn])
            yield
            for half in range(2):
                if alloc is None:
                    b = bank()
                else:
                    b = yield from alloc()
                for j in range(4):
                    kk = half * 4 + j
                    E("pe", lambda e, j=j, kk=kk, b=b: e.transpose(b[:, j * 128:(j + 1) * 128],
                                                                    xn[:, kk * 128:(kk + 1) * 128], ident_f),
                      r=[xn, cst], w=[b], inc=(j == 3))
                yield
                for j in range(4):
                    kk = half * 4 + j
                    if half == 0:
                        E("dve", lambda e, j=j, kk=kk, b=b: e.tensor_scalar(
                            dst[:, kk, dcol:dcol + 128], b[:, j * 128:(j + 1) * 128],
                            acol[:, kk:kk + 1], shcol[:, kk:kk + 1], ALU.mult, ALU.add), r=[b, modc, a1, a2], w=[dst])
                    else:
                        E("act", lambda e, j=j, kk=kk, b=b: e.activation(
                            dst[:, kk, dcol:dcol + 128], b[:, j * 128:(j + 1) * 128], AF.Identity,
                            bias=shcol[:, kk:kk + 1], scale=acol[:, kk:kk + 1]), r=[b, modc, a1, a2], w=[dst])
                if router_dst is not None:
                    E("act", lambda e, half=half, b=b: e.copy(router_dst[:, half * 512:(half + 1) * 512], b[:]),
                      r=[b], w=[router_dst])
                if rel is not None:
                    rel(b)
                yield

        def norm_tile(t, acol, shcol, dst, dcol, sc, router_dst=None):
            for _ in norm_gen(t, acol, shcol, dst, dcol, sc, router_dst):
                pass

        for l in range(nl):
            if dbg <= 1:
                break
            sy.barrier()
            DMA("sp", prml[:], prml_d[l], w=[prml], key="prml")
            with ExitStack() as ms:
                biasT = mk(f"biasT_{l}", [128, 2048], BF16, ms)
                DMA("pool", biasT[:], biasT_d, w=[biasT], key="biasT")
                ring = [mk(f"ring{i}_{l}", [128, 8, 512], BF16, ms) for i in range(3)]
                wbr = mk(f"wbr_{l}", [128, 2, 4, D], BF16, ms)
                wout = mk(f"wout_{l}", [128, 8, D], BF16, ms)
                hT2 = [mk(f"hT{i}_{l}", [128, 8, G], BF16, ms) for i in range(2)]
                qkpre = [mk(f"qkpre{i}_{l}", [128, 3 + G], F32, ms) for i in range(4)]
                qkT = mk(f"qkT_{l}", [128, 4, G], BF16, ms)
                qaT = None
                kaT = mk(f"kaT_{l}", [128, 2, 128 + G], BF16, ms)
                vaug = [mk(f"vaug{i}_{l}", [128, 2, 72], BF16, ms) for i in range(3)]
                vhalo = mk(f"vhalo_{l}", [128, 2, 72], BF16, ms)
                state_f = mk(f"state_f_{l}", [128, 2, 129], F32, ms)
                state_b = mk(f"state_b_{l}", [128, 2, 129], BF16, ms)
                hmT = None
                haT = None
                mT = None
                esink = mk(f"esink_{l}", [128, 8], F32, ms)
                nsc = (None, mk(f"ssum_{l}", [128, 1], F32, ms),
                       mk(f"tmp1_{l}", [128, 1], F32, ms), mk(f"tmp2_{l}", [128, 1], F32, ms),
                       mk(f"rstd_{l}", [128, 1], F32, ms), mk(f"xn_{l}", [128, D], F32, ms))
                gsb = [mk(f"gsb{i}_{l}", [128, 8], F32, ms) for i in range(TPG)]
                ef4 = [mk(f"ef4{i}_{l}", [128, 4], F32, ms) for i in range(TPG)]
                logf = [mk(f"logf{i}_{l}", [128, 4], F32, ms) for i in range(TPG)]
                lhi = [mk(f"lhi{i}_{l}", [128, 4], BF16, ms) for i in range(TPG)]
                llo = [mk(f"llo{i}_{l}", [128, 4], BF16, ms) for i in range(TPG)]
                w4 = [mk(f"w4{i}_{l}", [128, 4], F32, ms) for i in range(TPG)]
                sc4 = [mk(f"sc4{i}_{l}", [128, 4], F32, ms) for i in range(TPG)]
                eb4 = [mk(f"eb4{i}_{l}", [128, 4], F32, ms) for i in range(TPG)]
                vpr = [mk(f"vpr{i}_{l}", [128, 4, 136], BF16, ms) for i in range(TPG)]
                som = None
                ktok = [mk(f"ktok{i}_{l}", [128, 2, 2, 128], BF16, ms) for i in range(TPG)]
                ebsel = [mk(f"ebsel{i}_{l}", [128, 2], F32, ms) for i in range(TPG)]
                sd = None
                den4 = None
                r4 = None
                hm = None
                sqj = None
                ss4 = None
                rn4 = None
                hmo = None
                tmpS = None
                pT = None
                dn4 = None
                hao = None
                sg1 = mk(f"sg1_{l}", [128, G], F32, ms)
                acc = sg1
                for i in range(TPG):
                    E("dve", lambda e, i=i: e.memset(ktok[i][:], 0.0), w=[ktok[i]])
                sg2 = None

                for i in range(2):
                    DMA("pool", wbr[:, i, :, :], wbr_d[l, i].rearrange("(k p) n -> p k n", p=128), w=[wbr], key="wbr")
                for k in range(8):
                    DMA("pool", wout[:, k, :], wout_d[l, k * 128:(k + 1) * 128, :], w=[wout], key="wout")
                flag = pg("flag")
                E("act", lambda e: e.activation(esink[:], pl("sinks"), AF.Exp), r=[prml], w=[esink])
                for i in range(3):
                    E("dve", lambda e, i=i: e.memset(vaug[i][:], 1.0), w=[vaug[i]])
                ib_d = nc.dram_tensor(f"ib{l}", [128, 656], F32)
                ob_d = nc.dram_tensor(f"ob{l}", [256, 656], F32)
                ibv = ib_d.ap()
                obv = ob_d.ap()

                class RO:
                    def __init__(self, n):
                        self.r = Res(n)
                ib_parts = [RO(f"ibp{i}_{l}") for i in range(7)]
                obR = RO(f"ob_{l}")
                pieces = [(0, 512), (512, 512), (1024, 392), (1416, 512), (1928, 512),
                          (2440, 512), (2952, 512), (3464, 512), (3976, 512)]

                class _Stop(Exception):
                    pass

                def chk(k):
                    if dbg == k:
                        raise _Stop()
                CUR = {"free": list(range(8))}
                ada_es = ExitStack()
                aring = [mk(f"aring{i}_{l}", [128, 8, 512], BF16, ada_es) for i in range(3)]
                prmb = mk(f"prmb_{l}", [128, 2048], F32, ada_es)
                DMA("sp", prmb[:], prmb_d[l], w=[prmb], key="prmb")
                cond_bc = mk(f"cond_bc_{l}", [128, 8, 128], BF16, ada_es)
                E("dve", lambda e: e.tensor_copy(cond_bc[:], cond_f[:].unsqueeze(2).to_broadcast([128, 8, 128])),
                  r=[cond_f], w=[cond_bc])

                def ada_issue(j, l=l):
                    rb_ = aring[j % 3]
                    DMA("pool", rb_[:], wada_d[l, :, j * 512:(j + 1) * 512].rearrange("(k p) n -> p k n", p=128),
                        w=[rb_], key=f"aring{j % 3}")

                def ada_steps(js, l=l):
                    ada_issue(js[0])
                    for ji, j in enumerate(js):
                        if ji + 1 < len(js):
                            ada_issue(js[ji + 1])
                        yield
                        while not CUR["free"]:
                            yield
                        bi_ = CUR["free"].pop(0)
                        b = banks[bi_]
                        rb_ = aring[j % 3]
                        if j in (4, 5, 10, 11):
                            gb = g1bc if j < 6 else g2bc
                            boff_ = 0 if j < 6 else 1024
                            c0 = (j % 2) * 512 if j < 6 else (j - 10) * 512
                            mm_group(b[:], [(cond_bc[:, k, :], rb_[:, k, :]) for k in range(8)], r=[cond_bc, rb_], w=[b])
                            E("dve", lambda e, b=b, gb=gb, boff_=boff_, c0=c0: e.tensor_tensor(
                                gb[:, c0:c0 + 512], b[:], prmb[:, boff_ + c0:boff_ + c0 + 512], ALU.add), r=[b, prmb], w=[gb])
                        else:
                            for jt in range(4):
                                mm_group(b[:, jt:jt + 1],
                                         [(rb_[:, k, jt * 128:(jt + 1) * 128], cond_bf[:, k:k + 1]) for k in range(8)],
                                         r=[cond_bf, rb_], w=[b])
                            E("dve", lambda e, b=b, j=j: e.tensor_tensor(modc[:, 4 * j:4 * j + 4], b[:, 0:4],
                                                                         pl("bada", 4 * j, 4 * j + 4), ALU.add),
                              r=[b, prml], w=[modc])
                        CUR["free"].append(bi_)
                        yield

                for _ in ada_steps([0, 1, 2, 3]):
                    pass
                E("dve", lambda e: e.scalar_tensor_tensor(a1[:], modc[:, 8:16], 1.0, pl("wn1"), ALU.add, ALU.mult),
                  r=[modc, prml], w=[a1])

                def ada_rest():
                    yield from ada_steps([4, 5, 6, 7, 8, 9, 10, 11])
                    E("dve", lambda e: e.scalar_tensor_tensor(a2[:], modc[:, 32:40], 1.0, pl("wn2"), ALU.add, ALU.mult),
                      r=[modc, prml], w=[a2])
                background = [ada_rest()]
                sh1 = modc[:, 0:8]
                sh2 = modc[:, 24:32]


                p2s = ExitStack()
                for pss in (1, 2):
                    if pss == 1:
                        E("dve", lambda e: e.memset(state_f[:], 0.0), w=[state_f])
                        E("dve", lambda e: e.memset(state_b[:], 0.0), w=[state_b])
                        for i in range(4):
                            E("dve", lambda e, i=i: e.memset(qkpre[i][:, 0:3], 0.0), w=[qkpre[i]])
                        E("dve", lambda e: e.memset(kaT[:, :, 0:128], 0.0), w=[kaT])
                    else:
                        for gb_ in background:
                            for _ in gb_:
                                pass
                        background = []
                        sy.barrier(skip=(f"cc{l}",))
                        ada_es.close()
                        qaT = mk(f"qaT_{l}", [128, 4, G], BF16, p2s)
                        hmT = mk(f"hmT_{l}", [128, 4, G], BF16, p2s)
                        haT = mk(f"haT_{l}", [128, 4, G], BF16, p2s)
                        mT = mk(f"mT_{l}", [128, 8, G], BF16, p2s)
                        som = [mk(f"som{i}_{l}", [128, 512], BF16, p2s) for i in range(TPG)]
                        sd = [mk(f"sd{i}_{l}", [128, 4, 128], BF16, p2s) for i in range(TPG)]
                        den4 = [mk(f"den4{i}_{l}", [128, 4], F32, p2s) for i in range(TPG)]
                        r4 = [mk(f"r4{i}_{l}", [128, 4], F32, p2s) for i in range(TPG)]
                        hm = [mk(f"hm{i}_{l}", [128, 4, 128], F32, p2s) for i in range(TPG)]
                        sqj = [mk(f"sqj{i}_{l}", [128, 128], F32, p2s) for i in range(TPG)]
                        ss4 = [mk(f"ss4{i}_{l}", [128, 4], F32, p2s) for i in range(TPG)]
                        rn4 = [mk(f"rn4{i}_{l}", [128, 4], F32, p2s) for i in range(TPG)]
                        hmo = [mk(f"hmo{i}_{l}", [128, 512], BF16, p2s) for i in range(TPG)]
                        tmpS = [mk(f"tmpS{i}_{l}", [128, 2, 512], F32, p2s) for i in range(TPG)]
                        pT = [mk(f"pT{i}_{l}", [128, 2, 512], BF16, p2s) for i in range(TPG)]
                        dn4 = [mk(f"dn4{i}_{l}", [128, 4], F32, p2s) for i in range(TPG)]
                        hao = [mk(f"hao{i}_{l}", [128, 512], BF16, p2s) for i in range(TPG)]
                        sg2 = mk(f"sg2_{l}", [128, G], F32, p2s)
                        for k in range(8):
                            E("dve", lambda e, k=k: e.tensor_tensor(wout[:, k, :], wout[:, k, :], g1bc[:], ALU.mult),
                              r=[wout, g1bc], w=[wout])
                        DMA("sp", state_f[:], obv[0:128, 0:258].rearrange("p (a b) -> p a b", a=2), r=[obR], w=[state_f], key="h_st")
                        E("dve", lambda e: e.tensor_scalar(state_f[:], state_f[:], flag[:, 0:1], None, ALU.mult),
                          r=[state_f, prmg], w=[state_f])
                        E("act", lambda e: e.copy(state_b[:], state_f[:]), r=[state_f], w=[state_b])
                        for i in range(4):
                            DMA("sp", qkpre[i][:, 0:3], obv[0:128, 258 + 3 * i:261 + 3 * i], r=[obR], w=[qkpre[i]], key=f"h_cv{i}")
                            E("dve", lambda e, i=i: e.tensor_scalar(qkpre[i][:, 0:3], qkpre[i][:, 0:3], flag[:, 0:1], None,
                                                                      ALU.mult), r=[qkpre[i], prmg], w=[qkpre[i]])
                        DMA("pool", kaT[:, :, 0:128], obv[0:128, 270:526].rearrange("p (a b) -> p a b", a=2), r=[obR], w=[kaT], key="h_ka")
                        DMA("pool", vhalo[:, :, 0:65], obv[0:128, 526:656].rearrange("p (a b) -> p a b", a=2), r=[obR], w=[vhalo], key="h_va")
                        E("dve", lambda e: e.tensor_scalar(vhalo[:, :, 0:65], vhalo[:, :, 0:65], flag[:, 0:1], None, ALU.mult),
                          r=[vhalo, prmg], w=[vhalo])
                    pcs = (0, 2, 3) if pss == 1 else tuple(range(9))
                    seq = [(g, p) for g in range(NG) for p in pcs]
                    rstate = {"issued": 0}

                    def ring_issue(seq=seq, rstate=rstate):
                        i = rstate["issued"]
                        if i >= len(seq):
                            return
                        _, p = seq[i]
                        c0, wd_ = pieces[p]
                        rb_ = ring[i % 3]
                        DMA("pool", rb_[:, :, 0:wd_], win_d[l, :, c0:c0 + wd_].rearrange("(k p) n -> p k n", p=128),
                            w=[rb_], key=f"ring{i % 3}")
                        rstate["issued"] += 1

                    ring_issue()
                    ring_issue()
                    uidx = {"i": 0}

                    def ring_get(uidx=uidx):
                        i = uidx["i"]
                        uidx["i"] += 1
                        return ring[i % 3]

                    try:
                        for g in range(NG):
                            if dbg <= 3 or (dbg <= 30 and g >= 1):
                                break
                            hT = hT2[g % 2]
                            if g == 0:
                                for tt in range(TPG):
                                    norm_tile(g * TPG + tt, a1, sh1, hT, tt * 128, nsc)
                            chk(11)
                            rb_ = ring_get()
                            for mt in range(4):
                                if pss == 1 and mt < 2 and g < NG - 1:
                                    continue
                                b = bank()
                                mm_group(b[:, 0:G], [(rb_[:, k, mt * 128:(mt + 1) * 128], hT[:, k, :]) for k in range(8)],
                                         r=[rb_, hT], w=[b])
                                E("act", lambda e, b=b, mt=mt: e.copy(qkpre[mt][:, 3:3 + G], b[:, 0:G]), r=[b], w=[qkpre[mt]])
                                if pss == 2 or mt >= 2:
                                    cw = lambda j, mt=mt: pl("convw", mt * 4 + j, mt * 4 + j + 1)
                                    E("dve", lambda e, mt=mt, cw=cw: e.tensor_scalar(acc[:], qkpre[mt][:, 0:G], cw(0),
                                                                                       pl("convb", mt, mt + 1), ALU.mult, ALU.add),
                                      r=[qkpre[mt], prml], w=[acc])
                                    for j in range(1, 4):
                                        E("dve", lambda e, mt=mt, j=j, cw=cw: e.scalar_tensor_tensor(
                                            acc[:], qkpre[mt][:, j:j + G], cw(j), acc[:], ALU.mult, ALU.add),
                                          r=[qkpre[mt], prml, acc], w=[acc])
                                    E("act", lambda e, mt=mt: e.activation(qkT[:, mt, :], acc[:], AF.Silu), r=[acc], w=[qkT])
                                E("act", lambda e, mt=mt: e.copy(qkpre[mt][:, 0:3], qkpre[mt][:, G:G + 3]),
                                  r=[qkpre[mt]], w=[qkpre[mt]])
                            ring_issue()
                            chk(12)
                            if pss == 2:
                                rb_ = ring_get()
                                for mt in range(4):
                                    b = bank()
                                    mm_group(b[:, 0:G], [(rb_[:, k, mt * 128:(mt + 1) * 128], hT[:, k, :]) for k in range(8)],
                                             r=[rb_, hT], w=[b])
                                    E("act", lambda e, b=b, mt=mt: e.copy(qaT[:, mt, :], b[:, 0:G]), r=[b], w=[qaT])
                                ring_issue()
                            chk(13)
                            rb_ = ring_get()
                            for j in range(2):
                                b = bank()
                                mm_group(b[:, 0:G], [(rb_[:, k, j * 128:(j + 1) * 128], hT[:, k, :]) for k in range(8)],
                                         r=[rb_, hT], w=[b])
                                E("act", lambda e, b=b, j=j: e.copy(kaT[:, j, 128:128 + G], b[:, 0:G]), r=[b], w=[kaT])
                            chk(131)
                            def gate_gen(tt, g=g):
                                t = g * TPG + tt
                                va = vaug[t % 3]
                                b = bank()
                                yield
                                mm_group(b[:, 0:136], [(hT[:, k, tt * 128:(tt + 1) * 128], rb_[:, k, 256:392]) for k in range(8)],
                                         r=[rb_, hT], w=[b])
                                yield
                                E("dve", lambda e, b=b, tt=tt: e.tensor_tensor(gsb[tt][:], b[:, 0:8], pl("bif"), ALU.add),
                                  r=[b, prml], w=[gsb[tt]])
                                yield
                                E("act", lambda e, b=b, va=va: e.copy(va[:, :, 0:64],
                                                                        b[:, 8:136].rearrange("p (a b) -> p a b", a=2)),
                                  r=[b], w=[va])
                                yield
                                E("act", lambda e, tt=tt: e.activation(ef4[tt][:], gsb[tt][:, 4:8], AF.Exp, scale=-1.0),
                                  r=[gsb[tt]], w=[ef4[tt]])
                                yield
                                E("act", lambda e, tt=tt: e.activation(ef4[tt][:], ef4[tt][:], AF.Ln, bias=1.0),
                                  r=[ef4[tt]], w=[ef4[tt]])
                                yield
                                E("dve", lambda e, tt=tt: e.tensor_scalar(logf[tt][:], ef4[tt][:], -1.0, None, ALU.mult),
                                  r=[ef4[tt]], w=[logf[tt]])
                                yield
                                b2 = bank()
                                yield
                                E("dve", lambda e, tt=tt: e.tensor_copy(lhi[tt][:], logf[tt][:]), r=[logf[tt]], w=[lhi[tt]])
                                yield
                                E("dve", lambda e, tt=tt: e.tensor_tensor(llo[tt][:], logf[tt][:], lhi[tt][:], ALU.subtract),
                                  r=[logf[tt], lhi[tt]], w=[llo[tt]])
                                yield
                                mm_group(b2[:, 0:4], [(tri_b[:], lhi[tt][:]), (tri_b[:], llo[tt][:])],
                                         r=[tri_b, lhi[tt], llo[tt]], w=[b2])
                                yield
                                mm_group(b2[:, 4:8], [(ones_b[:], lhi[tt][:]), (ones_b[:], llo[tt][:])],
                                         r=[ones_b, lhi[tt], llo[tt]], w=[b2])
                                yield
                                E("dve", lambda e, b2=b2, tt=tt: e.tensor_tensor(w4[tt][:], b2[:, 0:4], gsb[tt][:, 0:4],
                                                                                   ALU.subtract), r=[gsb[tt], b2], w=[w4[tt]])
                                yield
                                E("act", lambda e, tt=tt: e.activation(w4[tt][:], w4[tt][:], AF.Exp, scale=-1.0), r=[w4[tt]], w=[w4[tt]])
                                yield
                                E("act", lambda e, b2=b2, tt=tt: e.activation(sc4[tt][:], b2[:, 0:4], AF.Exp),
                                  r=[b2], w=[sc4[tt]])
                                yield
                                E("act", lambda e, b2=b2, tt=tt: e.activation(eb4[tt][:], b2[:, 4:8], AF.Exp),
                                  r=[b2], w=[eb4[tt]])
                                eb2 = eb4[tt][:, 0:4].rearrange("p (j r) -> p j r", r=2)
                                yield
                                E("act", lambda e, tt=tt, eb2=eb2: e.copy(ebsel[tt][0:64, :], eb2[0:64, :, 0]),
                                  r=[eb4[tt]], w=[ebsel[tt]])
                                E("act", lambda e, tt=tt, eb2=eb2: e.copy(ebsel[tt][64:128, :], eb2[64:128, :, 1]),
                                  r=[eb4[tt]], w=[ebsel[tt]])

                            ggs = [gate_gen(tt) for tt in range(TPG)]
                            while ggs:
                                for gg_ in list(ggs):
                                    try:
                                        next(gg_)
                                    except StopIteration:
                                        ggs.remove(gg_)
                            chk(138)
                            ring_issue()
                            chk(14)
                            rb_ = ring_get()
                            for tt in range(TPG):
                                b = bank()
                                mm_group(b[:], [(hT[:, k, tt * 128:(tt + 1) * 128], rb_[:, k, :]) for k in range(8)],
                                         r=[rb_, hT], w=[b])
                                E("dve", lambda e, b=b, tt=tt: e.tensor_tensor(
                                    vpr[tt][:, :, 0:128], b[:].rearrange("p (a b) -> p a b", a=4),
                                    w4[tt][:].unsqueeze(2).to_broadcast([128, 4, 128]), ALU.mult), r=[b, w4[tt]], w=[vpr[tt]])
                                E("dve", lambda e, tt=tt: e.tensor_copy(vpr[tt][:, :, 128:129], w4[tt][:].unsqueeze(2)),
                                  r=[w4[tt]], w=[vpr[tt]])
                            ring_issue()
                            chk(15)
                            if pss == 2:
                                rb_ = ring_get()
                                for tt in range(TPG):
                                    b = bank()
                                    mm_group(b[:], [(hT[:, k, tt * 128:(tt + 1) * 128], rb_[:, k, :]) for k in range(8)],
                                             r=[rb_, hT], w=[b])
                                    E("act", lambda e, b=b, tt=tt: e.activation(som[tt][:], b[:], AF.Sigmoid), r=[b], w=[som[tt]])
                                    E("dve", lambda e, tt=tt: e.tensor_tensor(som[tt][:], som[tt][:], pl("wmn"), ALU.mult),
                                      r=[som[tt], prml], w=[som[tt]])
                                ring_issue()
                            chk(16)
                            free_banks = list(range(8))
                            CUR["free"] = free_banks

                            def galloc():
                                while not free_banks:
                                    yield
                                return banks[free_banks.pop(0)]

                            def grel(b_):
                                free_banks.append(banks.index(b_))

                            def mlstm_gen(tt, g=g):
                                t = g * TPG + tt
                                cs = slice(tt * 128, (tt + 1) * 128)
                                ktok_ = ktok[tt]
                                if pss == 2:
                                    sd_, den4_, r4_, hm_, ss4_, rn4_, hmo_, sqj_ = (
                                        sd[tt], den4[tt], r4[tt], hm[tt], ss4[tt], rn4[tt], hmo[tt], sqj[tt])
                                b = yield from galloc()
                                bb = b[:].bitcast(BF16)
                                for j in range(2):
                                    E("pe", lambda e, j=j, bb=bb: e.transpose(bb[:, j * 128:(j + 1) * 128], qkT[:, 2 + j, cs],
                                                                               ident_b[:]),
                                      r=[qkT, ident_b], w=[b], inc=(j == 1))
                                bb3 = bb[:, 0:256].rearrange("p (j c) -> p j c", j=2)
                                for w_ in range(2):
                                    E("act", lambda e, w_=w_: e.copy(ktok_[:, :, w_, w_ * 64:(w_ + 1) * 64],
                                                                     bb3[:, :, w_ * 64:(w_ + 1) * 64]), r=[b], w=[ktok_])
                                grel(b)
                                if pss == 2:
                                    bsr = [(yield from galloc()), (yield from galloc())]
                                    for h in range(4):
                                        p0 = 64 * (h % 2)
                                        rg, jj = h % 2, h // 2
                                        E("pe", lambda e, h=h, p0=p0, rg=rg, jj=jj: e.matmul(
                                            bsr[rg][:, jj * 128:(jj + 1) * 128], qkT[p0:p0 + 64, 2 + h // 2, cs],
                                            qkT[p0:p0 + 64, h // 2, cs], start=True, stop=True), r=[qkT], w=[bsr[rg]], inc=True)
                                    sd4 = sd_[:].rearrange("p (j r) l -> p j r l", r=2)
                                    for rg in range(2):
                                        E("dve", lambda e, rg=rg: e.tensor_tensor(
                                            sd4[:, :, rg, :], bsr[rg][:, 0:256].rearrange("p (a b) -> p a b", a=2),
                                            tri_f.unsqueeze(1).to_broadcast([128, 2, 128]), ALU.mult), r=[bsr[rg], cst], w=[sd_])
                                    grel(bsr[0])
                                    grel(bsr[1])
                                    bn = [(yield from galloc()), (yield from galloc())]
                                    for h in range(4):
                                        p0 = 64 * (h % 2)
                                        o = bn[h // 2][:, (h % 2) * 256:(h % 2) * 256 + 129]
                                        E("pe", lambda e, h=h, o=o: e.matmul(o, sd_[:, h, :], vpr[tt][:, h, 0:129], start=True,
                                                                             stop=False),
                                          r=[sd_, vpr[tt]], w=[bn[h // 2]], inc=False)
                                        E("pe", lambda e, h=h, o=o, p0=p0: e.matmul(o, qkT[p0:p0 + 64, h // 2, cs],
                                                                                    state_b[p0:p0 + 64, h // 2, :],
                                                                                    start=False, stop=True),
                                          r=[qkT, state_b], w=[bn[h // 2]], inc=True)
                                bd = yield from galloc()
                                for j in range(2):
                                    o = bd[:, j * 256:j * 256 + 129]
                                    E("pe", lambda e, j=j, o=o: e.matmul(o, ktok_[:, j, 0, :], vpr[tt][:, 2 * j, 0:129],
                                                                         start=True, stop=False),
                                      r=[ktok_, vpr[tt]], w=[bd], inc=False)
                                    E("pe", lambda e, j=j, o=o: e.matmul(o, ktok_[:, j, 1, :], vpr[tt][:, 2 * j + 1, 0:129],
                                                                         start=False, stop=True),
                                      r=[ktok_, vpr[tt]], w=[bd], inc=True)
                                E("dve", lambda e: e.tensor_tensor(
                                    state_f[:], bd[:, 0:512].rearrange("p (j c) -> p j c", j=2)[:, :, 0:129], state_f[:],
                                    ALU.add), r=[state_f, bd], w=[state_f])
                                E("dve", lambda e: e.tensor_tensor(
                                    state_f[:], state_f[:], ebsel[tt][:].unsqueeze(2).to_broadcast([128, 2, 129]), ALU.mult),
                                  r=[state_f, ebsel[tt]], w=[state_f])
                                E("act", lambda e: e.copy(state_b[:], state_f[:]), r=[state_f], w=[state_b])
                                grel(bd)
                                yield "state_done"
                                if pss != 2:
                                    return
                                for j in range(2):
                                    v3 = bn[j][:].rearrange("p (a b) -> p a b", a=2)
                                    E("dve", lambda e, j=j, v3=v3: e.scalar_tensor_tensor(
                                        den4_[:, 2 * j:2 * j + 2], v3[:, :, 128], 0.125, sc4[tt][:, 2 * j:2 * j + 2],
                                        ALU.mult, ALU.mult), r=[bn[j], sc4[tt]], w=[den4_])
                                E("act", lambda e: e.activation(den4_[:], den4_[:], AF.Abs), r=[den4_], w=[den4_])
                                yield
                                E("dve", lambda e: e.tensor_scalar(den4_[:], den4_[:], 1.0, None, ALU.max), r=[den4_], w=[den4_])
                                E("dve", lambda e: e.reciprocal(den4_[:], den4_[:]), r=[den4_], w=[den4_])
                                E("dve", lambda e: e.scalar_tensor_tensor(r4_[:], sc4[tt][:], 0.125, den4_[:], ALU.mult, ALU.mult),
                                  r=[sc4[tt], den4_], w=[r4_])
                                yield
                                for j in range(2):
                                    v3 = bn[j][:].rearrange("p (a b) -> p a b", a=2)
                                    E("dve", lambda e, j=j, v3=v3: e.tensor_tensor(
                                        hm_[:, 2 * j:2 * j + 2, :], v3[:, :, 0:128],
                                        r4_[:, 2 * j:2 * j + 2].unsqueeze(2).to_broadcast([128, 2, 128]), ALU.mult),
                                      r=[bn[j], r4_], w=[hm_])
                                E("dve", lambda e: e.memset(ss4_[:], 0.0), w=[ss4_])
                                grel(bn[0])
                                grel(bn[1])
                                yield
                                for h in range(4):
                                    E("act", lambda e, h=h: e.activation(sqj_[:], hm_[:, h, :], AF.Square,
                                                                         accum_out=ss4_[:, h:h + 1]),
                                      r=[hm_, ss4_], w=[sqj_, ss4_])
                                yield
                                E("dve", lambda e: e.tensor_scalar(ss4_[:], ss4_[:], 1.0 / 128, EPS, ALU.mult, ALU.add),
                                  r=[ss4_], w=[ss4_])
                                E("act", lambda e: e.sqrt(ss4_[:], ss4_[:]), r=[ss4_], w=[ss4_])
                                yield
                                E("dve", lambda e: e.reciprocal(rn4_[:], ss4_[:]), r=[ss4_], w=[rn4_])
                                E("dve", lambda e: e.tensor_tensor(hm_[:], hm_[:], rn4_[:].unsqueeze(2).to_broadcast([128, 4, 128]),
                                                                   ALU.mult), r=[hm_, rn4_], w=[hm_])
                                yield
                                hm2 = hm_[:].rearrange("p a b -> p (a b)")
                                E("dve", lambda e: e.tensor_tensor(hmo_[:], hm2, som[tt][:], ALU.mult), r=[hm_, som[tt]], w=[hmo_])
                                yield
                                b = yield from galloc()
                                bb = b[:].bitcast(BF16)
                                for f in range(4):
                                    E("pe", lambda e, f=f, bb=bb: e.transpose(bb[:, f * 128:(f + 1) * 128],
                                                                               hmo_[:, f * 128:(f + 1) * 128], ident_b[:]),
                                      r=[hmo_, ident_b], w=[b], inc=(f == 3))
                                E("act", lambda e, bb=bb: e.copy(hmT[:, :, cs], bb[:, 0:512].rearrange("p (a b) -> p a b", a=4)),
                                  r=[b], w=[hmT])
                                grel(b)

                            def swa_gen(tt, g=g):
                                t = g * TPG + tt
                                cs = slice(tt * 128, (tt + 1) * 128)
                                tmpS_, pT_, dn4_, hao_ = tmpS[tt], pT[tt], dn4[tt], hao[tt]
                                vprev = vaug[(t - 1) % 3] if t > 0 else vhalo
                                vcur = vaug[t % 3]
                                pc = slice(tt * 128, tt * 128 + 128)
                                cc = slice(128 + tt * 128, 256 + tt * 128)
                                for kv in range(2):
                                    bS = [(yield from galloc()), (yield from galloc())]
                                    for rg in range(2):
                                        for bi, ks in enumerate((pc, cc)):
                                            for gg in range(2):
                                                g4 = gg * 2 + rg
                                                h = kv * 4 + g4
                                                p0 = 64 * rg
                                                sl_ = (bi * 2 + gg) * 128
                                                E("pe", lambda e, rg=rg, ks=ks, sl_=sl_, h=h, p0=p0: e.matmul(
                                                    bS[rg][:, sl_:sl_ + 128], kaT[p0:p0 + 64, kv, ks],
                                                    qaT[p0:p0 + 64, h // 2, cs], start=True, stop=True),
                                                  r=[kaT, qaT], w=[bS[rg]], inc=True)
                                        boff = (kv * 2 + rg) * 512
                                        E("dve", lambda e, rg=rg, boff=boff: e.scalar_tensor_tensor(
                                            tmpS_[:, rg, :], bS[rg][:], 0.125, biasT[:, boff:boff + 512], ALU.mult, ALU.add),
                                          r=[bS[rg], biasT], w=[tmpS_])
                                    grel(bS[0])
                                    grel(bS[1])
                                    yield
                                    E("act", lambda e: e.activation(pT_[:], tmpS_[:], AF.Exp), r=[tmpS_], w=[pT_])
                                    yield
                                    bp = yield from galloc()
                                    for g4 in range(4):
                                        o = bp[:, g4 * 65:(g4 + 1) * 65]
                                        rg, gg = g4 % 2, g4 // 2
                                        E("pe", lambda e, rg=rg, gg=gg, o=o: e.matmul(
                                            o, pT_[:, rg, gg * 128:(gg + 1) * 128], vprev[:, kv, 0:65], start=True, stop=False),
                                          r=[pT_, vprev], w=[bp], inc=False)
                                        E("pe", lambda e, rg=rg, gg=gg, o=o: e.matmul(
                                            o, pT_[:, rg, (2 + gg) * 128:(3 + gg) * 128], vcur[:, kv, 0:65], start=False,
                                            stop=True), r=[pT_, vcur], w=[bp], inc=True)
                                    yield
                                    pv3 = bp[:, 0:260].rearrange("p (a b) -> p a b", a=4)
                                    E("dve", lambda e, pv3=pv3: e.tensor_tensor(dn4_[:], pv3[:, :, 64],
                                                                                esink[:, kv * 4:(kv + 1) * 4], ALU.add),
                                      r=[bp, esink], w=[dn4_])
                                    E("dve", lambda e: e.reciprocal(dn4_[:], dn4_[:]), r=[dn4_], w=[dn4_])
                                    E("dve", lambda e, pv3=pv3: e.tensor_tensor(
                                        hao_[:, kv * 256:(kv + 1) * 256].rearrange("p (a b) -> p a b", a=4), pv3[:, :, 0:64],
                                        dn4_[:].unsqueeze(2).to_broadcast([128, 4, 64]), ALU.mult), r=[bp, dn4_], w=[hao_])
                                    grel(bp)
                                    yield
                                b = yield from galloc()
                                bb = b[:].bitcast(BF16)
                                for f in range(4):
                                    E("pe", lambda e, f=f, bb=bb: e.transpose(bb[:, f * 128:(f + 1) * 128],
                                                                               hao_[:, f * 128:(f + 1) * 128], ident_b[:]),
                                      r=[hao_, ident_b], w=[b], inc=(f == 3))
                                E("act", lambda e, bb=bb: e.copy(haT[:, :, cs], bb[:, 0:512].rearrange("p (a b) -> p a b", a=4)),
                                  r=[b], w=[haT])
                                grel(b)

                            pending_m = [mlstm_gen(tt) for tt in range(TPG)]
                            if os.environ.get("KSEQ") == "1":
                                for gm in pending_m:
                                    for _ in gm:
                                        pass
                                pending_m = []
                                if pss == 2:
                                    for tt in range(TPG):
                                        for _ in swa_gen(tt):
                                            pass
                                pss_swa = False
                            elif os.environ.get("KSEQ") == "2":
                                pss_swa = False
                            else:
                                pss_swa = (pss == 2)
                            chains = []
                            if pss_swa:
                                chains += [swa_gen(tt) for tt in range(TPG)]
                            if g + 1 < NG and os.environ.get("KSEQ") is None:
                                def nnext_gen(g=g):
                                    for tt in range(TPG):
                                        yield from norm_gen((g + 1) * TPG + tt, a1, sh1, hT2[(g + 1) % 2], tt * 128, nsc,
                                                            alloc=galloc, rel=grel)
                                chains.append(nnext_gen())
                            elif g + 1 < NG:
                                for tt in range(TPG):
                                    norm_tile((g + 1) * TPG + tt, a1, sh1, hT2[(g + 1) % 2], tt * 128, nsc)
                            started = 0
                            mchains = []
                            next_ok = True
                            while pending_m or chains or mchains:
                                if pending_m and next_ok:
                                    gm = pending_m.pop(0)
                                    mchains.append(gm)
                                    next_ok = False
                                    fresh = gm
                                else:
                                    fresh = None
                                for gm in list(mchains):
                                    try:
                                        v = next(gm)
                                        if v == "state_done":
                                            next_ok = True
                                    except StopIteration:
                                        mchains.remove(gm)
                                        if gm is fresh or True:
                                            pass
                                for gs in list(chains):
                                    try:
                                        next(gs)
                                    except StopIteration:
                                        chains.remove(gs)
                                for gb_ in list(background):
                                    try:
                                        next(gb_)
                                    except StopIteration:
                                        background.remove(gb_)
                            if os.environ.get("KSEQ") == "2" and pss == 2:
                                for tt in range(TPG):
                                    for _ in swa_gen(tt):
                                        pass
                            chk(23)
                            E("act", lambda e: e.copy(kaT[:, :, 0:128], kaT[:, :, G:G + 128]), r=[kaT], w=[kaT])
                            chk(24)
                            if pss == 2:
                                for pi in range(4):
                                    rb_ = ring_get()
                                    for oi in range(2):
                                        ot = 2 * pi + oi
                                        bgm, bga, bbm, bba = bank(), bank(), bank(), bank()
                                        mm_group(bgm[:, 0:G], [(rb_[:, k, (2 * oi) * 128:(2 * oi + 1) * 128], hT[:, k, :])
                                                               for k in range(8)], r=[rb_, hT], w=[bgm])
                                        mm_group(bga[:, 0:G], [(rb_[:, k, (2 * oi + 1) * 128:(2 * oi + 2) * 128], hT[:, k, :])
                                                               for k in range(8)], r=[rb_, hT], w=[bga])
                                        mm_group(bbm[:, 0:G], [(wbr[:, 0, k, ot * 128:(ot + 1) * 128], hmT[:, k, :]) for k in range(4)],
                                                 r=[wbr, hmT], w=[bbm])
                                        mm_group(bba[:, 0:G], [(wbr[:, 1, k, ot * 128:(ot + 1) * 128], haT[:, k, :]) for k in range(4)],
                                                 r=[wbr, haT], w=[bba])
                                        E("act", lambda e, bgm=bgm: e.activation(sg1[:], bgm[:, 0:G], AF.Sigmoid), r=[bgm], w=[sg1])
                                        E("act", lambda e, bga=bga: e.activation(sg2[:], bga[:, 0:G], AF.Sigmoid), r=[bga], w=[sg2])
                                        E("dve", lambda e, bbm=bbm: e.tensor_tensor(sg1[:], bbm[:, 0:G], sg1[:], ALU.mult),
                                          r=[sg1, bbm], w=[sg1])
                                        E("dve", lambda e, bba=bba: e.tensor_tensor(sg2[:], bba[:, 0:G], sg2[:], ALU.mult),
                                          r=[sg2, bba], w=[sg2])
                                        E("dve", lambda e, ot=ot: e.tensor_tensor(mT[:, ot, :], sg1[:], sg2[:], ALU.add),
                                          r=[sg1, sg2], w=[mT])
                                    ring_issue()
                            chk(25)
                            for tt in (range(TPG) if pss == 2 else ()):
                                t = g * TPG + tt
                                for oc in range(2):
                                    b = bank()
                                    mm_group(b[:], [(mT[:, k, tt * 128:(tt + 1) * 128], wout[:, k, oc * 512:(oc + 1) * 512])
                                                    for k in range(8)], r=[mT, wout], w=[b])
                                    E("dve", lambda e, b=b, t=t, oc=oc: e.tensor_tensor(
                                        x[:, t, oc * 512:(oc + 1) * 512], b[:], x[:, t, oc * 512:(oc + 1) * 512], ALU.add),
                                      r=[b, xt[t]], w=[xt[t]])
                    except _Stop:
                        pass
                    if pss == 1:
                        lastc = slice(128 + (TPG - 1) * 128, 256 + (TPG - 1) * 128)
                        vlast = vaug[(NT - 1) % 3]
                        DMA("sp", ibv[:, 0:258].rearrange("p (a b) -> p a b", a=2), state_f[:], r=[state_f], w=[ib_parts[0]], key="x_st")
                        for i in range(4):
                            DMA("sp", ibv[:, 258 + 3 * i:261 + 3 * i], qkpre[i][:, 0:3], r=[qkpre[i]], w=[ib_parts[1 + i]], key=f"x_cv{i}")
                        DMA("pool", ibv[:, 270:526].rearrange("p (a b) -> p a b", a=2), kaT[:, :, 0:128], r=[kaT], w=[ib_parts[5]], key="x_ka")
                        DMA("pool", ibv[:, 526:656].rearrange("p (a b) -> p a b", a=2), vlast[:, :, 0:65], r=[vlast], w=[ib_parts[6]], key="x_va")
                        sy.emit_cc(lambda e: e.collective_compute("AllGather", ALU.bypass,
                                                                  replica_groups=[[2 * i, 2 * i + 1] for i in range(NCORES // 2)],
                                                                  ins=[ibv.opt()], outs=[obv.opt()]),
                                   reads=[t.r for t in ib_parts], writes=[obR.r], key=f"cc{l}")
                sy.barrier()
                p2s.close()

            if not moe:
                continue
            with ExitStack() as ms:
                h2T = mk(f"h2T_{l}", [128, 8, NTOK], BF16, ms)
                gates = mk(f"gates_{l}", [128, NT, NE], F32, ms)
                xnT2 = [mk(f"xnT{i}_{l}", [128, D], F32, ms) for i in range(2)]
                xnT = xnT2[0]
                wr2 = mk(f"wr2_{l}", [128, 8, NE], F32, ms)
                cbc = mk(f"cbc_{l}", [128, NE], F32, ms)
                nsc2 = [(None, mk(f"ssumb{i}_{l}", [128, 1], F32, ms),
                         mk(f"tmp1b{i}_{l}", [128, 1], F32, ms), mk(f"tmp2b{i}_{l}", [128, 1], F32, ms),
                         mk(f"rstdb{i}_{l}", [128, 1], F32, ms), mk(f"xnb{i}_{l}", [128, D], F32, ms)) for i in range(2)]
                wgb = [mk(f"wgb{i}_{l}", [128, 8, DE], BF16, ms) for i in range(2)]
                wub = [mk(f"wub{i}_{l}", [128, 8, DE], BF16, ms) for i in range(2)]
                wdb = [mk(f"wdb{i}_{l}", [128, 4, D], BF16, ms) for i in range(2)]
                actT = [mk(f"actT{i}_{l}", [128, 4, 512], BF16, ms) for i in range(2)]
                sil = [mk(f"sil{i}_{l}", [128, 512], F32, ms) for i in range(2)]
                R16s = [{n: mk(f"r_{n}{i}_{l}", [128, 16], F32, ms) for n in
                         ["sc", "sel", "selm", "m1", "selm2", "m2", "wts"]} for i in range(2)]
                R4s = [{n: mk(f"q_{n}{i}_{l}", [128, 4], F32, ms) for n in
                        ["hi1", "lo1", "hi2", "lo2", "top1", "mm2", "mm3", "grp", "gmask"]} for i in range(2)]
                R1s = [{n: mk(f"s_{n}{i}_{l}", [128, 1], F32, ms) for n in ["gmax", "t1", "t2", "den"]} for i in range(2)]

                def load_expert(e_):
                    i = e_ % 2
                    DMA("pool", wgb[i][:], wg_d[l, e_].rearrange("(k p) n -> p k n", p=128), w=[wgb[i]], key=f"wg{i}")
                    DMA("pool", wub[i][:], wu_d[l, e_].rearrange("(k p) n -> p k n", p=128), w=[wub[i]], key=f"wu{i}")
                    DMA("pool", wdb[i][:], wd_d[l, e_].rearrange("(k p) n -> p k n", p=128), w=[wdb[i]], key=f"wd{i}")

                load_expert(0)
                load_expert(1)
                E("dve", lambda e: e.tensor_tensor(wr2[:], pg("wr").rearrange("p (a b) -> p a b", a=8),
                                                   a2[:].unsqueeze(2).to_broadcast([128, 8, NE]), ALU.mult),
                  r=[prmg, a2], w=[wr2])
                sh2bc = sil[0]
                sh2bc = xnT
                sh2v = sh2bc[:].rearrange("p (a b) -> p a b", a=8)
                E("dve", lambda e: e.tensor_copy(sh2v, sh2.unsqueeze(2).to_broadcast([128, 8, 128])),
                  r=[modc], w=[sh2bc])
                b = bank()
                mm_group(b[:, 0:NE], [(sh2v[:, k, :], pg("wr", k * NE, (k + 1) * NE)) for k in range(8)],
                         r=[sh2bc, prmg], w=[b])
                E("dve", lambda e, b=b: e.tensor_copy(cbc[:], b[:, 0:NE]), r=[b], w=[cbc])

                mfree = list(range(8))

                def malloc_():
                    while not mfree:
                        yield
                    return banks[mfree.pop(0)]

                def mrel(b_):
                    mfree.append(banks.index(b_))

                def tile_chain(t):
                    par = t % 2
                    xnT = xnT2[par]
                    R16, R4, R1 = R16s[par], R4s[par], R1s[par]
                    yield from norm_gen(t, a2, sh2, h2T, t * 128, nsc2[par], router_dst=xnT, alloc=malloc_, rel=mrel)
                    yield "norm_done"
                    b = yield from malloc_()
                    mm_group(b[:, 0:NE], [(xnT[:, k * 128:(k + 1) * 128], wr2[:, k, :]) for k in range(8)],
                             r=[xnT, wr2], w=[b])
                    r16, r4_, r1 = R16, R4, R1
                    E("dve", lambda e, b=b: e.tensor_tensor(r16["sel"][:], b[:, 0:NE], cbc[:], ALU.add),
                      r=[b, cbc], w=[r16["sel"]])
                    mrel(b)
                    yield
                    E("act", lambda e: e.activation(r16["sc"][:], r16["sel"][:], AF.Sigmoid), r=[r16["sel"]], w=[r16["sc"]])
                    E("dve", lambda e: e.tensor_tensor(r16["sel"][:], r16["sc"][:], pg("rb"), ALU.add),
                      r=[r16["sc"], prmg], w=[r16["sel"]])
                    s3 = r16["sel"][:].rearrange("p (a b) -> p a b", a=4)

                    def tt_(o, i0, i1, op, rr, ww):
                        E("dve", lambda e: e.tensor_tensor(o, i0, i1, op), r=rr, w=ww)
                    tt_(r4_["hi1"][:], s3[:, :, 0], s3[:, :, 1], ALU.max, [r16["sel"]], [r4_["hi1"]])
                    tt_(r4_["lo1"][:], s3[:, :, 0], s3[:, :, 1], ALU.min, [r16["sel"]], [r4_["lo1"]])
                    tt_(r4_["hi2"][:], s3[:, :, 2], s3[:, :, 3], ALU.max, [r16["sel"]], [r4_["hi2"]])
                    tt_(r4_["lo2"][:], s3[:, :, 2], s3[:, :, 3], ALU.min, [r16["sel"]], [r4_["lo2"]])
                    tt_(r4_["top1"][:], r4_["hi1"][:], r4_["hi2"][:], ALU.max, [r4_["hi1"], r4_["hi2"]], [r4_["top1"]])
                    tt_(r4_["mm2"][:], r4_["hi1"][:], r4_["hi2"][:], ALU.min, [r4_["hi1"], r4_["hi2"]], [r4_["mm2"]])
                    tt_(r4_["mm3"][:], r4_["lo1"][:], r4_["lo2"][:], ALU.max, [r4_["lo1"], r4_["lo2"]], [r4_["mm3"]])
                    tt_(r4_["mm2"][:], r4_["mm2"][:], r4_["mm3"][:], ALU.max, [r4_["mm2"], r4_["mm3"]], [r4_["mm2"]])
                    tt_(r4_["grp"][:], r4_["top1"][:], r4_["mm2"][:], ALU.add, [r4_["top1"], r4_["mm2"]], [r4_["grp"]])
                    yield
                    E("dve", lambda e: e.tensor_reduce(r1["gmax"][:], r4_["grp"][:], AX.X, ALU.max),
                      r=[r4_["grp"]], w=[r1["gmax"]])
                    E("dve", lambda e: e.tensor_scalar(r4_["gmask"][:], r4_["grp"][:], r1["gmax"][:, 0:1], None, ALU.is_ge),
                      r=[r4_["grp"], r1["gmax"]], w=[r4_["gmask"]])
                    E("dve", lambda e: e.tensor_scalar(r4_["gmask"][:], r4_["gmask"][:], 1e9, -1e9, ALU.mult, ALU.add),
                      r=[r4_["gmask"]], w=[r4_["gmask"]])
                    E("dve", lambda e: e.tensor_tensor(r16["selm"][:].rearrange("p (a b) -> p a b", a=4), s3,
                                                       r4_["gmask"][:].unsqueeze(2).to_broadcast([128, 4, 4]), ALU.add),
                      r=[r16["sel"], r4_["gmask"]], w=[r16["selm"]])
                    yield
                    E("dve", lambda e: e.tensor_reduce(r1["t1"][:], r16["selm"][:], AX.X, ALU.max),
                      r=[r16["selm"]], w=[r1["t1"]])
                    E("dve", lambda e: e.tensor_scalar(r16["m1"][:], r16["selm"][:], r1["t1"][:, 0:1], None, ALU.is_ge),
                      r=[r16["selm"], r1["t1"]], w=[r16["m1"]])
                    E("dve", lambda e: e.scalar_tensor_tensor(r16["selm2"][:], r16["m1"][:], -1e9, r16["selm"][:],
                                                              ALU.mult, ALU.add),
                      r=[r16["m1"], r16["selm"]], w=[r16["selm2"]])
                    yield
                    E("dve", lambda e: e.tensor_reduce(r1["t2"][:], r16["selm2"][:], AX.X, ALU.max),
                      r=[r16["selm2"]], w=[r1["t2"]])
                    E("dve", lambda e: e.tensor_scalar(r16["m2"][:], r16["selm2"][:], r1["t2"][:, 0:1], None, ALU.is_ge),
                      r=[r16["selm2"], r1["t2"]], w=[r16["m2"]])
                    tt_(r16["m1"][:], r16["m1"][:], r16["m2"][:], ALU.add, [r16["m1"], r16["m2"]], [r16["m1"]])
                    tt_(r16["wts"][:], r16["sc"][:], r16["m1"][:], ALU.mult, [r16["sc"], r16["m1"]], [r16["wts"]])
                    yield
                    E("dve", lambda e: e.tensor_reduce(r1["den"][:], r16["wts"][:], AX.X, ALU.add),
                      r=[r16["wts"]], w=[r1["den"]])
                    E("dve", lambda e: e.reciprocal(r1["den"][:], r1["den"][:]), r=[r1["den"]], w=[r1["den"]])
                    E("dve", lambda e, t=t: e.tensor_scalar(gates[:, t, :], r16["wts"][:], r1["den"][:, 0:1], None, ALU.mult),
                      r=[r16["wts"], r1["den"]], w=[gates])


                tchains = []
                nxt = 0
                ok = True
                while nxt < NT or tchains:
                    if ok and nxt < NT and len(tchains) < 2:
                        tchains.append(tile_chain(nxt))
                        nxt += 1
                        ok = False
                    for gtc in list(tchains):
                        try:
                            v = next(gtc)
                            if v == "norm_done":
                                ok = True
                        except StopIteration:
                            tchains.remove(gtc)
                            ok = True
                for e_ in range(NE):
                    i = e_ % 2
                    for k in range(4):
                        E("dve", lambda e, k=k, i=i: e.tensor_tensor(wdb[i][:, k, :], wdb[i][:, k, :], g2bc[:], ALU.mult),
                          r=[wdb[i], g2bc], w=[wdb[i]])
                    for gq in range(NTOK // 512):
                        at = actT[gq % 2]
                        ts = slice(gq * 512, (gq + 1) * 512)
                        for dt_ in range(4):
                            bg, bu = bank(), bank()
                            mm_group(bg[:], [(wgb[i][:, k, dt_ * 128:(dt_ + 1) * 128], h2T[:, k, ts]) for k in range(8)],
                                     r=[wgb[i], h2T], w=[bg])
                            mm_group(bu[:], [(wub[i][:, k, dt_ * 128:(dt_ + 1) * 128], h2T[:, k, ts]) for k in range(8)],
                                     r=[wub[i], h2T], w=[bu])
                            sl = sil[dt_ % 2]
                            E("act", lambda e, bg=bg, sl=sl: e.activation(sl[:], bg[:], AF.Silu), r=[bg], w=[sl])
                            E("dve", lambda e, bu=bu, sl=sl, at=at, dt_=dt_: e.tensor_tensor(at[:, dt_, :], bu[:], sl[:],
                                                                                           ALU.mult),
                              r=[sl, bu], w=[at])
                        for tt in range(4):
                            t = gq * 4 + tt
                            for oc in range(2):
                                b = bank()
                                mm_group(b[:], [(at[:, k, tt * 128:(tt + 1) * 128], wdb[i][:, k, oc * 512:(oc + 1) * 512])
                                                for k in range(4)], r=[at, wdb[i]], w=[b])
                                E("dve", lambda e, b=b, t=t, oc=oc, e_=e_: e.scalar_tensor_tensor(
                                    x[:, t, oc * 512:(oc + 1) * 512], b[:], gates[:, t, e_:e_ + 1],
                                    x[:, t, oc * 512:(oc + 1) * 512], ALU.mult, ALU.add),
                                  r=[b, gates, xt[t]], w=[xt[t]])
                    if e_ + 2 < NE:
                        load_expert(e_ + 2)
                sy.barrier()

        yv = y_d.rearrange("(t p) d -> p t d", p=128)
        if final:
            with ExitStack() as ms:
                fss = [(mk(f"fjunk{i}", [128, D], BF16, ms), mk(f"fssum{i}", [128, 1], F32, ms),
                        mk(f"ftmp1{i}", [128, 1], F32, ms), mk(f"ftmp2{i}", [128, 1], F32, ms),
                        mk(f"frstd{i}", [128, 1], F32, ms)) for i in range(2)]
                yb = [mk(f"yb{i}", [128, D], F32, ms) for i in range(2)]

                def fin_gen(t):
                    junk, ssum, tmp1, tmp2, rstd = fss[t % 2]
                    E("dve", lambda e: e.memset(ssum[:], 0.0), w=[ssum])
                    E("act", lambda e, t=t: e.activation(junk[:], x[:, t, :], AF.Square, accum_out=ssum[:, 0:1]),
                      r=[xt[t], ssum], w=[junk, ssum])
                    yield
                    E("dve", lambda e: e.tensor_scalar(tmp1[:], ssum[:], 1.0 / D, EPS, ALU.mult, ALU.add), r=[ssum], w=[tmp1])
                    E("act", lambda e: e.sqrt(tmp2[:], tmp1[:]), r=[tmp1], w=[tmp2])
                    yield
                    E("dve", lambda e: e.reciprocal(rstd[:], tmp2[:]), r=[tmp2], w=[rstd])
                    o = yb[t % 2]
                    E("dve", lambda e, t=t, o=o: e.scalar_tensor_tensor(o[:], x[:, t, :], rstd[:, 0:1], pg("wfin"),
                                                                        ALU.mult, ALU.mult),
                      r=[xt[t], rstd, prmg], w=[o])
                    yield
                    DMA("sp", yv[:, t, :], o[:], r=[o], key=f"out{t % 2}")

                fch = []
                nxt_ = 0
                while nxt_ < NT or fch:
                    if nxt_ < NT and len(fch) < 2:
                        fch.append(fin_gen(nxt_))
                        nxt_ += 1
                    for gf in list(fch):
                        try:
                            next(gf)
                        except StopIteration:
                            fch.remove(gf)
                sy.barrier()
        else:
            for t in range(NT):
                DMA("sp", yv[:, t, :], x[:, t, :], r=[xt[t]], key="out")
            sy.barrier()
    return nc


def _t5_bucket(n):
    max_exact = 16
    large = max_exact + (np.log(np.maximum(n, 1) / max_exact) / math.log(128 / max_exact) * (32 - max_exact)).astype(np.int32)
    large = np.minimum(large, 31)
    return np.where(n < max_exact, n, large).astype(np.int32)


def _colmajor(v, ncol):
    return np.ascontiguousarray(np.asarray(v, np.float32).reshape(ncol, 128).T)


def _bc(v):
    v = np.asarray(v, np.float32).reshape(1, -1)
    return np.ascontiguousarray(np.broadcast_to(v, (128, v.shape[1])))


_WIN_PERM = None


def _win_perm():
    q_m = list(range(0, 256)); k_m = list(range(256, 512)); v_m = list(range(512, 1024)); o_m = list(range(1024, 1536))
    i_g = list(range(1536, 1540)); f_g = list(range(1540, 1544)); q_a = list(range(1544, 2056))
    k_a = list(range(2056, 2184)); v_a = list(range(2184, 2312)); g_m = list(range(2312, 3336)); g_a = list(range(3336, 4360))
    cols = []
    cols += q_m + k_m
    cols += q_a
    cols += k_a[0:64] + k_a[0:64] + k_a[64:128] + k_a[64:128] + i_g + f_g + v_a
    cols += v_m
    cols += o_m
    for pi in range(4):
        for oi in range(2):
            ot = 2 * pi + oi
            cols += g_m[ot * 128:(ot + 1) * 128] + g_a[ot * 128:(ot + 1) * 128]
    assert len(cols) == NWIN
    return np.asarray(cols, np.int64)


def _consts(rel_bias):
    cst = np.zeros((128, 384), np.float32)
    cst[:, 0:128] = np.eye(128, dtype=np.float32)
    s = np.arange(128)[:, None]
    l_ = np.arange(128)[None, :]
    cst[:, 128:256] = (s <= l_).astype(np.float32)
    cst[:, 256:384] = 1.0
    q = np.arange(128)[None, :]
    sk = np.arange(128)[:, None]
    bT = np.zeros((128, 2, 2, 2, 2, 128), np.float32)
    for blk in range(2):
        dist = q + 128 - (sk + blk * 128)
        ok = (dist >= 0) & (dist < 128)
        bucket = _t5_bucket(np.maximum(dist, 0))
        vals = np.asarray(rel_bias, np.float32)[bucket]
        vals = np.where(ok[:, :, None], vals, np.float32(-30000.0))
        for h in range(8):
            kv, g4 = h // 4, h % 4
            bT[:, kv, g4 % 2, blk, g4 // 2, :] = vals[:, :, h]
    return cst, np.ascontiguousarray(bT.reshape(128, 2048))


_PROGS = {}


def _prog(nl, final):
    key = (nl, final)
    if key not in _PROGS:
        _PROGS[key] = build(nl, final)
    return _PROGS[key]


def _prep_common(inp):
    perm = _win_perm()
    cst, biasT = _consts(inp["rel_bias"])
    return perm, cst, biasT


def _layer_arrays(inp, ls, perm):
    f = lambda a: np.ascontiguousarray(np.asarray(a, np.float32))
    d = {}
    d["w_ada"] = f(inp["w_ada"][ls])
    d["w_in"] = f(np.asarray(inp["w_in"])[ls][:, :, perm])
    d["w_br"] = f(np.stack([np.asarray(inp["w_br_m"])[ls], np.asarray(inp["w_br_a"])[ls]], axis=1))
    d["w_out"] = f(inp["w_out"][ls])
    d["wg"] = f(inp["w_gate_e"][ls])
    d["wu"] = f(inp["w_up_e"][ls])
    d["wd"] = f(inp["w_down_e"][ls])
    prml = []
    for l in ls:
        ba = np.asarray(inp["b_ada"][l], np.float32)
        parts = [
            _colmajor(inp["w_norm1"][l], 8), _colmajor(inp["w_norm2"][l], 8), _colmajor(ba, 48),
            np.ascontiguousarray(np.asarray(inp["conv_w"][l], np.float32).reshape(4, 4, 128).transpose(2, 1, 0).reshape(128, 16)),
            _colmajor(inp["conv_b"][l], 4),
            _bc(np.concatenate([np.asarray(inp["b_igate"][l]), np.asarray(inp["b_fgate"][l])])),
            _bc(inp["w_mnorm"][l]), _bc(inp["sinks"][l]),
        ]
        p = np.concatenate(parts, axis=1)
        assert p.shape == (128, NPL), p.shape
        prml.append(p)
    d["prml"] = np.ascontiguousarray(np.stack(prml, 0))
    d["prmb"] = np.ascontiguousarray(np.stack([np.concatenate(
        [_bc(np.asarray(inp["b_ada"][l], np.float32)[2048:3072]), _bc(np.asarray(inp["b_ada"][l], np.float32)[5120:6144])],
        axis=1) for l in ls], 0))
    return d


def _prmg(inp, b, half):
    wr = np.asarray(inp["w_router"], np.float32).reshape(8, 128, NE).transpose(1, 0, 2).reshape(128, 8 * NE)
    parts = [_colmajor(np.asarray(inp["c"])[b], 8), np.full((128, 1), float(half), np.float32),
             _bc(inp["router_bias"]), np.ascontiguousarray(wr), _bc(inp["w_final"])]
    p = np.concatenate(parts, axis=1)
    assert p.shape == (128, NPG)
    return np.ascontiguousarray(p)


def kernel(**inp):
    inp = {k: np.asarray(v) for k, v in inp.items()}
    perm, cst, biasT = _prep_common(inp)
    la = _layer_arrays(inp, [0, 1], perm)
    nc = _prog(2, True)
    in_maps = []
    for c in range(8):
        m = {"x": np.ascontiguousarray(inp["x"][c // 2, (c % 2) * NTOK:(c % 2 + 1) * NTOK, :], dtype=np.float32),
             "prmg": _prmg(inp, c // 2, c % 2), "cst": cst, "biasT": biasT}
        m.update(la)
        in_maps.append(m)
    res = run_bass_kernel_spmd(nc, in_maps, core_ids=list(range(8)))
    out = np.zeros((4, 4096, D), np.float32)
    for c in range(8):
        out[c // 2, (c % 2) * NTOK:(c % 2 + 1) * NTOK, :] = np.asarray(res.results[c]["y"], dtype=np.float32)
    return out
```

```python
import math
import os
from contextlib import ExitStack

import numpy as np
import concourse.bass as bass
import concourse.mybir as mybir
from concourse.bass_utils import run_bass_kernel_spmd

F32 = mybir.dt.float32
BF16 = mybir.dt.bfloat16
AF = mybir.ActivationFunctionType
ALU = mybir.AluOpType
AX = mybir.AxisListType

D = 1024
NTOK = 2048
NT = 16
G = 256
TPG = G // 128
NG = NTOK // G
EPS = 1e-6
NE = 16
DE = 512
NWIN = 4488
LN8 = math.log(0.125)
NCORES = 8

PL = {}
_o = 0
for _n, _w in [("wn1", 8), ("wn2", 8), ("bada", 48), ("convw", 16),
               ("convb", 4), ("bif", 8), ("wmn", 512), ("sinks", 8)]:
    PL[_n] = (_o, _o + _w)
    _o += _w
NPL = _o
PG = {}
_o = 0
for _n, _w in [("cT", 8), ("flag", 1), ("rb", 16), ("wr", 128), ("wfin", 1024)]:
    PG[_n] = (_o, _o + _w)
    _o += _w
NPG = _o


class Res:
    __slots__ = ("name", "w", "r")

    def __init__(self, name):
        self.name = name
        self.w = None
        self.r = {}


class Sync:
    def __init__(self, nc, es):
        self.nc = nc
        self.es = es
        self.engs = {"pe": nc.tensor, "act": nc.scalar, "dve": nc.vector, "pool": nc.gpsimd, "sp": nc.sync}
        self.sems = {}
        self.cnt = {}
        for k in self.engs:
            self.sems[k] = es.enter_context(nc.semaphore("s_" + k))
            self.cnt[k] = 0
        self.seen = {k: {} for k in self.engs}

    def dma_sem(self, key):
        if key not in self.sems:
            self.sems[key] = self.es.enter_context(self.nc.semaphore("d_" + key))
            self.cnt[key] = 0
        return key

    def need(self, eng, ev, same_ok=False):
        if ev is None:
            return
        k, v = ev
        if k == eng and not same_ok:
            return
        if self.seen[eng].get(k, 0) >= v:
            return
        self.engs[eng].wait_ge(self.sems[k], v)
        self.seen[eng][k] = v

    def deps(self, eng, reads, writes, force_same=False):
        for r in reads:
            self.need(eng, r.w, same_ok=True)
        for w in writes:
            self.need(eng, w.w, same_ok=force_same)
            for k, v in w.r.items():
                self.need(eng, (k, v), same_ok=force_same)

    def emit(self, eng, fn, reads=(), writes=(), inc=True):
        self.deps(eng, reads, writes)
        inst = fn(self.engs[eng])
        if inc:
            inst.then_inc(self.sems[eng], 1)
            self.cnt[eng] += 1
            ev = (eng, self.cnt[eng])
        else:
            ev = (eng, self.cnt[eng] + 1)
        for r in reads:
            r.r[eng] = max(r.r.get(eng, 0), ev[1])
        for w in writes:
            w.w = ev
            w.r = {}
        return inst

    def dma(self, queue, out, in_, reads=(), writes=(), semkey=None):
        self.deps(queue, reads, writes, force_same=True)
        semkey = self.dma_sem(semkey)
        inst = self.engs[queue].dma_start(out=out, in_=in_)
        inst.then_inc(self.sems[semkey], 16)
        self.cnt[semkey] += 16
        ev = (semkey, self.cnt[semkey])
        for r in reads:
            r.r[semkey] = ev[1]
        for w in writes:
            w.w = ev
            w.r = {}
        return inst

    def emit_cc(self, fn, reads, writes, key):
        self.deps("pool", reads, writes, force_same=True)
        key = self.dma_sem(key)
        inst = fn(self.engs["pool"])
        inst.then_inc(self.sems[key])
        self.cnt[key] = 1
        ev = (key, 1)
        for r in reads:
            r.r[key] = 1
        for w in writes:
            w.w = ev
            w.r = {}
        return inst

    def barrier(self, skip=()):
        for e in self.engs:
            for k in self.sems:
                if k != e and self.cnt[k] > 0 and k not in skip:
                    self.need(e, (k, self.cnt[k]))


class T:
    def __init__(self, es, nc, name, shape, dtype, psum=False):
        if psum:
            self.t = es.enter_context(nc.psum_tensor("p_" + name, shape, dtype))
        else:
            self.t = es.enter_context(nc.sbuf_tensor("t_" + name, shape, dtype))
        self.r = Res(name)
        self.name = name
        self.psum = psum

    def __getitem__(self, k):
        return self.t[k]


def build(nl, final, moe=True, dbg=99):
    nc = bass.Bass("TRN2", target_bir_lowering=False)

    def din(name, shape, dt=F32):
        return nc.dram_tensor(name, shape, dt, kind="ExternalInput").ap()

    def dout(name, shape, dt=F32):
        return nc.dram_tensor(name, shape, dt, kind="ExternalOutput").ap()

    x_d = din("x", [NTOK, D])
    prmg_d = din("prmg", [128, NPG])
    prml_d = din("prml", [nl, 128, NPL])
    prmb_d = din("prmb", [nl, 128, 2048])
    cst_d = din("cst", [128, 384])
    biasT_d = din("biasT", [128, 2048])
    wada_d = din("w_ada", [nl, D, 6 * D])
    win_d = din("w_in", [nl, D, NWIN])
    wbr_d = din("w_br", [nl, 2, 512, D])
    wout_d = din("w_out", [nl, D, D])
    wg_d = din("wg", [nl, NE, D, DE])
    wu_d = din("wu", [nl, NE, D, DE])
    wd_d = din("wd", [nl, NE, DE, D])
    y_d = dout("y", [NTOK, D])

    with ExitStack() as es:
        sy = Sync(nc, es)

        def E(eng, fn, r=(), w=(), inc=True):
            w = list(w) + [t for t in r if getattr(t, "psum", False) and t not in w]
            return sy.emit(eng, fn, reads=[t.r for t in r], writes=[t.r for t in w], inc=inc)

        def DMA(q, out, in_, r=(), w=(), key=None):
            return sy.dma(q, out, in_, reads=[t.r for t in r], writes=[t.r for t in w], semkey=key)

        def mk(name, shape, dt, stack=es):
            return T(stack, nc, name, shape, dt)

        x = mk("x", [128, NT, D], F32)
        xr = [Res(f"x{t}") for t in range(NT)]

        class XT:
            def __init__(self, t):
                self.r = xr[t]
        xt = [XT(t) for t in range(NT)]
        prmg = mk("prmg", [128, NPG], F32)
        prml = mk("prml", [128, NPL], F32)
        cst = mk("cst", [128, 384], F32)
        ident_b = mk("ident_b", [128, 128], BF16)
        tri_b = mk("tri_b", [128, 128], BF16)
        modc = mk("modc", [128, 48], F32)
        a1 = mk("a1", [128, 8], F32)
        a2 = mk("a2", [128, 8], F32)
        g1bc = mk("g1bc", [128, D], F32)
        g2bc = mk("g2bc", [128, D], F32)
        cond_bf = mk("cond_bf", [128, 8], BF16)
        cond_f = mk("cond_f", [128, 8], F32)
        banks = [T(es, nc, f"bank{i}", [128, 512], F32, psum=True) for i in range(8)]
        bstate = {"i": 0}

        def bank():
            b = banks[bstate["i"] % 8]
            bstate["i"] += 1
            return b

        ident_f = cst[:, 0:128]
        tri_f = cst[:, 128:256]
        ones_f = cst[:, 256:384]

        def pl(name, a=None, b=None):
            lo, hi = PL[name]
            if a is None:
                return prml[:, lo:hi]
            return prml[:, lo + a:lo + b]

        def pg(name, a=None, b=None):
            lo, hi = PG[name]
            if a is None:
                return prmg[:, lo:hi]
            return prmg[:, lo + a:lo + b]

        xv = x_d.rearrange("(t p) d -> p t d", p=128)
        for t in range(NT):
            DMA("sp", x[:, t, :], xv[:, t, :], w=[xt[t]], key=f"x{t}")
        DMA("sp", prmg[:], prmg_d, w=[prmg], key="prmg")
        DMA("sp", cst[:], cst_d, w=[cst], key="cst")
        E("dve", lambda e: e.tensor_copy(ident_b[:], ident_f), r=[cst], w=[ident_b])
        E("dve", lambda e: e.tensor_copy(tri_b[:], tri_f), r=[cst], w=[tri_b])
        ones_b = mk("ones_b", [128, 128], BF16)
        E("dve", lambda e: e.tensor_copy(ones_b[:], ones_f), r=[cst], w=[ones_b])
        E("act", lambda e: e.activation(cond_f[:], pg("cT"), AF.Silu), r=[prmg], w=[cond_f])
        E("dve", lambda e: e.tensor_copy(cond_bf[:], cond_f[:]), r=[cond_f], w=[cond_bf])

        def mm_group(out_ap, pairs, r, w):
            n = len(pairs)
            for i, (lt, rh) in enumerate(pairs):
                E("pe", lambda e, lt=lt, rh=rh, i=i: e.matmul(out_ap, lt, rh, start=(i == 0), stop=(i == n - 1)),
                  r=r, w=w, inc=(i == n - 1))

        sm = {}

        def small(name, shape, dt=F32, stack=es):
            if name not in sm:
                sm[name] = mk(name, shape, dt, stack)
            return sm[name]

        def norm_gen(t, acol, shcol, dst, dcol, sc, router_dst=None, alloc=None, rel=None):
            junk, ssum, tmp1, tmp2, rstd, xn = sc
            junk = xn
            E("dve", lambda e: e.memset(ssum[:], 0.0), w=[ssum])
            E("act", lambda e: e.activation(junk[:], x[:, t, :], AF.Square, accum_out=ssum[:, 0:1]),
              r=[xt[t], ssum], w=[junk, ssum])
            yield
            E("dve", lambda e: e.tensor_scalar(tmp1[:], ssum[:], 1.0 / D, EPS, ALU.mult, ALU.add), r=[ssum], w=[tmp1])
            E("act", lambda e: e.sqrt(tmp2[:], tmp1[:]), r=[tmp1], w=[tmp2])
            yield
            E("dve", lambda e: e.reciprocal(rstd[:], tmp2[:]), r=[tmp2], w=[rstd])
            E("act", lambda e: e.mul(xn[:], x[:, t, :], rstd[:, 0:1]), r=[xt[t], rstd], w=[xn])
            yield
            for half in range(2):
                if alloc is None:
                    b = bank()
                else:
                    b = yield from alloc()
                for j in range(4):
                    kk = half * 4 + j
                    E("pe", lambda e, j=j, kk=kk, b=b: e.transpose(b[:, j * 128:(j + 1) * 128],
                                                                    xn[:, kk * 128:(kk + 1) * 128], ident_f),
                      r=[xn, cst], w=[b], inc=(j == 3))
                yield
                for j in range(4):
                    kk = half * 4 + j
                    if half == 0:
                        E("dve", lambda e, j=j, kk=kk, b=b: e.tensor_scalar(
                            dst[:, kk, dcol:dcol + 128], b[:, j * 128:(j + 1) * 128],
                            acol[:, kk:kk + 1], shcol[:, kk:kk + 1], ALU.mult, ALU.add), r=[b, modc, a1, a2], w=[dst])
                    else:
                        E("act", lambda e, j=j, kk=kk, b=b: e.activation(
                            dst[:, kk, dcol:dcol + 128], b[:, j * 128:(j + 1) * 128], AF.Identity,
                            bias=shcol[:, kk:kk + 1], scale=acol[:, kk:kk + 1]), r=[b, modc, a1, a2], w=[dst])
                if router_dst is not None:
                    E("act", lambda e, half=half, b=b: e.copy(router_dst[:, half * 512:(half + 1) * 512], b[:]),
                      r=[b], w=[router_dst])
                if rel is not None:
                    rel(b)
                yield

        def norm_tile(t, acol, shcol, dst, dcol, sc, router_dst=None):
            for _ in norm_gen(t, acol, shcol, dst, dcol, sc, router_dst):
                pass

        for l in range(nl):
            if dbg <= 1:
                break
            sy.barrier()
            DMA("sp", prml[:], prml_d[l], w=[prml], key="prml")
            with ExitStack() as ms:
                biasT = mk(f"biasT_{l}", [128, 2048], BF16, ms)
                DMA("pool", biasT[:], biasT_d, w=[biasT], key="biasT")
                ring = [mk(f"ring{i}_{l}", [128, 8, 512], BF16, ms) for i in range(3)]
                wbr = mk(f"wbr_{l}", [128, 2, 4, D], BF16, ms)
                wout = mk(f"wout_{l}", [128, 8, D], BF16, ms)
                hT2 = [mk(f"hT{i}_{l}", [128, 8, G], BF16, ms) for i in range(2)]
                qkpre = [mk(f"qkpre{i}_{l}", [128, 3 + G], F32, ms) for i in range(4)]
                qkT = mk(f"qkT_{l}", [128, 4, G], BF16, ms)
                qaT = None
                kaT = mk(f"kaT_{l}", [128, 2, 128 + G], BF16, ms)
                vaug = [mk(f"vaug{i}_{l}", [128, 2, 72], BF16, ms) for i in range(3)]
                vhalo = mk(f"vhalo_{l}", [128, 2, 72], BF16, ms)
                state_f = mk(f"state_f_{l}", [128, 2, 129], F32, ms)
                state_b = mk(f"state_b_{l}", [128, 2, 129], BF16, ms)
                hmT = None
                haT = None
                mT = None
                esink = mk(f"esink_{l}", [128, 8], F32, ms)
                nsc = (None, mk(f"ssum_{l}", [128, 1], F32, ms),
                       mk(f"tmp1_{l}", [128, 1], F32, ms), mk(f"tmp2_{l}", [128, 1], F32, ms),
                       mk(f"rstd_{l}", [128, 1], F32, ms), mk(f"xn_{l}", [128, D], F32, ms))
                gsb = [mk(f"gsb{i}_{l}", [128, 8], F32, ms) for i in range(TPG)]
                ef4 = [mk(f"ef4{i}_{l}", [128, 4], F32, ms) for i in range(TPG)]
                logf = [mk(f"logf{i}_{l}", [128, 4], F32, ms) for i in range(TPG)]
                lhi = [mk(f"lhi{i}_{l}", [128, 4], BF16, ms) for i in range(TPG)]
                llo = [mk(f"llo{i}_{l}", [128, 4], BF16, ms) for i in range(TPG)]
                w4 = [mk(f"w4{i}_{l}", [128, 4], F32, ms) for i in range(TPG)]
                sc4 = [mk(f"sc4{i}_{l}", [128, 4], F32, ms) for i in range(TPG)]
                eb4 = [mk(f"eb4{i}_{l}", [128, 4], F32, ms) for i in range(TPG)]
                vpr = [mk(f"vpr{i}_{l}", [128, 4, 136], BF16, ms) for i in range(TPG)]
                som = None
                ktok = [mk(f"ktok{i}_{l}", [128, 2, 2, 128], BF16, ms) for i in range(TPG)]
                ebsel = [mk(f"ebsel{i}_{l}", [128, 2], F32, ms) for i in range(TPG)]
                sd = None
                den4 = None
                r4 = None
                hm = None
                sqj = None
                ss4 = None
                rn4 = None
                hmo = None
                tmpS = None
                pT = None
                dn4 = None
                hao = None
                sg1 = mk(f"sg1_{l}", [128, G], F32, ms)
                acc = sg1
                for i in range(TPG):
                    E("dve", lambda e, i=i: e.memset(ktok[i][:], 0.0), w=[ktok[i]])
                sg2 = None

                for i in range(2):
                    DMA("pool", wbr[:, i, :, :], wbr_d[l, i].rearrange("(k p) n -> p k n", p=128), w=[wbr], key="wbr")
                for k in range(8):
                    DMA("pool", wout[:, k, :], wout_d[l, k * 128:(k + 1) * 128, :], w=[wout], key="wout")
                flag = pg("flag")
                E("act", lambda e: e.activation(esink[:], pl("sinks"), AF.Exp), r=[prml], w=[esink])
                for i in range(3):
                    E("dve", lambda e, i=i: e.memset(vaug[i][:], 1.0), w=[vaug[i]])
                ib_d = nc.dram_tensor(f"ib{l}", [128, 656], F32)
                ob_d = nc.dram_tensor(f"ob{l}", [256, 656], F32)
                ibv = ib_d.ap()
                obv = ob_d.ap()

                class RO:
                    def __init__(self, n):
                        self.r = Res(n)
                ib_parts = [RO(f"ibp{i}_{l}") for i in range(7)]
                obR = RO(f"ob_{l}")
                pieces = [(0, 512), (512, 512), (1024, 392), (1416, 512), (1928, 512),
                          (2440, 512), (2952, 512), (3464, 512), (3976, 512)]

                class _Stop(Exception):
                    pass

                def chk(k):
                    if dbg == k:
                        raise _Stop()
                CUR = {"free": list(range(8))}
                ada_es = ExitStack()
                aring = [mk(f"aring{i}_{l}", [128, 8, 512], BF16, ada_es) for i in range(3)]
                prmb = mk(f"prmb_{l}", [128, 2048], F32, ada_es)
                DMA("sp", prmb[:], prmb_d[l], w=[prmb], key="prmb")
                cond_bc = mk(f"cond_bc_{l}", [128, 8, 128], BF16, ada_es)
                E("dve", lambda e: e.tensor_copy(cond_bc[:], cond_f[:].unsqueeze(2).to_broadcast([128, 8, 128])),
                  r=[cond_f], w=[cond_bc])

                def ada_issue(j, l=l):
                    rb_ = aring[j % 3]
                    DMA("pool", rb_[:], wada_d[l, :, j * 512:(j + 1) * 512].rearrange("(k p) n -> p k n", p=128),
                        w=[rb_], key=f"aring{j % 3}")

                def ada_steps(js, l=l):
                    ada_issue(js[0])
                    for ji, j in enumerate(js):
                        if ji + 1 < len(js):
                            ada_issue(js[ji + 1])
                        yield
                        while not CUR["free"]:
                            yield
                        bi_ = CUR["free"].pop(0)
                        b = banks[bi_]
                        rb_ = aring[j % 3]
                        if j in (4, 5, 10, 11):
                            gb = g1bc if j < 6 else g2bc
                            boff_ = 0 if j < 6 else 1024
                            c0 = (j % 2) * 512 if j < 6 else (j - 10) * 512
                            mm_group(b[:], [(cond_bc[:, k, :], rb_[:, k, :]) for k in range(8)], r=[cond_bc, rb_], w=[b])
                            E("dve", lambda e, b=b, gb=gb, boff_=boff_, c0=c0: e.tensor_tensor(
                                gb[:, c0:c0 + 512], b[:], prmb[:, boff_ + c0:boff_ + c0 + 512], ALU.add), r=[b, prmb], w=[gb])
                        else:
                            for jt in range(4):
                                mm_group(b[:, jt:jt + 1],
                                         [(rb_[:, k, jt * 128:(jt + 1) * 128], cond_bf[:, k:k + 1]) for k in range(8)],
                                         r=[cond_bf, rb_], w=[b])
                            E("dve", lambda e, b=b, j=j: e.tensor_tensor(modc[:, 4 * j:4 * j + 4], b[:, 0:4],
                                                                         pl("bada", 4 * j, 4 * j + 4), ALU.add),
                              r=[b, prml], w=[modc])
                        CUR["free"].append(bi_)
                        yield

                for _ in ada_steps([0, 1, 2, 3]):
                    pass
                E("dve", lambda e: e.scalar_tensor_tensor(a1[:], modc[:, 8:16], 1.0, pl("wn1"), ALU.add, ALU.mult),
                  r=[modc, prml], w=[a1])

                def ada_rest():
                    yield from ada_steps([4, 5, 6, 7, 8, 9, 10, 11])
                    E("dve", lambda e: e.scalar_tensor_tensor(a2[:], modc[:, 32:40], 1.0, pl("wn2"), ALU.add, ALU.mult),
                      r=[modc, prml], w=[a2])
                background = [ada_rest()]
                sh1 = modc[:, 0:8]
                sh2 = modc[:, 24:32]


                p2s = ExitStack()
                for pss in (1, 2):
                    if pss == 1:
                        E("dve", lambda e: e.memset(state_f[:], 0.0), w=[state_f])
                        E("dve", lambda e: e.memset(state_b[:], 0.0), w=[state_b])
                        for i in range(4):
                            E("dve", lambda e, i=i: e.memset(qkpre[i][:, 0:3], 0.0), w=[qkpre[i]])
                        E("dve", lambda e: e.memset(kaT[:, :, 0:128], 0.0), w=[kaT])
                    else:
                        for gb_ in background:
                            for _ in gb_:
                                pass
                        background = []
                        sy.barrier(skip=(f"cc{l}",))
                        ada_es.close()
                        qaT = mk(f"qaT_{l}", [128, 4, G], BF16, p2s)
                        hmT = mk(f"hmT_{l}", [128, 4, G], BF16, p2s)
                        haT = mk(f"haT_{l}", [128, 4, G], BF16, p2s)
                        mT = mk(f"mT_{l}", [128, 8, G], BF16, p2s)
                        som = [mk(f"som{i}_{l}", [128, 512], BF16, p2s) for i in range(TPG)]
                        sd = [mk(f"sd{i}_{l}", [128, 4, 128], BF16, p2s) for i in range(TPG)]
                        den4 = [mk(f"den4{i}_{l}", [128, 4], F32, p2s) for i in range(TPG)]
                        r4 = [mk(f"r4{i}_{l}", [128, 4], F32, p2s) for i in range(TPG)]
                        hm = [mk(f"hm{i}_{l}", [128, 4, 128], F32, p2s) for i in range(TPG)]
                        sqj = [mk(f"sqj{i}_{l}", [128, 128], F32, p2s) for i in range(TPG)]
                        ss4 = [mk(f"ss4{i}_{l}", [128, 4], F32, p2s) for i in range(TPG)]
                        rn4 = [mk(f"rn4{i}_{l}", [128, 4], F32, p2s) for i in range(TPG)]
                        hmo = [mk(f"hmo{i}_{l}", [128, 512], BF16, p2s) for i in range(TPG)]
                        tmpS = [mk(f"tmpS{i}_{l}", [128, 2, 512], F32, p2s) for i in range(TPG)]
                        pT = [mk(f"pT{i}_{l}", [128, 2, 512], BF16, p2s) for i in range(TPG)]
                        dn4 = [mk(f"dn4{i}_{l}", [128, 4], F32, p2s) for i in range(TPG)]
                        hao = [mk(f"hao{i}_{l}", [128, 512], BF16, p2s) for i in range(TPG)]
                        sg2 = mk(f"sg2_{l}", [128, G], F32, p2s)
                        for k in range(8):
                            E("dve", lambda e, k=k: e.tensor_tensor(wout[:, k, :], wout[:, k, :], g1bc[:], ALU.mult),
                              r=[wout, g1bc], w=[wout])
                        DMA("sp", state_f[:], obv[0:128, 0:258].rearrange("p (a b) -> p a b", a=2), r=[obR], w=[state_f], key="h_st")
                        E("dve", lambda e: e.tensor_scalar(state_f[:], state_f[:], flag[:, 0:1], None, ALU.mult),
                          r=[state_f, prmg], w=[state_f])
                        E("act", lambda e: e.copy(state_b[:], state_f[:]), r=[state_f], w=[state_b])
                        for i in range(4):
                            DMA("sp", qkpre[i][:, 0:3], obv[0:128, 258 + 3 * i:261 + 3 * i], r=[obR], w=[qkpre[i]], key=f"h_cv{i}")
                            E("dve", lambda e, i=i: e.tensor_scalar(qkpre[i][:, 0:3], qkpre[i][:, 0:3], flag[:, 0:1], None,
                                                                      ALU.mult), r=[qkpre[i], prmg], w=[qkpre[i]])
                        DMA("pool", kaT[:, :, 0:128], obv[0:128, 270:526].rearrange("p (a b) -> p a b", a=2), r=[obR], w=[kaT], key="h_ka")
                        DMA("pool", vhalo[:, :, 0:65], obv[0:128, 526:656].rearrange("p (a b) -> p a b", a=2), r=[obR], w=[vhalo], key="h_va")
                        E("dve", lambda e: e.tensor_scalar(vhalo[:, :, 0:65], vhalo[:, :, 0:65], flag[:, 0:1], None, ALU.mult),
                          r=[vhalo, prmg], w=[vhalo])
                    pcs = (0, 2, 3) if pss == 1 else tuple(range(9))
                    seq = [(g, p) for g in range(NG) for p in pcs]
                    rstate = {"issued": 0}

                    def ring_issue(seq=seq, rstate=rstate):
                        i = rstate["issued"]
                        if i >= len(seq):
                            return
                        _, p = seq[i]
                        c0, wd_ = pieces[p]
                        rb_ = ring[i % 3]
                        DMA("pool", rb_[:, :, 0:wd_], win_d[l, :, c0:c0 + wd_].rearrange("(k p) n -> p k n", p=128),
                            w=[rb_], key=f"ring{i % 3}")
                        rstate["issued"] += 1

                    ring_issue()
                    ring_issue()
                    uidx = {"i": 0}

                    def ring_get(uidx=uidx):
                        i = uidx["i"]
                        uidx["i"] += 1
                        return ring[i % 3]

                    try:
                        for g in range(NG):
                            if dbg <= 3 or (dbg <= 30 and g >= 1):
                                break
                            hT = hT2[g % 2]
                            if g == 0 and (pss == 1 or os.environ.get("KSEQ") is not None):
                                for tt in range(TPG):
                                    norm_tile(g * TPG + tt, a1, sh1, hT, tt * 128, nsc)
                            chk(11)
                            rb_ = ring_get()
                            for mt in range(4):
                                if pss == 1 and mt < 2 and g < NG - 1:
                                    continue
                                b = bank()
                                mm_group(b[:, 0:G], [(rb_[:, k, mt * 128:(mt + 1) * 128], hT[:, k, :]) for k in range(8)],
                                         r=[rb_, hT], w=[b])
                                E("act", lambda e, b=b, mt=mt: e.copy(qkpre[mt][:, 3:3 + G], b[:, 0:G]), r=[b], w=[qkpre[mt]])
                                if pss == 2 or mt >= 2:
                                    cw = lambda j, mt=mt: pl("convw", mt * 4 + j, mt * 4 + j + 1)
                                    E("dve", lambda e, mt=mt, cw=cw: e.tensor_scalar(acc[:], qkpre[mt][:, 0:G], cw(0),
                                                                                       pl("convb", mt, mt + 1), ALU.mult, ALU.add),
                                      r=[qkpre[mt], prml], w=[acc])
                                    for j in range(1, 4):
                                        E("dve", lambda e, mt=mt, j=j, cw=cw: e.scalar_tensor_tensor(
                                            acc[:], qkpre[mt][:, j:j + G], cw(j), acc[:], ALU.mult, ALU.add),
                                          r=[qkpre[mt], prml, acc], w=[acc])
                                    E("act", lambda e, mt=mt: e.activation(qkT[:, mt, :], acc[:], AF.Silu), r=[acc], w=[qkT])
                                E("act", lambda e, mt=mt: e.copy(qkpre[mt][:, 0:3], qkpre[mt][:, G:G + 3]),
                                  r=[qkpre[mt]], w=[qkpre[mt]])
                            ring_issue()
                            chk(12)
                            if pss == 2:
                                rb_ = ring_get()
                                for mt in range(4):
                                    b = bank()
                                    mm_group(b[:, 0:G], [(rb_[:, k, mt * 128:(mt + 1) * 128], hT[:, k, :]) for k in range(8)],
                                             r=[rb_, hT], w=[b])
                                    E("act", lambda e, b=b, mt=mt: e.copy(qaT[:, mt, :], b[:, 0:G]), r=[b], w=[qaT])
                                ring_issue()
                            chk(13)
                            rb_ = ring_get()
                            for j in range(2):
                                b = bank()
                                mm_group(b[:, 0:G], [(rb_[:, k, j * 128:(j + 1) * 128], hT[:, k, :]) for k in range(8)],
                                         r=[rb_, hT], w=[b])
                                E("act", lambda e, b=b, j=j: e.copy(kaT[:, j, 128:128 + G], b[:, 0:G]), r=[b], w=[kaT])
                            chk(131)
                            def gate_gen(tt, g=g):
                                t = g * TPG + tt
                                va = vaug[t % 3]
                                b = bank()
                                yield
                                mm_group(b[:, 0:136], [(hT[:, k, tt * 128:(tt + 1) * 128], rb_[:, k, 256:392]) for k in range(8)],
                                         r=[rb_, hT], w=[b])
                                yield
                                E("dve", lambda e, b=b, tt=tt: e.tensor_tensor(gsb[tt][:], b[:, 0:8], pl("bif"), ALU.add),
                                  r=[b, prml], w=[gsb[tt]])
                                yield
                                E("act", lambda e, b=b, va=va: e.copy(va[:, :, 0:64],
                                                                        b[:, 8:136].rearrange("p (a b) -> p a b", a=2)),
                                  r=[b], w=[va])
                                yield
                                E("act", lambda e, tt=tt: e.activation(ef4[tt][:], gsb[tt][:, 4:8], AF.Exp, scale=-1.0),
                                  r=[gsb[tt]], w=[ef4[tt]])
                                yield
                                E("act", lambda e, tt=tt: e.activation(ef4[tt][:], ef4[tt][:], AF.Ln, bias=1.0),
                                  r=[ef4[tt]], w=[ef4[tt]])
                                yield
                                E("dve", lambda e, tt=tt: e.tensor_scalar(logf[tt][:], ef4[tt][:], -1.0, None, ALU.mult),
                                  r=[ef4[tt]], w=[logf[tt]])
                                yield
                                b2 = bank()
                                yield
                                E("dve", lambda e, tt=tt: e.tensor_copy(lhi[tt][:], logf[tt][:]), r=[logf[tt]], w=[lhi[tt]])
                                yield
                                E("dve", lambda e, tt=tt: e.tensor_tensor(llo[tt][:], logf[tt][:], lhi[tt][:], ALU.subtract),
                                  r=[logf[tt], lhi[tt]], w=[llo[tt]])
                                yield
                                mm_group(b2[:, 0:4], [(tri_b[:], lhi[tt][:]), (tri_b[:], llo[tt][:])],
                                         r=[tri_b, lhi[tt], llo[tt]], w=[b2])
                                yield
                                mm_group(b2[:, 4:8], [(ones_b[:], lhi[tt][:]), (ones_b[:], llo[tt][:])],
                                         r=[ones_b, lhi[tt], llo[tt]], w=[b2])
                                yield
                                E("dve", lambda e, b2=b2, tt=tt: e.tensor_tensor(w4[tt][:], b2[:, 0:4], gsb[tt][:, 0:4],
                                                                                   ALU.subtract), r=[gsb[tt], b2], w=[w4[tt]])
                                yield
                                E("act", lambda e, tt=tt: e.activation(w4[tt][:], w4[tt][:], AF.Exp, scale=-1.0), r=[w4[tt]], w=[w4[tt]])
                                yield
                                E("act", lambda e, b2=b2, tt=tt: e.activation(sc4[tt][:], b2[:, 0:4], AF.Exp),
                                  r=[b2], w=[sc4[tt]])
                                yield
                                E("act", lambda e, b2=b2, tt=tt: e.activation(eb4[tt][:], b2[:, 4:8], AF.Exp),
                                  r=[b2], w=[eb4[tt]])
                                eb2 = eb4[tt][:, 0:4].rearrange("p (j r) -> p j r", r=2)
                                yield
                                E("act", lambda e, tt=tt, eb2=eb2: e.copy(ebsel[tt][0:64, :], eb2[0:64, :, 0]),
                                  r=[eb4[tt]], w=[ebsel[tt]])
                                E("act", lambda e, tt=tt, eb2=eb2: e.copy(ebsel[tt][64:128, :], eb2[64:128, :, 1]),
                                  r=[eb4[tt]], w=[ebsel[tt]])

                            ggs = [gate_gen(tt) for tt in range(TPG)]
                            while ggs:
                                for gg_ in list(ggs):
                                    try:
                                        next(gg_)
                                    except StopIteration:
                                        ggs.remove(gg_)
                            chk(138)
                            ring_issue()
                            chk(14)
                            rb_ = ring_get()
                            for tt in range(TPG):
                                b = bank()
                                mm_group(b[:], [(hT[:, k, tt * 128:(tt + 1) * 128], rb_[:, k, :]) for k in range(8)],
                                         r=[rb_, hT], w=[b])
                                E("dve", lambda e, b=b, tt=tt: e.tensor_tensor(
                                    vpr[tt][:, :, 0:128], b[:].rearrange("p (a b) -> p a b", a=4),
                                    w4[tt][:].unsqueeze(2).to_broadcast([128, 4, 128]), ALU.mult), r=[b, w4[tt]], w=[vpr[tt]])
                                E("dve", lambda e, tt=tt: e.tensor_copy(vpr[tt][:, :, 128:129], w4[tt][:].unsqueeze(2)),
                                  r=[w4[tt]], w=[vpr[tt]])
                            ring_issue()
                            chk(15)
                            if pss == 2:
                                rb_ = ring_get()
                                for tt in range(TPG):
                                    b = bank()
                                    mm_group(b[:], [(hT[:, k, tt * 128:(tt + 1) * 128], rb_[:, k, :]) for k in range(8)],
                                             r=[rb_, hT], w=[b])
                                    E("act", lambda e, b=b, tt=tt: e.activation(som[tt][:], b[:], AF.Sigmoid), r=[b], w=[som[tt]])
                                    E("dve", lambda e, tt=tt: e.tensor_tensor(som[tt][:], som[tt][:], pl("wmn"), ALU.mult),
                                      r=[som[tt], prml], w=[som[tt]])
                                ring_issue()
                            chk(16)
                            free_banks = list(range(8))
                            CUR["free"] = free_banks

                            def galloc():
                                while not free_banks:
                                    yield
                                return banks[free_banks.pop(0)]

                            def grel(b_):
                                free_banks.append(banks.index(b_))

                            def mlstm_gen(tt, g=g):
                                t = g * TPG + tt
                                cs = slice(tt * 128, (tt + 1) * 128)
                                ktok_ = ktok[tt]
                                if pss == 2:
                                    sd_, den4_, r4_, hm_, ss4_, rn4_, hmo_, sqj_ = (
                                        sd[tt], den4[tt], r4[tt], hm[tt], ss4[tt], rn4[tt], hmo[tt], sqj[tt])
                                b = yield from galloc()
                                bb = b[:].bitcast(BF16)
                                for j in range(2):
                                    E("pe", lambda e, j=j, bb=bb: e.transpose(bb[:, j * 128:(j + 1) * 128], qkT[:, 2 + j, cs],
                                                                               ident_b[:]),
                                      r=[qkT, ident_b], w=[b], inc=(j == 1))
                                bb3 = bb[:, 0:256].rearrange("p (j c) -> p j c", j=2)
                                for w_ in range(2):
                                    E("act", lambda e, w_=w_: e.copy(ktok_[:, :, w_, w_ * 64:(w_ + 1) * 64],
                                                                     bb3[:, :, w_ * 64:(w_ + 1) * 64]), r=[b], w=[ktok_])
                                grel(b)
                                if pss == 2:
                                    bsr = [(yield from galloc()), (yield from galloc())]
                                    for h in range(4):
                                        p0 = 64 * (h % 2)
                                        rg, jj = h % 2, h // 2
                                        E("pe", lambda e, h=h, p0=p0, rg=rg, jj=jj: e.matmul(
                                            bsr[rg][:, jj * 128:(jj + 1) * 128], qkT[p0:p0 + 64, 2 + h // 2, cs],
                                            qkT[p0:p0 + 64, h // 2, cs], start=True, stop=True), r=[qkT], w=[bsr[rg]], inc=True)
                                    sd4 = sd_[:].rearrange("p (j r) l -> p j r l", r=2)
                                    for rg in range(2):
                                        E("dve", lambda e, rg=rg: e.tensor_tensor(
                                            sd4[:, :, rg, :], bsr[rg][:, 0:256].rearrange("p (a b) -> p a b", a=2),
                                            tri_f.unsqueeze(1).to_broadcast([128, 2, 128]), ALU.mult), r=[bsr[rg], cst], w=[sd_])
                                    grel(bsr[0])
                                    grel(bsr[1])
                                    bn = [(yield from galloc()), (yield from galloc())]
                                    for h in range(4):
                                        p0 = 64 * (h % 2)
                                        o = bn[h // 2][:, (h % 2) * 256:(h % 2) * 256 + 129]
                                        E("pe", lambda e, h=h, o=o: e.matmul(o, sd_[:, h, :], vpr[tt][:, h, 0:129], start=True,
                                                                             stop=False),
                                          r=[sd_, vpr[tt]], w=[bn[h // 2]], inc=False)
                                        E("pe", lambda e, h=h, o=o, p0=p0: e.matmul(o, qkT[p0:p0 + 64, h // 2, cs],
                                                                                    state_b[p0:p0 + 64, h // 2, :],
                                                                                    start=False, stop=True),
                                          r=[qkT, state_b], w=[bn[h // 2]], inc=True)
                                bd = yield from galloc()
                                for j in range(2):
                                    o = bd[:, j * 256:j * 256 + 129]
                                    E("pe", lambda e, j=j, o=o: e.matmul(o, ktok_[:, j, 0, :], vpr[tt][:, 2 * j, 0:129],
                                                                         start=True, stop=False),
                                      r=[ktok_, vpr[tt]], w=[bd], inc=False)
                                    E("pe", lambda e, j=j, o=o: e.matmul(o, ktok_[:, j, 1, :], vpr[tt][:, 2 * j + 1, 0:129],
                                                                         start=False, stop=True),
                                      r=[ktok_, vpr[tt]], w=[bd], inc=True)
                                E("dve", lambda e: e.tensor_tensor(
                                    state_f[:], bd[:, 0:512].rearrange("p (j c) -> p j c", j=2)[:, :, 0:129], state_f[:],
                                    ALU.add), r=[state_f, bd], w=[state_f])
                                E("dve", lambda e: e.tensor_tensor(
                                    state_f[:], state_f[:], ebsel[tt][:].unsqueeze(2).to_broadcast([128, 2, 129]), ALU.mult),
                                  r=[state_f, ebsel[tt]], w=[state_f])
                                E("act", lambda e: e.copy(state_b[:], state_f[:]), r=[state_f], w=[state_b])
                                grel(bd)
                                yield "state_done"
                                if pss != 2:
                                    return
                                for j in range(2):
                                    v3 = bn[j][:].rearrange("p (a b) -> p a b", a=2)
                                    E("dve", lambda e, j=j, v3=v3: e.scalar_tensor_tensor(
                                        den4_[:, 2 * j:2 * j + 2], v3[:, :, 128], 0.125, sc4[tt][:, 2 * j:2 * j + 2],
                                        ALU.mult, ALU.mult), r=[bn[j], sc4[tt]], w=[den4_])
                                E("act", lambda e: e.activation(den4_[:], den4_[:], AF.Abs), r=[den4_], w=[den4_])
                                yield
                                E("dve", lambda e: e.tensor_scalar(den4_[:], den4_[:], 1.0, None, ALU.max), r=[den4_], w=[den4_])
                                E("dve", lambda e: e.reciprocal(den4_[:], den4_[:]), r=[den4_], w=[den4_])
                                E("dve", lambda e: e.scalar_tensor_tensor(r4_[:], sc4[tt][:], 0.125, den4_[:], ALU.mult, ALU.mult),
                                  r=[sc4[tt], den4_], w=[r4_])
                                yield
                                for j in range(2):
                                    v3 = bn[j][:].rearrange("p (a b) -> p a b", a=2)
                                    E("dve", lambda e, j=j, v3=v3: e.tensor_tensor(
                                        hm_[:, 2 * j:2 * j + 2, :], v3[:, :, 0:128],
                                        r4_[:, 2 * j:2 * j + 2].unsqueeze(2).to_broadcast([128, 2, 128]), ALU.mult),
                                      r=[bn[j], r4_], w=[hm_])
                                E("dve", lambda e: e.memset(ss4_[:], 0.0), w=[ss4_])
                                grel(bn[0])
                                grel(bn[1])
                                yield
                                for h in range(4):
                                    E("act", lambda e, h=h: e.activation(sqj_[:], hm_[:, h, :], AF.Square,
                                                                         accum_out=ss4_[:, h:h + 1]),
                                      r=[hm_, ss4_], w=[sqj_, ss4_])
                                yield
                                E("dve", lambda e: e.tensor_scalar(ss4_[:], ss4_[:], 1.0 / 128, EPS, ALU.mult, ALU.add),
                                  r=[ss4_], w=[ss4_])
                                E("act", lambda e: e.sqrt(ss4_[:], ss4_[:]), r=[ss4_], w=[ss4_])
                                yield
                                E("dve", lambda e: e.reciprocal(rn4_[:], ss4_[:]), r=[ss4_], w=[rn4_])
                                E("dve", lambda e: e.tensor_tensor(hm_[:], hm_[:], rn4_[:].unsqueeze(2).to_broadcast([128, 4, 128]),
                                                                   ALU.mult), r=[hm_, rn4_], w=[hm_])
                                yield
                                hm2 = hm_[:].rearrange("p a b -> p (a b)")
                                E("dve", lambda e: e.tensor_tensor(hmo_[:], hm2, som[tt][:], ALU.mult), r=[hm_, som[tt]], w=[hmo_])
                                yield
                                b = yield from galloc()
                                bb = b[:].bitcast(BF16)
                                for f in range(4):
                                    E("pe", lambda e, f=f, bb=bb: e.transpose(bb[:, f * 128:(f + 1) * 128],
                                                                               hmo_[:, f * 128:(f + 1) * 128], ident_b[:]),
                                      r=[hmo_, ident_b], w=[b], inc=(f == 3))
                                E("act", lambda e, bb=bb: e.copy(hmT[:, :, cs], bb[:, 0:512].rearrange("p (a b) -> p a b", a=4)),
                                  r=[b], w=[hmT])
                                grel(b)

                            def swa_gen(tt, g=g):
                                t = g * TPG + tt
                                cs = slice(tt * 128, (tt + 1) * 128)
                                tmpS_, pT_, dn4_, hao_ = tmpS[tt], pT[tt], dn4[tt], hao[tt]
                                vprev = vaug[(t - 1) % 3] if t > 0 else vhalo
                                vcur = vaug[t % 3]
                                pc = slice(tt * 128, tt * 128 + 128)
                                cc = slice(128 + tt * 128, 256 + tt * 128)
                                for kv in range(2):
                                    bS = [(yield from galloc()), (yield from galloc())]
                                    for rg in range(2):
                                        for bi, ks in enumerate((pc, cc)):
                                            for gg in range(2):
                                                g4 = gg * 2 + rg
                                                h = kv * 4 + g4
                                                p0 = 64 * rg
                                                sl_ = (bi * 2 + gg) * 128
                                                E("pe", lambda e, rg=rg, ks=ks, sl_=sl_, h=h, p0=p0: e.matmul(
                                                    bS[rg][:, sl_:sl_ + 128], kaT[p0:p0 + 64, kv, ks],
                                                    qaT[p0:p0 + 64, h // 2, cs], start=True, stop=True),
                                                  r=[kaT, qaT], w=[bS[rg]], inc=True)
                                        boff = (kv * 2 + rg) * 512
                                        E("dve", lambda e, rg=rg, boff=boff: e.scalar_tensor_tensor(
                                            tmpS_[:, rg, :], bS[rg][:], 0.125, biasT[:, boff:boff + 512], ALU.mult, ALU.add),
                                          r=[bS[rg], biasT], w=[tmpS_])
                                    grel(bS[0])
                                    grel(bS[1])
                                    yield
                                    E("act", lambda e: e.activation(pT_[:], tmpS_[:], AF.Exp), r=[tmpS_], w=[pT_])
                                    yield
                                    bp = yield from galloc()
                                    for g4 in range(4):
                                        o = bp[:, g4 * 65:(g4 + 1) * 65]
                                        rg, gg = g4 % 2, g4 // 2
                                        E("pe", lambda e, rg=rg, gg=gg, o=o: e.matmul(
                                            o, pT_[:, rg, gg * 128:(gg + 1) * 128], vprev[:, kv, 0:65], start=True, stop=False),
                                          r=[pT_, vprev], w=[bp], inc=False)
                                        E("pe", lambda e, rg=rg, gg=gg, o=o: e.matmul(
                                            o, pT_[:, rg, (2 + gg) * 128:(3 + gg) * 128], vcur[:, kv, 0:65], start=False,
                                            stop=True), r=[pT_, vcur], w=[bp], inc=True)
                                    yield
                                    pv3 = bp[:, 0:260].rearrange("p (a b) -> p a b", a=4)
                                    E("dve", lambda e, pv3=pv3: e.tensor_tensor(dn4_[:], pv3[:, :, 64],
                                                                                esink[:, kv * 4:(kv + 1) * 4], ALU.add),
                                      r=[bp, esink], w=[dn4_])
                                    E("dve", lambda e: e.reciprocal(dn4_[:], dn4_[:]), r=[dn4_], w=[dn4_])
                                    E("dve", lambda e, pv3=pv3: e.tensor_tensor(
                                        hao_[:, kv * 256:(kv + 1) * 256].rearrange("p (a b) -> p a b", a=4), pv3[:, :, 0:64],
                                        dn4_[:].unsqueeze(2).to_broadcast([128, 4, 64]), ALU.mult), r=[bp, dn4_], w=[hao_])
                                    grel(bp)
                                    yield
                                b = yield from galloc()
                                bb = b[:].bitcast(BF16)
                                for f in range(4):
                                    E("pe", lambda e, f=f, bb=bb: e.transpose(bb[:, f * 128:(f + 1) * 128],
                                                                               hao_[:, f * 128:(f + 1) * 128], ident_b[:]),
                                      r=[hao_, ident_b], w=[b], inc=(f == 3))
                                E("act", lambda e, bb=bb: e.copy(haT[:, :, cs], bb[:, 0:512].rearrange("p (a b) -> p a b", a=4)),
                                  r=[b], w=[haT])
                                grel(b)

                            pending_m = [mlstm_gen(tt) for tt in range(TPG)]
                            if os.environ.get("KSEQ") == "1":
                                for gm in pending_m:
                                    for _ in gm:
                                        pass
                                pending_m = []
                                if pss == 2:
                                    for tt in range(TPG):
                                        for _ in swa_gen(tt):
                                            pass
                                pss_swa = False
                            elif os.environ.get("KSEQ") == "2":
                                pss_swa = False
                            else:
                                pss_swa = (pss == 2)
                            chains = []
                            if pss_swa:
                                chains += [swa_gen(tt) for tt in range(TPG)]
                            if g + 1 < NG and os.environ.get("KSEQ") is None:
                                def nnext_gen(g=g):
                                    for tt in range(TPG):
                                        yield from norm_gen((g + 1) * TPG + tt, a1, sh1, hT2[(g + 1) % 2], tt * 128, nsc,
                                                            alloc=galloc, rel=grel)
                                chains.append(nnext_gen())
                            elif pss == 1 and g + 1 == NG and os.environ.get("KSEQ") is None:
                                def nfirst_gen():
                                    for tt in range(TPG):
                                        yield from norm_gen(tt, a1, sh1, hT2[0], tt * 128, nsc, alloc=galloc, rel=grel)
                                chains.append(nfirst_gen())
                            elif g + 1 < NG:
                                for tt in range(TPG):
                                    norm_tile((g + 1) * TPG + tt, a1, sh1, hT2[(g + 1) % 2], tt * 128, nsc)
                            started = 0
                            mchains = []
                            next_ok = True
                            while pending_m or chains or mchains:
                                if pending_m and next_ok:
                                    gm = pending_m.pop(0)
                                    mchains.append(gm)
                                    next_ok = False
                                    fresh = gm
                                else:
                                    fresh = None
                                for gm in list(mchains):
                                    try:
                                        v = next(gm)
                                        if v == "state_done":
                                            next_ok = True
                                    except StopIteration:
                                        mchains.remove(gm)
                                        if gm is fresh or True:
                                            pass
                                for gs in list(chains):
                                    try:
                                        next(gs)
                                    except StopIteration:
                                        chains.remove(gs)
                                for gb_ in list(background):
                                    try:
                                        next(gb_)
                                    except StopIteration:
                                        background.remove(gb_)
                            if os.environ.get("KSEQ") == "2" and pss == 2:
                                for tt in range(TPG):
                                    for _ in swa_gen(tt):
                                        pass
                            chk(23)
                            E("act", lambda e: e.copy(kaT[:, :, 0:128], kaT[:, :, G:G + 128]), r=[kaT], w=[kaT])
                            chk(24)
                            if pss == 2:
                                for pi in range(4):
                                    rb_ = ring_get()
                                    for oi in range(2):
                                        ot = 2 * pi + oi
                                        bgm, bga, bbm, bba = bank(), bank(), bank(), bank()
                                        mm_group(bgm[:, 0:G], [(rb_[:, k, (2 * oi) * 128:(2 * oi + 1) * 128], hT[:, k, :])
                                                               for k in range(8)], r=[rb_, hT], w=[bgm])
                                        mm_group(bga[:, 0:G], [(rb_[:, k, (2 * oi + 1) * 128:(2 * oi + 2) * 128], hT[:, k, :])
                                                               for k in range(8)], r=[rb_, hT], w=[bga])
                                        mm_group(bbm[:, 0:G], [(wbr[:, 0, k, ot * 128:(ot + 1) * 128], hmT[:, k, :]) for k in range(4)],
                                                 r=[wbr, hmT], w=[bbm])
                                        mm_group(bba[:, 0:G], [(wbr[:, 1, k, ot * 128:(ot + 1) * 128], haT[:, k, :]) for k in range(4)],
                                                 r=[wbr, haT], w=[bba])
                                        E("act", lambda e, bgm=bgm: e.activation(sg1[:], bgm[:, 0:G], AF.Sigmoid), r=[bgm], w=[sg1])
                                        E("act", lambda e, bga=bga: e.activation(sg2[:], bga[:, 0:G], AF.Sigmoid), r=[bga], w=[sg2])
                                        E("dve", lambda e, bbm=bbm: e.tensor_tensor(sg1[:], bbm[:, 0:G], sg1[:], ALU.mult),
                                          r=[sg1, bbm], w=[sg1])
                                        E("dve", lambda e, bba=bba: e.tensor_tensor(sg2[:], bba[:, 0:G], sg2[:], ALU.mult),
                                          r=[sg2, bba], w=[sg2])
                                        E("dve", lambda e, ot=ot: e.tensor_tensor(mT[:, ot, :], sg1[:], sg2[:], ALU.add),
                                          r=[sg1, sg2], w=[mT])
                                    ring_issue()
                            chk(25)
                            for tt in (range(TPG) if pss == 2 else ()):
                                t = g * TPG + tt
                                for oc in range(2):
                                    b = bank()
                                    mm_group(b[:], [(mT[:, k, tt * 128:(tt + 1) * 128], wout[:, k, oc * 512:(oc + 1) * 512])
                                                    for k in range(8)], r=[mT, wout], w=[b])
                                    E("dve", lambda e, b=b, t=t, oc=oc: e.tensor_tensor(
                                        x[:, t, oc * 512:(oc + 1) * 512], b[:], x[:, t, oc * 512:(oc + 1) * 512], ALU.add),
                                      r=[b, xt[t]], w=[xt[t]])
                    except _Stop:
                        pass
                    if pss == 1:
                        lastc = slice(128 + (TPG - 1) * 128, 256 + (TPG - 1) * 128)
                        vlast = vaug[(NT - 1) % 3]
                        DMA("sp", ibv[:, 0:258].rearrange("p (a b) -> p a b", a=2), state_f[:], r=[state_f], w=[ib_parts[0]], key="x_st")
                        for i in range(4):
                            DMA("sp", ibv[:, 258 + 3 * i:261 + 3 * i], qkpre[i][:, 0:3], r=[qkpre[i]], w=[ib_parts[1 + i]], key=f"x_cv{i}")
                        DMA("pool", ibv[:, 270:526].rearrange("p (a b) -> p a b", a=2), kaT[:, :, 0:128], r=[kaT], w=[ib_parts[5]], key="x_ka")
                        DMA("pool", ibv[:, 526:656].rearrange("p (a b) -> p a b", a=2), vlast[:, :, 0:65], r=[vlast], w=[ib_parts[6]], key="x_va")
                        sy.emit_cc(lambda e: e.collective_compute("AllGather", ALU.bypass,
                                                                  replica_groups=[[2 * i, 2 * i + 1] for i in range(NCORES // 2)],
                                                                  ins=[ibv.opt()], outs=[obv.opt()]),
                                   reads=[t.r for t in ib_parts], writes=[obR.r], key=f"cc{l}")
                sy.barrier()
                p2s.close()

            if not moe:
                continue
            with ExitStack() as ms:
                h2T = mk(f"h2T_{l}", [128, 8, NTOK], BF16, ms)
                gates = mk(f"gates_{l}", [128, NT, NE], F32, ms)
                xnT2 = [mk(f"xnT{i}_{l}", [128, D], F32, ms) for i in range(2)]
                xnT = xnT2[0]
                wr2 = mk(f"wr2_{l}", [128, 8, NE], F32, ms)
                cbc = mk(f"cbc_{l}", [128, NE], F32, ms)
                nsc2 = [(None, mk(f"ssumb{i}_{l}", [128, 1], F32, ms),
                         mk(f"tmp1b{i}_{l}", [128, 1], F32, ms), mk(f"tmp2b{i}_{l}", [128, 1], F32, ms),
                         mk(f"rstdb{i}_{l}", [128, 1], F32, ms), mk(f"xnb{i}_{l}", [128, D], F32, ms)) for i in range(2)]
                wgb = [mk(f"wgb{i}_{l}", [128, 8, DE], BF16, ms) for i in range(2)]
                wub = [mk(f"wub{i}_{l}", [128, 8, DE], BF16, ms) for i in range(2)]
                wdb = [mk(f"wdb{i}_{l}", [128, 4, D], BF16, ms) for i in range(2)]
                actT = [mk(f"actT{i}_{l}", [128, 4, 512], BF16, ms) for i in range(2)]
                sil = [mk(f"sil{i}_{l}", [128, 512], F32, ms) for i in range(2)]
                R16s = [{n: mk(f"r_{n}{i}_{l}", [128, 16], F32, ms) for n in
                         ["sc", "sel", "selm", "m1", "selm2", "m2", "wts"]} for i in range(2)]
                R4s = [{n: mk(f"q_{n}{i}_{l}", [128, 4], F32, ms) for n in
                        ["hi1", "lo1", "hi2", "lo2", "top1", "mm2", "mm3", "grp", "gmask"]} for i in range(2)]
                R1s = [{n: mk(f"s_{n}{i}_{l}", [128, 1], F32, ms) for n in ["gmax", "t1", "t2", "den"]} for i in range(2)]

                def load_expert(e_):
                    i = e_ % 2
                    DMA("pool", wgb[i][:], wg_d[l, e_].rearrange("(k p) n -> p k n", p=128), w=[wgb[i]], key=f"wg{i}")
                    DMA("pool", wub[i][:], wu_d[l, e_].rearrange("(k p) n -> p k n", p=128), w=[wub[i]], key=f"wu{i}")
                    DMA("pool", wdb[i][:], wd_d[l, e_].rearrange("(k p) n -> p k n", p=128), w=[wdb[i]], key=f"wd{i}")

                load_expert(0)
                load_expert(1)
                E("dve", lambda e: e.tensor_tensor(wr2[:], pg("wr").rearrange("p (a b) -> p a b", a=8),
                                                   a2[:].unsqueeze(2).to_broadcast([128, 8, NE]), ALU.mult),
                  r=[prmg, a2], w=[wr2])
                sh2bc = sil[0]
                sh2bc = xnT
                sh2v = sh2bc[:].rearrange("p (a b) -> p a b", a=8)
                E("dve", lambda e: e.tensor_copy(sh2v, sh2.unsqueeze(2).to_broadcast([128, 8, 128])),
                  r=[modc], w=[sh2bc])
                b = bank()
                mm_group(b[:, 0:NE], [(sh2v[:, k, :], pg("wr", k * NE, (k + 1) * NE)) for k in range(8)],
                         r=[sh2bc, prmg], w=[b])
                E("dve", lambda e, b=b: e.tensor_copy(cbc[:], b[:, 0:NE]), r=[b], w=[cbc])

                mfree = list(range(8))

                def malloc_():
                    while not mfree:
                        yield
                    return banks[mfree.pop(0)]

                def mrel(b_):
                    mfree.append(banks.index(b_))

                def tile_chain(t):
                    par = t % 2
                    xnT = xnT2[par]
                    R16, R4, R1 = R16s[par], R4s[par], R1s[par]
                    yield from norm_gen(t, a2, sh2, h2T, t * 128, nsc2[par], router_dst=xnT, alloc=malloc_, rel=mrel)
                    yield "norm_done"
                    b = yield from malloc_()
                    mm_group(b[:, 0:NE], [(xnT[:, k * 128:(k + 1) * 128], wr2[:, k, :]) for k in range(8)],
                             r=[xnT, wr2], w=[b])
                    r16, r4_, r1 = R16, R4, R1
                    E("dve", lambda e, b=b: e.tensor_tensor(r16["sel"][:], b[:, 0:NE], cbc[:], ALU.add),
                      r=[b, cbc], w=[r16["sel"]])
                    mrel(b)
                    yield
                    E("act", lambda e: e.activation(r16["sc"][:], r16["sel"][:], AF.Sigmoid), r=[r16["sel"]], w=[r16["sc"]])
                    E("dve", lambda e: e.tensor_tensor(r16["sel"][:], r16["sc"][:], pg("rb"), ALU.add),
                      r=[r16["sc"], prmg], w=[r16["sel"]])
                    s3 = r16["sel"][:].rearrange("p (a b) -> p a b", a=4)

                    def tt_(o, i0, i1, op, rr, ww):
                        E("dve", lambda e: e.tensor_tensor(o, i0, i1, op), r=rr, w=ww)
                    tt_(r4_["hi1"][:], s3[:, :, 0], s3[:, :, 1], ALU.max, [r16["sel"]], [r4_["hi1"]])
                    tt_(r4_["lo1"][:], s3[:, :, 0], s3[:, :, 1], ALU.min, [r16["sel"]], [r4_["lo1"]])
                    tt_(r4_["hi2"][:], s3[:, :, 2], s3[:, :, 3], ALU.max, [r16["sel"]], [r4_["hi2"]])
                    tt_(r4_["lo2"][:], s3[:, :, 2], s3[:, :, 3], ALU.min, [r16["sel"]], [r4_["lo2"]])
                    tt_(r4_["top1"][:], r4_["hi1"][:], r4_["hi2"][:], ALU.max, [r4_["hi1"], r4_["hi2"]], [r4_["top1"]])
                    tt_(r4_["mm2"][:], r4_["hi1"][:], r4_["hi2"][:], ALU.min, [r4_["hi1"], r4_["hi2"]], [r4_["mm2"]])
                    tt_(r4_["mm3"][:], r4_["lo1"][:], r4_["lo2"][:], ALU.max, [r4_["lo1"], r4_["lo2"]], [r4_["mm3"]])
                    tt_(r4_["mm2"][:], r4_["mm2"][:], r4_["mm3"][:], ALU.max, [r4_["mm2"], r4_["mm3"]], [r4_["mm2"]])
                    tt_(r4_["grp"][:], r4_["top1"][:], r4_["mm2"][:], ALU.add, [r4_["top1"], r4_["mm2"]], [r4_["grp"]])
                    yield
                    E("dve", lambda e: e.tensor_reduce(r1["gmax"][:], r4_["grp"][:], AX.X, ALU.max),
                      r=[r4_["grp"]], w=[r1["gmax"]])
                    E("dve", lambda e: e.tensor_scalar(r4_["gmask"][:], r4_["grp"][:], r1["gmax"][:, 0:1], None, ALU.is_ge),
                      r=[r4_["grp"], r1["gmax"]], w=[r4_["gmask"]])
                    E("dve", lambda e: e.tensor_scalar(r4_["gmask"][:], r4_["gmask"][:], 1e9, -1e9, ALU.mult, ALU.add),
                      r=[r4_["gmask"]], w=[r4_["gmask"]])
                    E("dve", lambda e: e.tensor_tensor(r16["selm"][:].rearrange("p (a b) -> p a b", a=4), s3,
                                                       r4_["gmask"][:].unsqueeze(2).to_broadcast([128, 4, 4]), ALU.add),
                      r=[r16["sel"], r4_["gmask"]], w=[r16["selm"]])
                    yield
                    E("dve", lambda e: e.tensor_reduce(r1["t1"][:], r16["selm"][:], AX.X, ALU.max),
                      r=[r16["selm"]], w=[r1["t1"]])
                    E("dve", lambda e: e.tensor_scalar(r16["m1"][:], r16["selm"][:], r1["t1"][:, 0:1], None, ALU.is_ge),
                      r=[r16["selm"], r1["t1"]], w=[r16["m1"]])
                    E("dve", lambda e: e.scalar_tensor_tensor(r16["selm2"][:], r16["m1"][:], -1e9, r16["selm"][:],
                                                              ALU.mult, ALU.add),
                      r=[r16["m1"], r16["selm"]], w=[r16["selm2"]])
                    yield
                    E("dve", lambda e: e.tensor_reduce(r1["t2"][:], r16["selm2"][:], AX.X, ALU.max),
                      r=[r16["selm2"]], w=[r1["t2"]])
                    E("dve", lambda e: e.tensor_scalar(r16["m2"][:], r16["selm2"][:], r1["t2"][:, 0:1], None, ALU.is_ge),
                      r=[r16["selm2"], r1["t2"]], w=[r16["m2"]])
                    tt_(r16["m1"][:], r16["m1"][:], r16["m2"][:], ALU.add, [r16["m1"], r16["m2"]], [r16["m1"]])
                    tt_(r16["wts"][:], r16["sc"][:], r16["m1"][:], ALU.mult, [r16["sc"], r16["m1"]], [r16["wts"]])
                    yield
                    E("dve", lambda e: e.tensor_reduce(r1["den"][:], r16["wts"][:], AX.X, ALU.add),
                      r=[r16["wts"]], w=[r1["den"]])
                    E("dve", lambda e: e.reciprocal(r1["den"][:], r1["den"][:]), r=[r1["den"]], w=[r1["den"]])
                    E("dve", lambda e, t=t: e.tensor_scalar(gates[:, t, :], r16["wts"][:], r1["den"][:, 0:1], None, ALU.mult),
                      r=[r16["wts"], r1["den"]], w=[gates])


                tchains = []
                nxt = 0
                ok = True
                while nxt < NT or tchains:
                    if ok and nxt < NT and len(tchains) < 2:
                        tchains.append(tile_chain(nxt))
                        nxt += 1
                        ok = False
                    for gtc in list(tchains):
                        try:
                            v = next(gtc)
                            if v == "norm_done":
                                ok = True
                        except StopIteration:
                            tchains.remove(gtc)
                            ok = True
                for e_ in range(NE):
                    i = e_ % 2
                    for k in range(4):
                        E("dve", lambda e, k=k, i=i: e.tensor_tensor(wdb[i][:, k, :], wdb[i][:, k, :], g2bc[:], ALU.mult),
                          r=[wdb[i], g2bc], w=[wdb[i]])
                    for gq in range(NTOK // 512):
                        at = actT[gq % 2]
                        ts = slice(gq * 512, (gq + 1) * 512)
                        for dt_ in range(4):
                            bg, bu = bank(), bank()
                            mm_group(bg[:], [(wgb[i][:, k, dt_ * 128:(dt_ + 1) * 128], h2T[:, k, ts]) for k in range(8)],
                                     r=[wgb[i], h2T], w=[bg])
                            mm_group(bu[:], [(wub[i][:, k, dt_ * 128:(dt_ + 1) * 128], h2T[:, k, ts]) for k in range(8)],
                                     r=[wub[i], h2T], w=[bu])
                            sl = sil[dt_ % 2]
                            E("act", lambda e, bg=bg, sl=sl: e.activation(sl[:], bg[:], AF.Silu), r=[bg], w=[sl])
                            E("dve", lambda e, bu=bu, sl=sl, at=at, dt_=dt_: e.tensor_tensor(at[:, dt_, :], bu[:], sl[:],
                                                                                           ALU.mult),
                              r=[sl, bu], w=[at])
                        for tt in range(4):
                            t = gq * 4 + tt
                            for oc in range(2):
                                b = bank()
                                mm_group(b[:], [(at[:, k, tt * 128:(tt + 1) * 128], wdb[i][:, k, oc * 512:(oc + 1) * 512])
                                                for k in range(4)], r=[at, wdb[i]], w=[b])
                                E("dve", lambda e, b=b, t=t, oc=oc, e_=e_: e.scalar_tensor_tensor(
                                    x[:, t, oc * 512:(oc + 1) * 512], b[:], gates[:, t, e_:e_ + 1],
                                    x[:, t, oc * 512:(oc + 1) * 512], ALU.mult, ALU.add),
                                  r=[b, gates, xt[t]], w=[xt[t]])
                    if e_ + 2 < NE:
                        load_expert(e_ + 2)
                sy.barrier()

        yv = y_d.rearrange("(t p) d -> p t d", p=128)
        if final:
            with ExitStack() as ms:
                fss = [(mk(f"fjunk{i}", [128, D], BF16, ms), mk(f"fssum{i}", [128, 1], F32, ms),
                        mk(f"ftmp1{i}", [128, 1], F32, ms), mk(f"ftmp2{i}", [128, 1], F32, ms),
                        mk(f"frstd{i}", [128, 1], F32, ms)) for i in range(2)]
                yb = [mk(f"yb{i}", [128, D], F32, ms) for i in range(2)]

                def fin_gen(t):
                    junk, ssum, tmp1, tmp2, rstd = fss[t % 2]
                    E("dve", lambda e: e.memset(ssum[:], 0.0), w=[ssum])
                    E("act", lambda e, t=t: e.activation(junk[:], x[:, t, :], AF.Square, accum_out=ssum[:, 0:1]),
                      r=[xt[t], ssum], w=[junk, ssum])
                    yield
                    E("dve", lambda e: e.tensor_scalar(tmp1[:], ssum[:], 1.0 / D, EPS, ALU.mult, ALU.add), r=[ssum], w=[tmp1])
                    E("act", lambda e: e.sqrt(tmp2[:], tmp1[:]), r=[tmp1], w=[tmp2])
                    yield
                    E("dve", lambda e: e.reciprocal(rstd[:], tmp2[:]), r=[tmp2], w=[rstd])
                    o = yb[t % 2]
                    E("dve", lambda e, t=t, o=o: e.scalar_tensor_tensor(o[:], x[:, t, :], rstd[:, 0:1], pg("wfin"),
                                                                        ALU.mult, ALU.mult),
                      r=[xt[t], rstd, prmg], w=[o])
                    yield
                    DMA("sp", yv[:, t, :], o[:], r=[o], key=f"out{t % 2}")

                fch = []
                nxt_ = 0
                while nxt_ < NT or fch:
                    if nxt_ < NT and len(fch) < 2:
                        fch.append(fin_gen(nxt_))
                        nxt_ += 1
                    for gf in list(fch):
                        try:
                            next(gf)
                        except StopIteration:
                            fch.remove(gf)
                sy.barrier()
        else:
            for t in range(NT):
                DMA("sp", yv[:, t, :], x[:, t, :], r=[xt[t]], key="out")
            sy.barrier()
    return nc


def _t5_bucket(n):
    max_exact = 16
    large = max_exact + (np.log(np.maximum(n, 1) / max_exact) / math.log(128 / max_exact) * (32 - max_exact)).astype(np.int32)
    large = np.minimum(large, 31)
    return np.where(n < max_exact, n, large).astype(np.int32)


def _colmajor(v, ncol):
    return np.ascontiguousarray(np.asarray(v, np.float32).reshape(ncol, 128).T)


def _bc(v):
    v = np.asarray(v, np.float32).reshape(1, -1)
    return np.ascontiguousarray(np.broadcast_to(v, (128, v.shape[1])))


_WIN_PERM = None


def _win_perm():
    q_m = list(range(0, 256)); k_m = list(range(256, 512)); v_m = list(range(512, 1024)); o_m = list(range(1024, 1536))
    i_g = list(range(1536, 1540)); f_g = list(range(1540, 1544)); q_a = list(range(1544, 2056))
    k_a = list(range(2056, 2184)); v_a = list(range(2184, 2312)); g_m = list(range(2312, 3336)); g_a = list(range(3336, 4360))
    cols = []
    cols += q_m + k_m
    cols += q_a
    cols += k_a[0:64] + k_a[0:64] + k_a[64:128] + k_a[64:128] + i_g + f_g + v_a
    cols += v_m
    cols += o_m
    for pi in range(4):
        for oi in range(2):
            ot = 2 * pi + oi
            cols += g_m[ot * 128:(ot + 1) * 128] + g_a[ot * 128:(ot + 1) * 128]
    assert len(cols) == NWIN
    return np.asarray(cols, np.int64)


def _consts(rel_bias):
    cst = np.zeros((128, 384), np.float32)
    cst[:, 0:128] = np.eye(128, dtype=np.float32)
    s = np.arange(128)[:, None]
    l_ = np.arange(128)[None, :]
    cst[:, 128:256] = (s <= l_).astype(np.float32)
    cst[:, 256:384] = 1.0
    q = np.arange(128)[None, :]
    sk = np.arange(128)[:, None]
    bT = np.zeros((128, 2, 2, 2, 2, 128), np.float32)
    for blk in range(2):
        dist = q + 128 - (sk + blk * 128)
        ok = (dist >= 0) & (dist < 128)
        bucket = _t5_bucket(np.maximum(dist, 0))
        vals = np.asarray(rel_bias, np.float32)[bucket]
        vals = np.where(ok[:, :, None], vals, np.float32(-30000.0))
        for h in range(8):
            kv, g4 = h // 4, h % 4
            bT[:, kv, g4 % 2, blk, g4 // 2, :] = vals[:, :, h]
    return cst, np.ascontiguousarray(bT.reshape(128, 2048))


_PROGS = {}


def _prog(nl, final):
    key = (nl, final)
    if key not in _PROGS:
        _PROGS[key] = build(nl, final)
    return _PROGS[key]


def _prep_common(inp):
    perm = _win_perm()
    cst, biasT = _consts(inp["rel_bias"])
    return perm, cst, biasT


def _layer_arrays(inp, ls, perm):
    f = lambda a: np.ascontiguousarray(np.asarray(a, np.float32))
    d = {}
    d["w_ada"] = f(inp["w_ada"][ls])
    d["w_in"] = f(np.asarray(inp["w_in"])[ls][:, :, perm])
    d["w_br"] = f(np.stack([np.asarray(inp["w_br_m"])[ls], np.asarray(inp["w_br_a"])[ls]], axis=1))
    d["w_out"] = f(inp["w_out"][ls])
    d["wg"] = f(inp["w_gate_e"][ls])
    d["wu"] = f(inp["w_up_e"][ls])
    d["wd"] = f(inp["w_down_e"][ls])
    prml = []
    for l in ls:
        ba = np.asarray(inp["b_ada"][l], np.float32)
        parts = [
            _colmajor(inp["w_norm1"][l], 8), _colmajor(inp["w_norm2"][l], 8), _colmajor(ba, 48),
            np.ascontiguousarray(np.asarray(inp["conv_w"][l], np.float32).reshape(4, 4, 128).transpose(2, 1, 0).reshape(128, 16)),
            _colmajor(inp["conv_b"][l], 4),
            _bc(np.concatenate([np.asarray(inp["b_igate"][l]), np.asarray(inp["b_fgate"][l])])),
            _bc(inp["w_mnorm"][l]), _bc(inp["sinks"][l]),
        ]
        p = np.concatenate(parts, axis=1)
        assert p.shape == (128, NPL), p.shape
        prml.append(p)
    d["prml"] = np.ascontiguousarray(np.stack(prml, 0))
    d["prmb"] = np.ascontiguousarray(np.stack([np.concatenate(
        [_bc(np.asarray(inp["b_ada"][l], np.float32)[2048:3072]), _bc(np.asarray(inp["b_ada"][l], np.float32)[5120:6144])],
        axis=1) for l in ls], 0))
    return d


def _prmg(inp, b, half):
    wr = np.asarray(inp["w_router"], np.float32).reshape(8, 128, NE).transpose(1, 0, 2).reshape(128, 8 * NE)
    parts = [_colmajor(np.asarray(inp["c"])[b], 8), np.full((128, 1), float(half), np.float32),
             _bc(inp["router_bias"]), np.ascontiguousarray(wr), _bc(inp["w_final"])]
    p = np.concatenate(parts, axis=1)
    assert p.shape == (128, NPG)
    return np.ascontiguousarray(p)


def kernel(**inp):
    inp = {k: np.asarray(v) for k, v in inp.items()}
    perm, cst, biasT = _prep_common(inp)
    la = _layer_arrays(inp, [0, 1], perm)
    nc = _prog(2, True)
    in_maps = []
    for c in range(8):
        m = {"x": np.ascontiguousarray(inp["x"][c // 2, (c % 2) * NTOK:(c % 2 + 1) * NTOK, :], dtype=np.float32),
             "prmg": _prmg(inp, c // 2, c % 2), "cst": cst, "biasT": biasT}
        m.update(la)
        in_maps.append(m)
    res = run_bass_kernel_spmd(nc, in_maps, core_ids=list(range(8)))
    out = np.zeros((4, 4096, D), np.float32)
    for c in range(8):
        out[c // 2, (c % 2) * NTOK:(c % 2 + 1) * NTOK, :] = np.asarray(res.results[c]["y"], dtype=np.float32)
    return out
```

```python
import math
import os
from contextlib import ExitStack

import numpy as np
import concourse.bass as bass
import concourse.mybir as mybir
from concourse.bass_utils import run_bass_kernel_spmd

F32 = mybir.dt.float32
BF16 = mybir.dt.bfloat16
AF = mybir.ActivationFunctionType
ALU = mybir.AluOpType
AX = mybir.AxisListType

D = 1024
NTOK = 2048
NT = 16
G = 256
TPG = G // 128
NG = NTOK // G
EPS = 1e-6
NE = 16
DE = 512
NWIN = 4488
LN8 = math.log(0.125)
NCORES = 8

PL = {}
_o = 0
for _n, _w in [("wn1", 8), ("wn2", 8), ("bada", 48), ("convw", 16),
               ("convb", 4), ("bif", 8), ("wmn", 512), ("sinks", 8)]:
    PL[_n] = (_o, _o + _w)
    _o += _w
NPL = _o
PG = {}
_o = 0
for _n, _w in [("cT", 8), ("flag", 1), ("rb", 16), ("wr", 128), ("wfin", 1024)]:
    PG[_n] = (_o, _o + _w)
    _o += _w
NPG = _o


class Res:
    __slots__ = ("name", "w", "r")

    def __init__(self, name):
        self.name = name
        self.w = None
        self.r = {}


class Sync:
    def __init__(self, nc, es):
        self.nc = nc
        self.es = es
        self.engs = {"pe": nc.tensor, "act": nc.scalar, "dve": nc.vector, "pool": nc.gpsimd, "sp": nc.sync}
        self.sems = {}
        self.cnt = {}
        for k in self.engs:
            self.sems[k] = es.enter_context(nc.semaphore("s_" + k))
            self.cnt[k] = 0
        self.seen = {k: {} for k in self.engs}

    def dma_sem(self, key):
        if key not in self.sems:
            self.sems[key] = self.es.enter_context(self.nc.semaphore("d_" + key))
            self.cnt[key] = 0
        return key

    def need(self, eng, ev, same_ok=False):
        if ev is None:
            return
        k, v = ev
        if k == eng and not same_ok:
            return
        if self.seen[eng].get(k, 0) >= v:
            return
        self.engs[eng].wait_ge(self.sems[k], v)
        self.seen[eng][k] = v

    def deps(self, eng, reads, writes, force_same=False):
        for r in reads:
            self.need(eng, r.w, same_ok=True)
        for w in writes:
            self.need(eng, w.w, same_ok=force_same)
            for k, v in w.r.items():
                self.need(eng, (k, v), same_ok=force_same)

    def emit(self, eng, fn, reads=(), writes=(), inc=True):
        self.deps(eng, reads, writes)
        inst = fn(self.engs[eng])
        if inc:
            inst.then_inc(self.sems[eng], 1)
            self.cnt[eng] += 1
            ev = (eng, self.cnt[eng])
        else:
            ev = (eng, self.cnt[eng] + 1)
        for r in reads:
            r.r[eng] = max(r.r.get(eng, 0), ev[1])
        for w in writes:
            w.w = ev
            w.r = {}
        return inst

    def dma(self, queue, out, in_, reads=(), writes=(), semkey=None):
        self.deps(queue, reads, writes, force_same=True)
        semkey = self.dma_sem(semkey)
        inst = self.engs[queue].dma_start(out=out, in_=in_)
        inst.then_inc(self.sems[semkey], 16)
        self.cnt[semkey] += 16
        ev = (semkey, self.cnt[semkey])
        for r in reads:
            r.r[semkey] = ev[1]
        for w in writes:
            w.w = ev
            w.r = {}
        return inst

    def emit_cc(self, fn, reads, writes, key):
        self.deps("pool", reads, writes, force_same=True)
        key = self.dma_sem(key)
        inst = fn(self.engs["pool"])
        inst.then_inc(self.sems[key])
        self.cnt[key] = 1
        ev = (key, 1)
        for r in reads:
            r.r[key] = 1
        for w in writes:
            w.w = ev
            w.r = {}
        return inst

    def barrier(self, skip=()):
        for e in self.engs:
            for k in self.sems:
                if k != e and self.cnt[k] > 0 and k not in skip:
                    self.need(e, (k, self.cnt[k]))


class T:
    def __init__(self, es, nc, name, shape, dtype, psum=False):
        if psum:
            self.t = es.enter_context(nc.psum_tensor("p_" + name, shape, dtype))
        else:
            self.t = es.enter_context(nc.sbuf_tensor("t_" + name, shape, dtype))
        self.r = Res(name)
        self.name = name
        self.psum = psum

    def __getitem__(self, k):
        return self.t[k]


def build(nl, final, moe=True, dbg=99):
    nc = bass.Bass("TRN2", target_bir_lowering=False)

    def din(name, shape, dt=F32):
        return nc.dram_tensor(name, shape, dt, kind="ExternalInput").ap()

    def dout(name, shape, dt=F32):
        return nc.dram_tensor(name, shape, dt, kind="ExternalOutput").ap()

    x_d = din("x", [NTOK, D])
    prmg_d = din("prmg", [128, NPG])
    prml_d = din("prml", [nl, 128, NPL])
    prmb_d = din("prmb", [nl, 128, 2048])
    cst_d = din("cst", [128, 384])
    biasT_d = din("biasT", [128, 2048])
    wada_d = din("w_ada", [nl, D, 6 * D])
    win_d = din("w_in", [nl, D, NWIN])
    wbr_d = din("w_br", [nl, 2, 512, D])
    wout_d = din("w_out", [nl, D, D])
    wg_d = din("wg", [nl, NE, D, DE])
    wu_d = din("wu", [nl, NE, D, DE])
    wd_d = din("wd", [nl, NE, DE, D])
    y_d = dout("y", [NTOK, D])

    with ExitStack() as es:
        sy = Sync(nc, es)

        def E(eng, fn, r=(), w=(), inc=True):
            w = list(w) + [t for t in r if getattr(t, "psum", False) and t not in w]
            return sy.emit(eng, fn, reads=[t.r for t in r], writes=[t.r for t in w], inc=inc)

        def DMA(q, out, in_, r=(), w=(), key=None):
            return sy.dma(q, out, in_, reads=[t.r for t in r], writes=[t.r for t in w], semkey=key)

        def mk(name, shape, dt, stack=es):
            return T(stack, nc, name, shape, dt)

        x = mk("x", [128, NT, D], F32)
        xr = [Res(f"x{t}") for t in range(NT)]

        class XT:
            def __init__(self, t):
                self.r = xr[t]
        xt = [XT(t) for t in range(NT)]
        prmg = mk("prmg", [128, NPG], F32)
        prml = mk("prml", [128, NPL], F32)
        cst = mk("cst", [128, 384], F32)
        ident_b = mk("ident_b", [128, 128], BF16)
        tri_b = mk("tri_b", [128, 128], BF16)
        modc = mk("modc", [128, 48], F32)
        a1 = mk("a1", [128, 8], F32)
        a2 = mk("a2", [128, 8], F32)
        g1bc = mk("g1bc", [128, D], F32)
        g2bc = mk("g2bc", [128, D], F32)
        cond_bf = mk("cond_bf", [128, 8], BF16)
        cond_f = mk("cond_f", [128, 8], F32)
        banks = [T(es, nc, f"bank{i}", [128, 512], F32, psum=True) for i in range(8)]
        bstate = {"i": 0}

        def bank():
            b = banks[bstate["i"] % 8]
            bstate["i"] += 1
            return b

        ident_f = cst[:, 0:128]
        tri_f = cst[:, 128:256]
        ones_f = cst[:, 256:384]

        def pl(name, a=None, b=None):
            lo, hi = PL[name]
            if a is None:
                return prml[:, lo:hi]
            return prml[:, lo + a:lo + b]

        def pg(name, a=None, b=None):
            lo, hi = PG[name]
            if a is None:
                return prmg[:, lo:hi]
            return prmg[:, lo + a:lo + b]

        xv = x_d.rearrange("(t p) d -> p t d", p=128)
        for t0 in range(0, NT, 4):
            DMA("sp", x[:, t0:t0 + 4, :], xv[:, t0:t0 + 4, :], w=[xt[t0 + i] for i in range(4)], key=f"x{t0}")
        DMA("sp", prmg[:], prmg_d, w=[prmg], key="prmg")
        DMA("sp", cst[:], cst_d, w=[cst], key="cst")
        E("dve", lambda e: e.tensor_copy(ident_b[:], ident_f), r=[cst], w=[ident_b])
        E("dve", lambda e: e.tensor_copy(tri_b[:], tri_f), r=[cst], w=[tri_b])
        ones_b = mk("ones_b", [128, 128], BF16)
        E("dve", lambda e: e.tensor_copy(ones_b[:], ones_f), r=[cst], w=[ones_b])
        E("act", lambda e: e.activation(cond_f[:], pg("cT"), AF.Silu), r=[prmg], w=[cond_f])
        E("dve", lambda e: e.tensor_copy(cond_bf[:], cond_f[:]), r=[cond_f], w=[cond_bf])

        def mm_group(out_ap, pairs, r, w):
            n = len(pairs)
            for i, (lt, rh) in enumerate(pairs):
                E("pe", lambda e, lt=lt, rh=rh, i=i: e.matmul(out_ap, lt, rh, start=(i == 0), stop=(i == n - 1)),
                  r=r, w=w, inc=(i == n - 1))

        sm = {}

        def small(name, shape, dt=F32, stack=es):
            if name not in sm:
                sm[name] = mk(name, shape, dt, stack)
            return sm[name]

        def norm_gen(t, acol, shcol, dst, dcol, sc, router_dst=None, alloc=None, rel=None):
            junk, ssum, tmp1, tmp2, rstd, xn = sc
            junk = xn
            E("dve", lambda e: e.memset(ssum[:], 0.0), w=[ssum])
            E("act", lambda e: e.activation(junk[:], x[:, t, :], AF.Square, accum_out=ssum[:, 0:1]),
              r=[xt[t], ssum], w=[junk, ssum])
            yield
            E("dve", lambda e: e.tensor_scalar(tmp1[:], ssum[:], 1.0 / D, EPS, ALU.mult, ALU.add), r=[ssum], w=[tmp1])
            E("act", lambda e: e.sqrt(tmp2[:], tmp1[:]), r=[tmp1], w=[tmp2])
            yield
            E("dve", lambda e: e.reciprocal(rstd[:], tmp2[:]), r=[tmp2], w=[rstd])
            E("act", lambda e: e.mul(xn[:], x[:, t, :], rstd[:, 0:1]), r=[xt[t], rstd], w=[xn])
            yield
            for half in range(2):
                if alloc is None:
                    b = bank()
                else:
                    b = yield from alloc()
                for j in range(4):
                    kk = half * 4 + j
                    E("pe", lambda e, j=j, kk=kk, b=b: e.transpose(b[:, j * 128:(j + 1) * 128],
                                                                    xn[:, kk * 128:(kk + 1) * 128], ident_f),
                      r=[xn, cst], w=[b], inc=(j == 3))
                yield
                for j in range(4):
                    kk = half * 4 + j
                    if half == 0:
                        E("dve", lambda e, j=j, kk=kk, b=b: e.tensor_scalar(
                            dst[:, kk, dcol:dcol + 128], b[:, j * 128:(j + 1) * 128],
                            acol[:, kk:kk + 1], shcol[:, kk:kk + 1], ALU.mult, ALU.add), r=[b, modc, a1, a2], w=[dst])
                    else:
                        E("act", lambda e, j=j, kk=kk, b=b: e.activation(
                            dst[:, kk, dcol:dcol + 128], b[:, j * 128:(j + 1) * 128], AF.Identity,
                            bias=shcol[:, kk:kk + 1], scale=acol[:, kk:kk + 1]), r=[b, modc, a1, a2], w=[dst])
                if router_dst is not None:
                    E("act", lambda e, half=half, b=b: e.copy(router_dst[:, half * 512:(half + 1) * 512], b[:]),
                      r=[b], w=[router_dst])
                if rel is not None:
                    rel(b)
                yield

        def norm_tile(t, acol, shcol, dst, dcol, sc, router_dst=None):
            for _ in norm_gen(t, acol, shcol, dst, dcol, sc, router_dst):
                pass

        for l in range(nl):
            if dbg <= 1:
                break
            sy.barrier()
            DMA("sp", prml[:], prml_d[l], w=[prml], key="prml")
            with ExitStack() as ms:
                biasT = mk(f"biasT_{l}", [128, 2048], BF16, ms)
                DMA("pool", biasT[:], biasT_d, w=[biasT], key="biasT")
                ring = [mk(f"ring{i}_{l}", [128, 8, 512], BF16, ms) for i in range(3)]
                wbr = mk(f"wbr_{l}", [128, 2, 4, D], BF16, ms)
                wout = mk(f"wout_{l}", [128, 8, D], BF16, ms)
                hT2 = [mk(f"hT{i}_{l}", [128, 8, G], BF16, ms) for i in range(2)]
                qkpre = [mk(f"qkpre{i}_{l}", [128, 3 + G], F32, ms) for i in range(4)]
                qkT = mk(f"qkT_{l}", [128, 4, G], BF16, ms)
                qaT = None
                kaT = mk(f"kaT_{l}", [128, 2, 128 + G], BF16, ms)
                vaug = [mk(f"vaug{i}_{l}", [128, 2, 72], BF16, ms) for i in range(3)]
                vhalo = mk(f"vhalo_{l}", [128, 2, 72], BF16, ms)
                state_f = mk(f"state_f_{l}", [128, 2, 129], F32, ms)
                state_b = mk(f"state_b_{l}", [128, 2, 129], BF16, ms)
                hmT = None
                haT = None
                mT = None
                esink = mk(f"esink_{l}", [128, 8], F32, ms)
                nsc = (None, mk(f"ssum_{l}", [128, 1], F32, ms),
                       mk(f"tmp1_{l}", [128, 1], F32, ms), mk(f"tmp2_{l}", [128, 1], F32, ms),
                       mk(f"rstd_{l}", [128, 1], F32, ms), mk(f"xn_{l}", [128, D], F32, ms))
                gsb = [mk(f"gsb{i}_{l}", [128, 8], F32, ms) for i in range(TPG)]
                ef4 = [mk(f"ef4{i}_{l}", [128, 4], F32, ms) for i in range(TPG)]
                logf = [mk(f"logf{i}_{l}", [128, 4], F32, ms) for i in range(TPG)]
                lhi = [mk(f"lhi{i}_{l}", [128, 4], BF16, ms) for i in range(TPG)]
                llo = [mk(f"llo{i}_{l}", [128, 4], BF16, ms) for i in range(TPG)]
                w4 = [mk(f"w4{i}_{l}", [128, 4], F32, ms) for i in range(TPG)]
                sc4 = [mk(f"sc4{i}_{l}", [128, 4], F32, ms) for i in range(TPG)]
                eb4 = [mk(f"eb4{i}_{l}", [128, 4], F32, ms) for i in range(TPG)]
                vpr = [mk(f"vpr{i}_{l}", [128, 4, 136], BF16, ms) for i in range(TPG)]
                som = None
                ktok = [mk(f"ktok{i}_{l}", [128, 2, 2, 128], BF16, ms) for i in range(TPG)]
                ebsel = [mk(f"ebsel{i}_{l}", [128, 2], F32, ms) for i in range(TPG)]
                sd = None
                den4 = None
                r4 = None
                hm = None
                sqj = None
                ss4 = None
                rn4 = None
                hmo = None
                tmpS = None
                pT = None
                dn4 = None
                hao = None
                sg1 = mk(f"sg1_{l}", [128, G], F32, ms)
                acc = sg1
                for i in range(TPG):
                    E("dve", lambda e, i=i: e.memset(ktok[i][:], 0.0), w=[ktok[i]])
                sg2 = None

                for i in range(2):
                    DMA("pool", wbr[:, i, :, :], wbr_d[l, i].rearrange("(k p) n -> p k n", p=128), w=[wbr], key="wbr")
                DMA("pool", wout[:], wout_d[l].rearrange("(k p) n -> p k n", p=128), w=[wout], key="wout")
                flag = pg("flag")
                E("act", lambda e: e.activation(esink[:], pl("sinks"), AF.Exp), r=[prml], w=[esink])
                for i in range(3):
                    E("dve", lambda e, i=i: e.memset(vaug[i][:], 1.0), w=[vaug[i]])
                ib_d = nc.dram_tensor(f"ib{l}", [128, 656], F32)
                ob_d = nc.dram_tensor(f"ob{l}", [256, 656], F32)
                ibv = ib_d.ap()
                obv = ob_d.ap()

                class RO:
                    def __init__(self, n):
                        self.r = Res(n)
                ib_parts = [RO(f"ibp{i}_{l}") for i in range(7)]
                obR = RO(f"ob_{l}")
                pieces = [(0, 512), (512, 512), (1024, 392), (1416, 512), (1928, 512),
                          (2440, 512), (2952, 512), (3464, 512), (3976, 512)]

                class _Stop(Exception):
                    pass

                def chk(k):
                    if dbg == k:
                        raise _Stop()
                CUR = {"free": list(range(8))}
                ada_es = ExitStack()
                aring = [mk(f"aring{i}_{l}", [128, 8, 512], BF16, ada_es) for i in range(3)]
                prmb = mk(f"prmb_{l}", [128, 2048], F32, ada_es)
                DMA("sp", prmb[:], prmb_d[l], w=[prmb], key="prmb")
                cond_bc = mk(f"cond_bc_{l}", [128, 8, 128], BF16, ada_es)
                E("dve", lambda e: e.tensor_copy(cond_bc[:], cond_f[:].unsqueeze(2).to_broadcast([128, 8, 128])),
                  r=[cond_f], w=[cond_bc])

                def ada_issue(j, l=l):
                    rb_ = aring[j % 3]
                    DMA("pool", rb_[:], wada_d[l, :, j * 512:(j + 1) * 512].rearrange("(k p) n -> p k n", p=128),
                        w=[rb_], key=f"aring{j % 3}")

                def ada_steps(js, l=l):
                    ada_issue(js[0])
                    for ji, j in enumerate(js):
                        if ji + 1 < len(js):
                            ada_issue(js[ji + 1])
                        yield
                        while not CUR["free"]:
                            yield
                        bi_ = CUR["free"].pop(0)
                        b = banks[bi_]
                        rb_ = aring[j % 3]
                        if j in (4, 5, 10, 11):
                            gb = g1bc if j < 6 else g2bc
                            boff_ = 0 if j < 6 else 1024
                            c0 = (j % 2) * 512 if j < 6 else (j - 10) * 512
                            mm_group(b[:], [(cond_bc[:, k, :], rb_[:, k, :]) for k in range(8)], r=[cond_bc, rb_], w=[b])
                            E("dve", lambda e, b=b, gb=gb, boff_=boff_, c0=c0: e.tensor_tensor(
                                gb[:, c0:c0 + 512], b[:], prmb[:, boff_ + c0:boff_ + c0 + 512], ALU.add), r=[b, prmb], w=[gb])
                        else:
                            for jt in range(4):
                                mm_group(b[:, jt:jt + 1],
                                         [(rb_[:, k, jt * 128:(jt + 1) * 128], cond_bf[:, k:k + 1]) for k in range(8)],
                                         r=[cond_bf, rb_], w=[b])
                            E("dve", lambda e, b=b, j=j: e.tensor_tensor(modc[:, 4 * j:4 * j + 4], b[:, 0:4],
                                                                         pl("bada", 4 * j, 4 * j + 4), ALU.add),
                              r=[b, prml], w=[modc])
                        CUR["free"].append(bi_)
                        yield

                for _ in ada_steps([0, 1, 2, 3]):
                    pass
                E("dve", lambda e: e.scalar_tensor_tensor(a1[:], modc[:, 8:16], 1.0, pl("wn1"), ALU.add, ALU.mult),
                  r=[modc, prml], w=[a1])

                def ada_rest():
                    yield from ada_steps([4, 5, 6, 7, 8, 9, 10, 11])
                    E("dve", lambda e: e.scalar_tensor_tensor(a2[:], modc[:, 32:40], 1.0, pl("wn2"), ALU.add, ALU.mult),
                      r=[modc, prml], w=[a2])
                background = [ada_rest()]
                sh1 = modc[:, 0:8]
                sh2 = modc[:, 24:32]


                p2s = ExitStack()
                for pss in (1, 2):
                    if pss == 1:
                        E("dve", lambda e: e.memset(state_f[:], 0.0), w=[state_f])
                        E("dve", lambda e: e.memset(state_b[:], 0.0), w=[state_b])
                        for i in range(4):
                            E("dve", lambda e, i=i: e.memset(qkpre[i][:, 0:3], 0.0), w=[qkpre[i]])
                        E("dve", lambda e: e.memset(kaT[:, :, 0:128], 0.0), w=[kaT])
                    else:
                        for gb_ in background:
                            for _ in gb_:
                                pass
                        background = []
                        sy.barrier(skip=(f"cc{l}",))
                        ada_es.close()
                        qaT = mk(f"qaT_{l}", [128, 4, G], BF16, p2s)
                        hmT = mk(f"hmT_{l}", [128, 4, G], BF16, p2s)
                        haT = mk(f"haT_{l}", [128, 4, G], BF16, p2s)
                        mT = mk(f"mT_{l}", [128, 8, G], BF16, p2s)
                        som = [mk(f"som{i}_{l}", [128, 512], BF16, p2s) for i in range(TPG)]
                        sd = [mk(f"sd{i}_{l}", [128, 4, 128], BF16, p2s) for i in range(TPG)]
                        den4 = [mk(f"den4{i}_{l}", [128, 4], F32, p2s) for i in range(TPG)]
                        r4 = [mk(f"r4{i}_{l}", [128, 4], F32, p2s) for i in range(TPG)]
                        hm = [mk(f"hm{i}_{l}", [128, 4, 128], F32, p2s) for i in range(TPG)]
                        sqj = [mk(f"sqj{i}_{l}", [128, 128], F32, p2s) for i in range(TPG)]
                        ss4 = [mk(f"ss4{i}_{l}", [128, 4], F32, p2s) for i in range(TPG)]
                        rn4 = [mk(f"rn4{i}_{l}", [128, 4], F32, p2s) for i in range(TPG)]
                        hmo = [mk(f"hmo{i}_{l}", [128, 512], BF16, p2s) for i in range(TPG)]
                        tmpS = [mk(f"tmpS{i}_{l}", [128, 2, 512], F32, p2s) for i in range(TPG)]
                        pT = [mk(f"pT{i}_{l}", [128, 2, 512], BF16, p2s) for i in range(TPG)]
                        dn4 = [mk(f"dn4{i}_{l}", [128, 4], F32, p2s) for i in range(TPG)]
                        hao = [mk(f"hao{i}_{l}", [128, 512], BF16, p2s) for i in range(TPG)]
                        sg2 = mk(f"sg2_{l}", [128, G], F32, p2s)
                        for k in range(8):
                            E("dve", lambda e, k=k: e.tensor_tensor(wout[:, k, :], wout[:, k, :], g1bc[:], ALU.mult),
                              r=[wout, g1bc], w=[wout])
                        DMA("sp", state_f[:], obv[0:128, 0:258].rearrange("p (a b) -> p a b", a=2), r=[obR], w=[state_f], key="h_st")
                        E("dve", lambda e: e.tensor_scalar(state_f[:], state_f[:], flag[:, 0:1], None, ALU.mult),
                          r=[state_f, prmg], w=[state_f])
                        E("act", lambda e: e.copy(state_b[:], state_f[:]), r=[state_f], w=[state_b])
                        for i in range(4):
                            DMA("sp", qkpre[i][:, 0:3], obv[0:128, 258 + 3 * i:261 + 3 * i], r=[obR], w=[qkpre[i]], key=f"h_cv{i}")
                            E("dve", lambda e, i=i: e.tensor_scalar(qkpre[i][:, 0:3], qkpre[i][:, 0:3], flag[:, 0:1], None,
                                                                      ALU.mult), r=[qkpre[i], prmg], w=[qkpre[i]])
                        DMA("pool", kaT[:, :, 0:128], obv[0:128, 270:526].rearrange("p (a b) -> p a b", a=2), r=[obR], w=[kaT], key="h_ka")
                        DMA("pool", vhalo[:, :, 0:65], obv[0:128, 526:656].rearrange("p (a b) -> p a b", a=2), r=[obR], w=[vhalo], key="h_va")
                        E("dve", lambda e: e.tensor_scalar(vhalo[:, :, 0:65], vhalo[:, :, 0:65], flag[:, 0:1], None, ALU.mult),
                          r=[vhalo, prmg], w=[vhalo])
                    pcs = (0, 2, 3) if pss == 1 else tuple(range(9))
                    seq = [(g, p) for g in range(NG) for p in pcs]
                    rstate = {"issued": 0}

                    def ring_issue(seq=seq, rstate=rstate):
                        i = rstate["issued"]
                        if i >= len(seq):
                            return
                        _, p = seq[i]
                        c0, wd_ = pieces[p]
                        rb_ = ring[i % 3]
                        DMA("pool", rb_[:, :, 0:wd_], win_d[l, :, c0:c0 + wd_].rearrange("(k p) n -> p k n", p=128),
                            w=[rb_], key=f"ring{i % 3}")
                        rstate["issued"] += 1

                    ring_issue()
                    ring_issue()
                    uidx = {"i": 0}

                    def ring_get(uidx=uidx):
                        i = uidx["i"]
                        uidx["i"] += 1
                        return ring[i % 3]

                    try:
                        for g in range(NG):
                            if dbg <= 3 or (dbg <= 30 and g >= 1):
                                break
                            hT = hT2[g % 2]
                            if g == 0 and (pss == 1 or os.environ.get("KSEQ") is not None):
                                for tt in range(TPG):
                                    norm_tile(g * TPG + tt, a1, sh1, hT, tt * 128, nsc)
                            chk(11)
                            rb_ = ring_get()
                            for mt in range(4):
                                if pss == 1 and mt < 2 and g < NG - 1:
                                    continue
                                b = bank()
                                mm_group(b[:, 0:G], [(rb_[:, k, mt * 128:(mt + 1) * 128], hT[:, k, :]) for k in range(8)],
                                         r=[rb_, hT], w=[b])
                                E("act", lambda e, b=b, mt=mt: e.copy(qkpre[mt][:, 3:3 + G], b[:, 0:G]), r=[b], w=[qkpre[mt]])
                                if pss == 2 or mt >= 2:
                                    cw = lambda j, mt=mt: pl("convw", mt * 4 + j, mt * 4 + j + 1)
                                    E("dve", lambda e, mt=mt, cw=cw: e.tensor_scalar(acc[:], qkpre[mt][:, 0:G], cw(0),
                                                                                       pl("convb", mt, mt + 1), ALU.mult, ALU.add),
                                      r=[qkpre[mt], prml], w=[acc])
                                    for j in range(1, 4):
                                        E("dve", lambda e, mt=mt, j=j, cw=cw: e.scalar_tensor_tensor(
                                            acc[:], qkpre[mt][:, j:j + G], cw(j), acc[:], ALU.mult, ALU.add),
                                          r=[qkpre[mt], prml, acc], w=[acc])
                                    E("act", lambda e, mt=mt: e.activation(qkT[:, mt, :], acc[:], AF.Silu), r=[acc], w=[qkT])
                                E("act", lambda e, mt=mt: e.copy(qkpre[mt][:, 0:3], qkpre[mt][:, G:G + 3]),
                                  r=[qkpre[mt]], w=[qkpre[mt]])
                            ring_issue()
                            chk(12)
                            if pss == 2:
                                rb_ = ring_get()
                                for mt in range(4):
                                    b = bank()
                                    mm_group(b[:, 0:G], [(rb_[:, k, mt * 128:(mt + 1) * 128], hT[:, k, :]) for k in range(8)],
                                             r=[rb_, hT], w=[b])
                                    E("act", lambda e, b=b, mt=mt: e.copy(qaT[:, mt, :], b[:, 0:G]), r=[b], w=[qaT])
                                ring_issue()
                            chk(13)
                            rb_ = ring_get()
                            for j in range(2):
                                b = bank()
                                mm_group(b[:, 0:G], [(rb_[:, k, j * 128:(j + 1) * 128], hT[:, k, :]) for k in range(8)],
                                         r=[rb_, hT], w=[b])
                                E("act", lambda e, b=b, j=j: e.copy(kaT[:, j, 128:128 + G], b[:, 0:G]), r=[b], w=[kaT])
                            chk(131)
                            def gate_gen(tt, g=g):
                                t = g * TPG + tt
                                va = vaug[t % 3]
                                b = bank()
                                yield
                                mm_group(b[:, 0:136], [(hT[:, k, tt * 128:(tt + 1) * 128], rb_[:, k, 256:392]) for k in range(8)],
                                         r=[rb_, hT], w=[b])
                                yield
                                E("dve", lambda e, b=b, tt=tt: e.tensor_tensor(gsb[tt][:], b[:, 0:8], pl("bif"), ALU.add),
                                  r=[b, prml], w=[gsb[tt]])
                                yield
                                E("act", lambda e, b=b, va=va: e.copy(va[:, :, 0:64],
                                                                        b[:, 8:136].rearrange("p (a b) -> p a b", a=2)),
                                  r=[b], w=[va])
                                yield
                                E("act", lambda e, tt=tt: e.activation(ef4[tt][:], gsb[tt][:, 4:8], AF.Exp, scale=-1.0),
                                  r=[gsb[tt]], w=[ef4[tt]])
                                yield
                                E("act", lambda e, tt=tt: e.activation(ef4[tt][:], ef4[tt][:], AF.Ln, bias=1.0),
                                  r=[ef4[tt]], w=[ef4[tt]])
                                yield
                                E("dve", lambda e, tt=tt: e.tensor_scalar(logf[tt][:], ef4[tt][:], -1.0, None, ALU.mult),
                                  r=[ef4[tt]], w=[logf[tt]])
                                yield
                                b2 = bank()
                                yield
                                E("dve", lambda e, tt=tt: e.tensor_copy(lhi[tt][:], logf[tt][:]), r=[logf[tt]], w=[lhi[tt]])
                                yield
                                E("dve", lambda e, tt=tt: e.tensor_tensor(llo[tt][:], logf[tt][:], lhi[tt][:], ALU.subtract),
                                  r=[logf[tt], lhi[tt]], w=[llo[tt]])
                                yield
                                mm_group(b2[:, 0:4], [(tri_b[:], lhi[tt][:]), (tri_b[:], llo[tt][:])],
                                         r=[tri_b, lhi[tt], llo[tt]], w=[b2])
                                yield
                                mm_group(b2[:, 4:8], [(ones_b[:], lhi[tt][:]), (ones_b[:], llo[tt][:])],
                                         r=[ones_b, lhi[tt], llo[tt]], w=[b2])
                                yield
                                E("dve", lambda e, b2=b2, tt=tt: e.tensor_tensor(w4[tt][:], b2[:, 0:4], gsb[tt][:, 0:4],
                                                                                   ALU.subtract), r=[gsb[tt], b2], w=[w4[tt]])
                                yield
                                E("act", lambda e, tt=tt: e.activation(w4[tt][:], w4[tt][:], AF.Exp, scale=-1.0), r=[w4[tt]], w=[w4[tt]])
                                yield
                                E("act", lambda e, b2=b2, tt=tt: e.activation(sc4[tt][:], b2[:, 0:4], AF.Exp),
                                  r=[b2], w=[sc4[tt]])
                                yield
                                E("act", lambda e, b2=b2, tt=tt: e.activation(eb4[tt][:], b2[:, 4:8], AF.Exp),
                                  r=[b2], w=[eb4[tt]])
                                eb2 = eb4[tt][:, 0:4].rearrange("p (j r) -> p j r", r=2)
                                yield
                                E("act", lambda e, tt=tt, eb2=eb2: e.copy(ebsel[tt][0:64, :], eb2[0:64, :, 0]),
                                  r=[eb4[tt]], w=[ebsel[tt]])
                                E("act", lambda e, tt=tt, eb2=eb2: e.copy(ebsel[tt][64:128, :], eb2[64:128, :, 1]),
                                  r=[eb4[tt]], w=[ebsel[tt]])

                            ggs = [gate_gen(tt) for tt in range(TPG)]
                            while ggs:
                                for gg_ in list(ggs):
                                    try:
                                        next(gg_)
                                    except StopIteration:
                                        ggs.remove(gg_)
                            chk(138)
                            ring_issue()
                            chk(14)
                            rb_ = ring_get()
                            for tt in range(TPG):
                                b = bank()
                                mm_group(b[:], [(hT[:, k, tt * 128:(tt + 1) * 128], rb_[:, k, :]) for k in range(8)],
                                         r=[rb_, hT], w=[b])
                                E("dve", lambda e, b=b, tt=tt: e.tensor_tensor(
                                    vpr[tt][:, :, 0:128], b[:].rearrange("p (a b) -> p a b", a=4),
                                    w4[tt][:].unsqueeze(2).to_broadcast([128, 4, 128]), ALU.mult), r=[b, w4[tt]], w=[vpr[tt]])
                                E("dve", lambda e, tt=tt: e.tensor_copy(vpr[tt][:, :, 128:129], w4[tt][:].unsqueeze(2)),
                                  r=[w4[tt]], w=[vpr[tt]])
                            ring_issue()
                            chk(15)
                            if pss == 2:
                                rb_ = ring_get()
                                for tt in range(TPG):
                                    b = bank()
                                    mm_group(b[:], [(hT[:, k, tt * 128:(tt + 1) * 128], rb_[:, k, :]) for k in range(8)],
                                             r=[rb_, hT], w=[b])
                                    E("act", lambda e, b=b, tt=tt: e.activation(som[tt][:], b[:], AF.Sigmoid), r=[b], w=[som[tt]])
                                    E("dve", lambda e, tt=tt: e.tensor_tensor(som[tt][:], som[tt][:], pl("wmn"), ALU.mult),
                                      r=[som[tt], prml], w=[som[tt]])
                                ring_issue()
                            chk(16)
                            free_banks = list(range(8))
                            CUR["free"] = free_banks

                            def galloc():
                                while not free_banks:
                                    yield
                                return banks[free_banks.pop(0)]

                            def grel(b_):
                                free_banks.append(banks.index(b_))

                            def mlstm_gen(tt, g=g):
                                t = g * TPG + tt
                                cs = slice(tt * 128, (tt + 1) * 128)
                                ktok_ = ktok[tt]
                                if pss == 2:
                                    sd_, den4_, r4_, hm_, ss4_, rn4_, hmo_, sqj_ = (
                                        sd[tt], den4[tt], r4[tt], hm[tt], ss4[tt], rn4[tt], hmo[tt], sqj[tt])
                                b = yield from galloc()
                                bb = b[:].bitcast(BF16)
                                for j in range(2):
                                    E("pe", lambda e, j=j, bb=bb: e.transpose(bb[:, j * 128:(j + 1) * 128], qkT[:, 2 + j, cs],
                                                                               ident_b[:]),
                                      r=[qkT, ident_b], w=[b], inc=(j == 1))
                                bb3 = bb[:, 0:256].rearrange("p (j c) -> p j c", j=2)
                                for w_ in range(2):
                                    E("act", lambda e, w_=w_: e.copy(ktok_[:, :, w_, w_ * 64:(w_ + 1) * 64],
                                                                     bb3[:, :, w_ * 64:(w_ + 1) * 64]), r=[b], w=[ktok_])
                                grel(b)
                                if pss == 2:
                                    bsr = [(yield from galloc()), (yield from galloc())]
                                    for h in range(4):
                                        p0 = 64 * (h % 2)
                                        rg, jj = h % 2, h // 2
                                        E("pe", lambda e, h=h, p0=p0, rg=rg, jj=jj: e.matmul(
                                            bsr[rg][:, jj * 128:(jj + 1) * 128], qkT[p0:p0 + 64, 2 + h // 2, cs],
                                            qkT[p0:p0 + 64, h // 2, cs], start=True, stop=True), r=[qkT], w=[bsr[rg]], inc=True)
                                    sd4 = sd_[:].rearrange("p (j r) l -> p j r l", r=2)
                                    for rg in range(2):
                                        E("dve", lambda e, rg=rg: e.tensor_tensor(
                                            sd4[:, :, rg, :], bsr[rg][:, 0:256].rearrange("p (a b) -> p a b", a=2),
                                            tri_f.unsqueeze(1).to_broadcast([128, 2, 128]), ALU.mult), r=[bsr[rg], cst], w=[sd_])
                                    grel(bsr[0])
                                    grel(bsr[1])
                                    bn = [(yield from galloc()), (yield from galloc())]
                                    for h in range(4):
                                        p0 = 64 * (h % 2)
                                        o = bn[h // 2][:, (h % 2) * 256:(h % 2) * 256 + 129]
                                        E("pe", lambda e, h=h, o=o: e.matmul(o, sd_[:, h, :], vpr[tt][:, h, 0:129], start=True,
                                                                             stop=False),
                                          r=[sd_, vpr[tt]], w=[bn[h // 2]], inc=False)
                                        E("pe", lambda e, h=h, o=o, p0=p0: e.matmul(o, qkT[p0:p0 + 64, h // 2, cs],
                                                                                    state_b[p0:p0 + 64, h // 2, :],
                                                                                    start=False, stop=True),
                                          r=[qkT, state_b], w=[bn[h // 2]], inc=True)
                                bd = yield from galloc()
                                for j in range(2):
                                    o = bd[:, j * 256:j * 256 + 129]
                                    E("pe", lambda e, j=j, o=o: e.matmul(o, ktok_[:, j, 0, :], vpr[tt][:, 2 * j, 0:129],
                                                                         start=True, stop=False),
                                      r=[ktok_, vpr[tt]], w=[bd], inc=False)
                                    E("pe", lambda e, j=j, o=o: e.matmul(o, ktok_[:, j, 1, :], vpr[tt][:, 2 * j + 1, 0:129],
                                                                         start=False, stop=True),
                                      r=[ktok_, vpr[tt]], w=[bd], inc=True)
                                E("dve", lambda e: e.tensor_tensor(
                                    state_f[:], bd[:, 0:512].rearrange("p (j c) -> p j c", j=2)[:, :, 0:129], state_f[:],
                                    ALU.add), r=[state_f, bd], w=[state_f])
                                E("dve", lambda e: e.tensor_tensor(
                                    state_f[:], state_f[:], ebsel[tt][:].unsqueeze(2).to_broadcast([128, 2, 129]), ALU.mult),
                                  r=[state_f, ebsel[tt]], w=[state_f])
                                E("act", lambda e: e.copy(state_b[:], state_f[:]), r=[state_f], w=[state_b])
                                grel(bd)
                                yield "state_done"
                                if pss != 2:
                                    return
                                for j in range(2):
                                    v3 = bn[j][:].rearrange("p (a b) -> p a b", a=2)
                                    E("dve", lambda e, j=j, v3=v3: e.scalar_tensor_tensor(
                                        den4_[:, 2 * j:2 * j + 2], v3[:, :, 128], 0.125, sc4[tt][:, 2 * j:2 * j + 2],
                                        ALU.mult, ALU.mult), r=[bn[j], sc4[tt]], w=[den4_])
                                E("act", lambda e: e.activation(den4_[:], den4_[:], AF.Abs), r=[den4_], w=[den4_])
                                yield
                                E("dve", lambda e: e.tensor_scalar(den4_[:], den4_[:], 1.0, None, ALU.max), r=[den4_], w=[den4_])
                                E("dve", lambda e: e.reciprocal(den4_[:], den4_[:]), r=[den4_], w=[den4_])
                                E("dve", lambda e: e.scalar_tensor_tensor(r4_[:], sc4[tt][:], 0.125, den4_[:], ALU.mult, ALU.mult),
                                  r=[sc4[tt], den4_], w=[r4_])
                                yield
                                for j in range(2):
                                    v3 = bn[j][:].rearrange("p (a b) -> p a b", a=2)
                                    E("dve", lambda e, j=j, v3=v3: e.tensor_tensor(
                                        hm_[:, 2 * j:2 * j + 2, :], v3[:, :, 0:128],
                                        r4_[:, 2 * j:2 * j + 2].unsqueeze(2).to_broadcast([128, 2, 128]), ALU.mult),
                                      r=[bn[j], r4_], w=[hm_])
                                E("dve", lambda e: e.memset(ss4_[:], 0.0), w=[ss4_])
                                grel(bn[0])
                                grel(bn[1])
                                yield
                                for h in range(4):
                                    E("act", lambda e, h=h: e.activation(sqj_[:], hm_[:, h, :], AF.Square,
                                                                         accum_out=ss4_[:, h:h + 1]),
                                      r=[hm_, ss4_], w=[sqj_, ss4_])
                                yield
                                E("dve", lambda e: e.tensor_scalar(ss4_[:], ss4_[:], 1.0 / 128, EPS, ALU.mult, ALU.add),
                                  r=[ss4_], w=[ss4_])
                                E("act", lambda e: e.sqrt(ss4_[:], ss4_[:]), r=[ss4_], w=[ss4_])
                                yield
                                E("dve", lambda e: e.reciprocal(rn4_[:], ss4_[:]), r=[ss4_], w=[rn4_])
                                E("dve", lambda e: e.tensor_tensor(hm_[:], hm_[:], rn4_[:].unsqueeze(2).to_broadcast([128, 4, 128]),
                                                                   ALU.mult), r=[hm_, rn4_], w=[hm_])
                                yield
                                hm2 = hm_[:].rearrange("p a b -> p (a b)")
                                E("dve", lambda e: e.tensor_tensor(hmo_[:], hm2, som[tt][:], ALU.mult), r=[hm_, som[tt]], w=[hmo_])
                                yield
                                b = yield from galloc()
                                bb = b[:].bitcast(BF16)
                                for f in range(4):
                                    E("pe", lambda e, f=f, bb=bb: e.transpose(bb[:, f * 128:(f + 1) * 128],
                                                                               hmo_[:, f * 128:(f + 1) * 128], ident_b[:]),
                                      r=[hmo_, ident_b], w=[b], inc=(f == 3))
                                E("act", lambda e, bb=bb: e.copy(hmT[:, :, cs], bb[:, 0:512].rearrange("p (a b) -> p a b", a=4)),
                                  r=[b], w=[hmT])
                                grel(b)

                            def swa_gen(tt, g=g):
                                t = g * TPG + tt
                                cs = slice(tt * 128, (tt + 1) * 128)
                                tmpS_, pT_, dn4_, hao_ = tmpS[tt], pT[tt], dn4[tt], hao[tt]
                                vprev = vaug[(t - 1) % 3] if t > 0 else vhalo
                                vcur = vaug[t % 3]
                                pc = slice(tt * 128, tt * 128 + 128)
                                cc = slice(128 + tt * 128, 256 + tt * 128)
                                for kv in range(2):
                                    bS = [(yield from galloc()), (yield from galloc())]
                                    for rg in range(2):
                                        for bi, ks in enumerate((pc, cc)):
                                            for gg in range(2):
                                                g4 = gg * 2 + rg
                                                h = kv * 4 + g4
                                                p0 = 64 * rg
                                                sl_ = (bi * 2 + gg) * 128
                                                E("pe", lambda e, rg=rg, ks=ks, sl_=sl_, h=h, p0=p0: e.matmul(
                                                    bS[rg][:, sl_:sl_ + 128], kaT[p0:p0 + 64, kv, ks],
                                                    qaT[p0:p0 + 64, h // 2, cs], start=True, stop=True),
                                                  r=[kaT, qaT], w=[bS[rg]], inc=True)
                                        boff = (kv * 2 + rg) * 512
                                        E("dve", lambda e, rg=rg, boff=boff: e.scalar_tensor_tensor(
                                            tmpS_[:, rg, :], bS[rg][:], 0.125, biasT[:, boff:boff + 512], ALU.mult, ALU.add),
                                          r=[bS[rg], biasT], w=[tmpS_])
                                    grel(bS[0])
                                    grel(bS[1])
                                    yield
                                    E("act", lambda e: e.activation(pT_[:], tmpS_[:], AF.Exp), r=[tmpS_], w=[pT_])
                                    yield
                                    bp = yield from galloc()
                                    for g4 in range(4):
                                        o = bp[:, g4 * 65:(g4 + 1) * 65]
                                        rg, gg = g4 % 2, g4 // 2
                                        E("pe", lambda e, rg=rg, gg=gg, o=o: e.matmul(
                                            o, pT_[:, rg, gg * 128:(gg + 1) * 128], vprev[:, kv, 0:65], start=True, stop=False),
                                          r=[pT_, vprev], w=[bp], inc=False)
                                        E("pe", lambda e, rg=rg, gg=gg, o=o: e.matmul(
                                            o, pT_[:, rg, (2 + gg) * 128:(3 + gg) * 128], vcur[:, kv, 0:65], start=False,
                                            stop=True), r=[pT_, vcur], w=[bp], inc=True)
                                    yield
                                    pv3 = bp[:, 0:260].rearrange("p (a b) -> p a b", a=4)
                                    E("dve", lambda e, pv3=pv3: e.tensor_tensor(dn4_[:], pv3[:, :, 64],
                                                                                esink[:, kv * 4:(kv + 1) * 4], ALU.add),
                                      r=[bp, esink], w=[dn4_])
                                    E("dve", lambda e: e.reciprocal(dn4_[:], dn4_[:]), r=[dn4_], w=[dn4_])
                                    E("dve", lambda e, pv3=pv3: e.tensor_tensor(
                                        hao_[:, kv * 256:(kv + 1) * 256].rearrange("p (a b) -> p a b", a=4), pv3[:, :, 0:64],
                                        dn4_[:].unsqueeze(2).to_broadcast([128, 4, 64]), ALU.mult), r=[bp, dn4_], w=[hao_])
                                    grel(bp)
                                    yield
                                b = yield from galloc()
                                bb = b[:].bitcast(BF16)
                                for f in range(4):
                                    E("pe", lambda e, f=f, bb=bb: e.transpose(bb[:, f * 128:(f + 1) * 128],
                                                                               hao_[:, f * 128:(f + 1) * 128], ident_b[:]),
                                      r=[hao_, ident_b], w=[b], inc=(f == 3))
                                E("act", lambda e, bb=bb: e.copy(haT[:, :, cs], bb[:, 0:512].rearrange("p (a b) -> p a b", a=4)),
                                  r=[b], w=[haT])
                                grel(b)

                            pending_m = [mlstm_gen(tt) for tt in range(TPG)]
                            if os.environ.get("KSEQ") == "1":
                                for gm in pending_m:
                                    for _ in gm:
                                        pass
                                pending_m = []
                                if pss == 2:
                                    for tt in range(TPG):
                                        for _ in swa_gen(tt):
                                            pass
                                pss_swa = False
                            elif os.environ.get("KSEQ") == "2":
                                pss_swa = False
                            else:
                                pss_swa = (pss == 2)
                            chains = []
                            if pss_swa:
                                chains += [swa_gen(tt) for tt in range(TPG)]
                            if g + 1 < NG and os.environ.get("KSEQ") is None:
                                def nnext_gen(g=g):
                                    for tt in range(TPG):
                                        yield from norm_gen((g + 1) * TPG + tt, a1, sh1, hT2[(g + 1) % 2], tt * 128, nsc,
                                                            alloc=galloc, rel=grel)
                                chains.append(nnext_gen())
                            elif pss == 1 and g + 1 == NG and os.environ.get("KSEQ") is None:
                                def nfirst_gen():
                                    for tt in range(TPG):
                                        yield from norm_gen(tt, a1, sh1, hT2[0], tt * 128, nsc, alloc=galloc, rel=grel)
                                chains.append(nfirst_gen())
                            elif g + 1 < NG:
                                for tt in range(TPG):
                                    norm_tile((g + 1) * TPG + tt, a1, sh1, hT2[(g + 1) % 2], tt * 128, nsc)
                            started = 0
                            mchains = []
                            next_ok = True
                            while pending_m or chains or mchains:
                                if pending_m and next_ok:
                                    gm = pending_m.pop(0)
                                    mchains.append(gm)
                                    next_ok = False
                                    fresh = gm
                                else:
                                    fresh = None
                                for gm in list(mchains):
                                    try:
                                        v = next(gm)
                                        if v == "state_done":
                                            next_ok = True
                                    except StopIteration:
                                        mchains.remove(gm)
                                        if gm is fresh or True:
                                            pass
                                for gs in list(chains):
                                    try:
                                        next(gs)
                                    except StopIteration:
                                        chains.remove(gs)
                                for gb_ in list(background):
                                    try:
                                        next(gb_)
                                    except StopIteration:
                                        background.remove(gb_)
                            if os.environ.get("KSEQ") == "2" and pss == 2:
                                for tt in range(TPG):
                                    for _ in swa_gen(tt):
                                        pass
                            chk(23)
                            E("act", lambda e: e.copy(kaT[:, :, 0:128], kaT[:, :, G:G + 128]), r=[kaT], w=[kaT])
                            chk(24)
                            if pss == 2:
                                for pi in range(4):
                                    rb_ = ring_get()
                                    for oi in range(2):
                                        ot = 2 * pi + oi
                                        bgm, bga, bbm, bba = bank(), bank(), bank(), bank()
                                        mm_group(bgm[:, 0:G], [(rb_[:, k, (2 * oi) * 128:(2 * oi + 1) * 128], hT[:, k, :])
                                                               for k in range(8)], r=[rb_, hT], w=[bgm])
                                        mm_group(bga[:, 0:G], [(rb_[:, k, (2 * oi + 1) * 128:(2 * oi + 2) * 128], hT[:, k, :])
                                                               for k in range(8)], r=[rb_, hT], w=[bga])
                                        mm_group(bbm[:, 0:G], [(wbr[:, 0, k, ot * 128:(ot + 1) * 128], hmT[:, k, :]) for k in range(4)],
                                                 r=[wbr, hmT], w=[bbm])
                                        mm_group(bba[:, 0:G], [(wbr[:, 1, k, ot * 128:(ot + 1) * 128], haT[:, k, :]) for k in range(4)],
                                                 r=[wbr, haT], w=[bba])
                                        E("act", lambda e, bgm=bgm: e.activation(sg1[:], bgm[:, 0:G], AF.Sigmoid), r=[bgm], w=[sg1])
                                        E("act", lambda e, bga=bga: e.activation(sg2[:], bga[:, 0:G], AF.Sigmoid), r=[bga], w=[sg2])
                                        E("dve", lambda e, bbm=bbm: e.tensor_tensor(sg1[:], bbm[:, 0:G], sg1[:], ALU.mult),
                                          r=[sg1, bbm], w=[sg1])
                                        E("dve", lambda e, bba=bba: e.tensor_tensor(sg2[:], bba[:, 0:G], sg2[:], ALU.mult),
                                          r=[sg2, bba], w=[sg2])
                                        E("dve", lambda e, ot=ot: e.tensor_tensor(mT[:, ot, :], sg1[:], sg2[:], ALU.add),
                                          r=[sg1, sg2], w=[mT])
                                    ring_issue()
                            chk(25)
                            for tt in (range(TPG) if pss == 2 else ()):
                                t = g * TPG + tt
                                for oc in range(2):
                                    b = bank()
                                    mm_group(b[:], [(mT[:, k, tt * 128:(tt + 1) * 128], wout[:, k, oc * 512:(oc + 1) * 512])
                                                    for k in range(8)], r=[mT, wout], w=[b])
                                    E("dve", lambda e, b=b, t=t, oc=oc: e.tensor_tensor(
                                        x[:, t, oc * 512:(oc + 1) * 512], b[:], x[:, t, oc * 512:(oc + 1) * 512], ALU.add),
                                      r=[b, xt[t]], w=[xt[t]])
                    except _Stop:
                        pass
                    if pss == 1:
                        lastc = slice(128 + (TPG - 1) * 128, 256 + (TPG - 1) * 128)
                        vlast = vaug[(NT - 1) % 3]
                        DMA("sp", ibv[:, 0:258].rearrange("p (a b) -> p a b", a=2), state_f[:], r=[state_f], w=[ib_parts[0]], key="x_st")
                        for i in range(4):
                            DMA("sp", ibv[:, 258 + 3 * i:261 + 3 * i], qkpre[i][:, 0:3], r=[qkpre[i]], w=[ib_parts[1 + i]], key=f"x_cv{i}")
                        DMA("pool", ibv[:, 270:526].rearrange("p (a b) -> p a b", a=2), kaT[:, :, 0:128], r=[kaT], w=[ib_parts[5]], key="x_ka")
                        DMA("pool", ibv[:, 526:656].rearrange("p (a b) -> p a b", a=2), vlast[:, :, 0:65], r=[vlast], w=[ib_parts[6]], key="x_va")
                        sy.emit_cc(lambda e: e.collective_compute("AllGather", ALU.bypass,
                                                                  replica_groups=[[2 * i, 2 * i + 1] for i in range(NCORES // 2)],
                                                                  ins=[ibv.opt()], outs=[obv.opt()]),
                                   reads=[t.r for t in ib_parts], writes=[obR.r], key=f"cc{l}")
                sy.barrier()
                p2s.close()

            if not moe:
                continue
            with ExitStack() as ms:
                h2T = mk(f"h2T_{l}", [128, 8, NTOK], BF16, ms)
                gates = mk(f"gates_{l}", [128, NT, NE], F32, ms)
                xnT2 = [mk(f"xnT{i}_{l}", [128, D], F32, ms) for i in range(2)]
                xnT = xnT2[0]
                wr2 = mk(f"wr2_{l}", [128, 8, NE], F32, ms)
                cbc = mk(f"cbc_{l}", [128, NE], F32, ms)
                nsc2 = [(None, mk(f"ssumb{i}_{l}", [128, 1], F32, ms),
                         mk(f"tmp1b{i}_{l}", [128, 1], F32, ms), mk(f"tmp2b{i}_{l}", [128, 1], F32, ms),
                         mk(f"rstdb{i}_{l}", [128, 1], F32, ms), mk(f"xnb{i}_{l}", [128, D], F32, ms)) for i in range(2)]
                wgb = [mk(f"wgb{i}_{l}", [128, 8, DE], BF16, ms) for i in range(2)]
                wub = [mk(f"wub{i}_{l}", [128, 8, DE], BF16, ms) for i in range(2)]
                wdb = [mk(f"wdb{i}_{l}", [128, 4, D], BF16, ms) for i in range(2)]
                actT = [mk(f"actT{i}_{l}", [128, 4, 512], BF16, ms) for i in range(2)]
                sil = [mk(f"sil{i}_{l}", [128, 512], F32, ms) for i in range(2)]
                R16s = [{n: mk(f"r_{n}{i}_{l}", [128, 16], F32, ms) for n in
                         ["sc", "sel", "selm", "m1", "selm2", "m2", "wts"]} for i in range(2)]
                R4s = [{n: mk(f"q_{n}{i}_{l}", [128, 4], F32, ms) for n in
                        ["hi1", "lo1", "hi2", "lo2", "top1", "mm2", "mm3", "grp", "gmask"]} for i in range(2)]
                R1s = [{n: mk(f"s_{n}{i}_{l}", [128, 1], F32, ms) for n in ["gmax", "t1", "t2", "den"]} for i in range(2)]

                def load_expert(e_):
                    i = e_ % 2
                    DMA("pool", wgb[i][:], wg_d[l, e_].rearrange("(k p) n -> p k n", p=128), w=[wgb[i]], key=f"wg{i}")
                    DMA("pool", wub[i][:], wu_d[l, e_].rearrange("(k p) n -> p k n", p=128), w=[wub[i]], key=f"wu{i}")
                    DMA("pool", wdb[i][:], wd_d[l, e_].rearrange("(k p) n -> p k n", p=128), w=[wdb[i]], key=f"wd{i}")

                load_expert(0)
                load_expert(1)
                E("dve", lambda e: e.tensor_tensor(wr2[:], pg("wr").rearrange("p (a b) -> p a b", a=8),
                                                   a2[:].unsqueeze(2).to_broadcast([128, 8, NE]), ALU.mult),
                  r=[prmg, a2], w=[wr2])
                sh2bc = sil[0]
                sh2bc = xnT
                sh2v = sh2bc[:].rearrange("p (a b) -> p a b", a=8)
                E("dve", lambda e: e.tensor_copy(sh2v, sh2.unsqueeze(2).to_broadcast([128, 8, 128])),
                  r=[modc], w=[sh2bc])
                b = bank()
                mm_group(b[:, 0:NE], [(sh2v[:, k, :], pg("wr", k * NE, (k + 1) * NE)) for k in range(8)],
                         r=[sh2bc, prmg], w=[b])
                E("dve", lambda e, b=b: e.tensor_copy(cbc[:], b[:, 0:NE]), r=[b], w=[cbc])

                mfree = list(range(8))

                def malloc_():
                    while not mfree:
                        yield
                    return banks[mfree.pop(0)]

                def mrel(b_):
                    mfree.append(banks.index(b_))

                def tile_chain(t):
                    par = t % 2
                    xnT = xnT2[par]
                    R16, R4, R1 = R16s[par], R4s[par], R1s[par]
                    yield from norm_gen(t, a2, sh2, h2T, t * 128, nsc2[par], router_dst=xnT, alloc=malloc_, rel=mrel)
                    yield "norm_done"
                    b = yield from malloc_()
                    mm_group(b[:, 0:NE], [(xnT[:, k * 128:(k + 1) * 128], wr2[:, k, :]) for k in range(8)],
                             r=[xnT, wr2], w=[b])
                    r16, r4_, r1 = R16, R4, R1
                    E("dve", lambda e, b=b: e.tensor_tensor(r16["sel"][:], b[:, 0:NE], cbc[:], ALU.add),
                      r=[b, cbc], w=[r16["sel"]])
                    mrel(b)
                    yield
                    E("act", lambda e: e.activation(r16["sc"][:], r16["sel"][:], AF.Sigmoid), r=[r16["sel"]], w=[r16["sc"]])
                    E("dve", lambda e: e.tensor_tensor(r16["sel"][:], r16["sc"][:], pg("rb"), ALU.add),
                      r=[r16["sc"], prmg], w=[r16["sel"]])
                    s3 = r16["sel"][:].rearrange("p (a b) -> p a b", a=4)

                    def tt_(o, i0, i1, op, rr, ww):
                        E("dve", lambda e: e.tensor_tensor(o, i0, i1, op), r=rr, w=ww)
                    tt_(r4_["hi1"][:], s3[:, :, 0], s3[:, :, 1], ALU.max, [r16["sel"]], [r4_["hi1"]])
                    tt_(r4_["lo1"][:], s3[:, :, 0], s3[:, :, 1], ALU.min, [r16["sel"]], [r4_["lo1"]])
                    tt_(r4_["hi2"][:], s3[:, :, 2], s3[:, :, 3], ALU.max, [r16["sel"]], [r4_["hi2"]])
                    tt_(r4_["lo2"][:], s3[:, :, 2], s3[:, :, 3], ALU.min, [r16["sel"]], [r4_["lo2"]])
                    tt_(r4_["top1"][:], r4_["hi1"][:], r4_["hi2"][:], ALU.max, [r4_["hi1"], r4_["hi2"]], [r4_["top1"]])
                    tt_(r4_["mm2"][:], r4_["hi1"][:], r4_["hi2"][:], ALU.min, [r4_["hi1"], r4_["hi2"]], [r4_["mm2"]])
                    tt_(r4_["mm3"][:], r4_["lo1"][:], r4_["lo2"][:], ALU.max, [r4_["lo1"], r4_["lo2"]], [r4_["mm3"]])
                    tt_(r4_["mm2"][:], r4_["mm2"][:], r4_["mm3"][:], ALU.max, [r4_["mm2"], r4_["mm3"]], [r4_["mm2"]])
                    tt_(r4_["grp"][:], r4_["top1"][:], r4_["mm2"][:], ALU.add, [r4_["top1"], r4_["mm2"]], [r4_["grp"]])
                    yield
                    E("dve", lambda e: e.tensor_reduce(r1["gmax"][:], r4_["grp"][:], AX.X, ALU.max),
                      r=[r4_["grp"]], w=[r1["gmax"]])
                    E("dve", lambda e: e.tensor_scalar(r4_["gmask"][:], r4_["grp"][:], r1["gmax"][:, 0:1], None, ALU.is_ge),
                      r=[r4_["grp"], r1["gmax"]], w=[r4_["gmask"]])
                    E("dve", lambda e: e.tensor_scalar(r4_["gmask"][:], r4_["gmask"][:], 1e9, -1e9, ALU.mult, ALU.add),
                      r=[r4_["gmask"]], w=[r4_["gmask"]])
                    E("dve", lambda e: e.tensor_tensor(r16["selm"][:].rearrange("p (a b) -> p a b", a=4), s3,
                                                       r4_["gmask"][:].unsqueeze(2).to_broadcast([128, 4, 4]), ALU.add),
                      r=[r16["sel"], r4_["gmask"]], w=[r16["selm"]])
                    yield
                    E("dve", lambda e: e.tensor_reduce(r1["t1"][:], r16["selm"][:], AX.X, ALU.max),
                      r=[r16["selm"]], w=[r1["t1"]])
                    E("dve", lambda e: e.tensor_scalar(r16["m1"][:], r16["selm"][:], r1["t1"][:, 0:1], None, ALU.is_ge),
                      r=[r16["selm"], r1["t1"]], w=[r16["m1"]])
                    E("dve", lambda e: e.scalar_tensor_tensor(r16["selm2"][:], r16["m1"][:], -1e9, r16["selm"][:],
                                                              ALU.mult, ALU.add),
                      r=[r16["m1"], r16["selm"]], w=[r16["selm2"]])
                    yield
                    E("dve", lambda e: e.tensor_reduce(r1["t2"][:], r16["selm2"][:], AX.X, ALU.max),
                      r=[r16["selm2"]], w=[r1["t2"]])
                    E("dve", lambda e: e.tensor_scalar(r16["m2"][:], r16["selm2"][:], r1["t2"][:, 0:1], None, ALU.is_ge),
                      r=[r16["selm2"], r1["t2"]], w=[r16["m2"]])
                    tt_(r16["m1"][:], r16["m1"][:], r16["m2"][:], ALU.add, [r16["m1"], r16["m2"]], [r16["m1"]])
                    tt_(r16["wts"][:], r16["sc"][:], r16["m1"][:], ALU.mult, [r16["sc"], r16["m1"]], [r16["wts"]])
                    yield
                    E("dve", lambda e: e.tensor_reduce(r1["den"][:], r16["wts"][:], AX.X, ALU.add),
                      r=[r16["wts"]], w=[r1["den"]])
                    E("dve", lambda e: e.reciprocal(r1["den"][:], r1["den"][:]), r=[r1["den"]], w=[r1["den"]])
                    E("dve", lambda e, t=t: e.tensor_scalar(gates[:, t, :], r16["wts"][:], r1["den"][:, 0:1], None, ALU.mult),
                      r=[r16["wts"], r1["den"]], w=[gates])


                tchains = []
                nxt = 0
                ok = True
                while nxt < NT or tchains:
                    if ok and nxt < NT and len(tchains) < 2:
                        tchains.append(tile_chain(nxt))
                        nxt += 1
                        ok = False
                    for gtc in list(tchains):
                        try:
                            v = next(gtc)
                            if v == "norm_done":
                                ok = True
                        except StopIteration:
                            tchains.remove(gtc)
                            ok = True
                for e_ in range(NE):
                    i = e_ % 2
                    for k in range(4):
                        E("dve", lambda e, k=k, i=i: e.tensor_tensor(wdb[i][:, k, :], wdb[i][:, k, :], g2bc[:], ALU.mult),
                          r=[wdb[i], g2bc], w=[wdb[i]])
                    for gq in range(NTOK // 512):
                        at = actT[gq % 2]
                        ts = slice(gq * 512, (gq + 1) * 512)
                        for dt_ in range(4):
                            bg, bu = bank(), bank()
                            mm_group(bg[:], [(wgb[i][:, k, dt_ * 128:(dt_ + 1) * 128], h2T[:, k, ts]) for k in range(8)],
                                     r=[wgb[i], h2T], w=[bg])
                            mm_group(bu[:], [(wub[i][:, k, dt_ * 128:(dt_ + 1) * 128], h2T[:, k, ts]) for k in range(8)],
                                     r=[wub[i], h2T], w=[bu])
                            sl = sil[dt_ % 2]
                            E("act", lambda e, bg=bg, sl=sl: e.activation(sl[:], bg[:], AF.Silu), r=[bg], w=[sl])
                            E("dve", lambda e, bu=bu, sl=sl, at=at, dt_=dt_: e.tensor_tensor(at[:, dt_, :], bu[:], sl[:],
                                                                                           ALU.mult),
                              r=[sl, bu], w=[at])
                        for tt in range(4):
                            t = gq * 4 + tt
                            for oc in range(2):
                                b = bank()
                                mm_group(b[:], [(at[:, k, tt * 128:(tt + 1) * 128], wdb[i][:, k, oc * 512:(oc + 1) * 512])
                                                for k in range(4)], r=[at, wdb[i]], w=[b])
                                E("dve", lambda e, b=b, t=t, oc=oc, e_=e_: e.scalar_tensor_tensor(
                                    x[:, t, oc * 512:(oc + 1) * 512], b[:], gates[:, t, e_:e_ + 1],
                                    x[:, t, oc * 512:(oc + 1) * 512], ALU.mult, ALU.add),
                                  r=[b, gates, xt[t]], w=[xt[t]])
                    if e_ + 2 < NE:
                        load_expert(e_ + 2)
                sy.barrier()

        yv = y_d.rearrange("(t p) d -> p t d", p=128)
        if final:
            with ExitStack() as ms:
                fss = [(mk(f"fjunk{i}", [128, D], BF16, ms), mk(f"fssum{i}", [128, 1], F32, ms),
                        mk(f"ftmp1{i}", [128, 1], F32, ms), mk(f"ftmp2{i}", [128, 1], F32, ms),
                        mk(f"frstd{i}", [128, 1], F32, ms)) for i in range(2)]
                yb = [mk(f"yb{i}", [128, D], F32, ms) for i in range(2)]

                def fin_gen(t):
                    junk, ssum, tmp1, tmp2, rstd = fss[t % 2]
                    E("dve", lambda e: e.memset(ssum[:], 0.0), w=[ssum])
                    E("act", lambda e, t=t: e.activation(junk[:], x[:, t, :], AF.Square, accum_out=ssum[:, 0:1]),
                      r=[xt[t], ssum], w=[junk, ssum])
                    yield
                    E("dve", lambda e: e.tensor_scalar(tmp1[:], ssum[:], 1.0 / D, EPS, ALU.mult, ALU.add), r=[ssum], w=[tmp1])
                    E("act", lambda e: e.sqrt(tmp2[:], tmp1[:]), r=[tmp1], w=[tmp2])
                    yield
                    E("dve", lambda e: e.reciprocal(rstd[:], tmp2[:]), r=[tmp2], w=[rstd])
                    o = yb[t % 2]
                    E("dve", lambda e, t=t, o=o: e.scalar_tensor_tensor(o[:], x[:, t, :], rstd[:, 0:1], pg("wfin"),
                                                                        ALU.mult, ALU.mult),
                      r=[xt[t], rstd, prmg], w=[o])
                    yield
                    DMA("sp", yv[:, t, :], o[:], r=[o], key=f"out{t % 2}")

                fch = []
                nxt_ = 0
                while nxt_ < NT or fch:
                    if nxt_ < NT and len(fch) < 2:
                        fch.append(fin_gen(nxt_))
                        nxt_ += 1
                    for gf in list(fch):
                        try:
                            next(gf)
                        except StopIteration:
                            fch.remove(gf)
                sy.barrier()
        else:
            for t in range(NT):
                DMA("sp", yv[:, t, :], x[:, t, :], r=[xt[t]], key="out")
            sy.barrier()
    return nc


def _t5_bucket(n):
    max_exact = 16
    large = max_exact + (np.log(np.maximum(n, 1) / max_exact) / math.log(128 / max_exact) * (32 - max_exact)).astype(np.int32)
    large = np.minimum(large, 31)
    return np.where(n < max_exact, n, large).astype(np.int32)


def _colmajor(v, ncol):
    return np.ascontiguousarray(np.asarray(v, np.float32).reshape(ncol, 128).T)


def _bc(v):
    v = np.asarray(v, np.float32).reshape(1, -1)
    return np.ascontiguousarray(np.broadcast_to(v, (128, v.shape[1])))


_WIN_PERM = None


def _win_perm():
    q_m = list(range(0, 256)); k_m = list(range(256, 512)); v_m = list(range(512, 1024)); o_m = list(range(1024, 1536))
    i_g = list(range(1536, 1540)); f_g = list(range(1540, 1544)); q_a = list(range(1544, 2056))
    k_a = list(range(2056, 2184)); v_a = list(range(2184, 2312)); g_m = list(range(2312, 3336)); g_a = list(range(3336, 4360))
    cols = []
    cols += q_m + k_m
    cols += q_a
    cols += k_a[0:64] + k_a[0:64] + k_a[64:128] + k_a[64:128] + i_g + f_g + v_a
    cols += v_m
    cols += o_m
    for pi in range(4):
        for oi in range(2):
            ot = 2 * pi + oi
            cols += g_m[ot * 128:(ot + 1) * 128] + g_a[ot * 128:(ot + 1) * 128]
    assert len(cols) == NWIN
    return np.asarray(cols, np.int64)


def _consts(rel_bias):
    cst = np.zeros((128, 384), np.float32)
    cst[:, 0:128] = np.eye(128, dtype=np.float32)
    s = np.arange(128)[:, None]
    l_ = np.arange(128)[None, :]
    cst[:, 128:256] = (s <= l_).astype(np.float32)
    cst[:, 256:384] = 1.0
    q = np.arange(128)[None, :]
    sk = np.arange(128)[:, None]
    bT = np.zeros((128, 2, 2, 2, 2, 128), np.float32)
    for blk in range(2):
        dist = q + 128 - (sk + blk * 128)
        ok = (dist >= 0) & (dist < 128)
        bucket = _t5_bucket(np.maximum(dist, 0))
        vals = np.asarray(rel_bias, np.float32)[bucket]
        vals = np.where(ok[:, :, None], vals, np.float32(-30000.0))
        for h in range(8):
            kv, g4 = h // 4, h % 4
            bT[:, kv, g4 % 2, blk, g4 // 2, :] = vals[:, :, h]
    return cst, np.ascontiguousarray(bT.reshape(128, 2048))


_PROGS = {}


def _prog(nl, final):
    key = (nl, final)
    if key not in _PROGS:
        _PROGS[key] = build(nl, final)
    return _PROGS[key]


def _prep_common(inp):
    perm = _win_perm()
    cst, biasT = _consts(inp["rel_bias"])
    return perm, cst, biasT


def _layer_arrays(inp, ls, perm):
    f = lambda a: np.ascontiguousarray(np.asarray(a, np.float32))
    d = {}
    d["w_ada"] = f(inp["w_ada"][ls])
    d["w_in"] = f(np.asarray(inp["w_in"])[ls][:, :, perm])
    d["w_br"] = f(np.stack([np.asarray(inp["w_br_m"])[ls], np.asarray(inp["w_br_a"])[ls]], axis=1))
    d["w_out"] = f(inp["w_out"][ls])
    d["wg"] = f(inp["w_gate_e"][ls])
    d["wu"] = f(inp["w_up_e"][ls])
    d["wd"] = f(inp["w_down_e"][ls])
    prml = []
    for l in ls:
        ba = np.asarray(inp["b_ada"][l], np.float32)
        parts = [
            _colmajor(inp["w_norm1"][l], 8), _colmajor(inp["w_norm2"][l], 8), _colmajor(ba, 48),
            np.ascontiguousarray(np.asarray(inp["conv_w"][l], np.float32).reshape(4, 4, 128).transpose(2, 1, 0).reshape(128, 16)),
            _colmajor(inp["conv_b"][l], 4),
            _bc(np.concatenate([np.asarray(inp["b_igate"][l]), np.asarray(inp["b_fgate"][l])])),
            _bc(inp["w_mnorm"][l]), _bc(inp["sinks"][l]),
        ]
        p = np.concatenate(parts, axis=1)
        assert p.shape == (128, NPL), p.shape
        prml.append(p)
    d["prml"] = np.ascontiguousarray(np.stack(prml, 0))
    d["prmb"] = np.ascontiguousarray(np.stack([np.concatenate(
        [_bc(np.asarray(inp["b_ada"][l], np.float32)[2048:3072]), _bc(np.asarray(inp["b_ada"][l], np.float32)[5120:6144])],
        axis=1) for l in ls], 0))
    return d


def _prmg(inp, b, half):
    wr = np.asarray(inp["w_router"], np.float32).reshape(8, 128, NE).transpose(1, 0, 2).reshape(128, 8 * NE)
    parts = [_colmajor(np.asarray(inp["c"])[b], 8), np.full((128, 1), float(half), np.float32),
             _bc(inp["router_bias"]), np.ascontiguousarray(wr), _bc(inp["w_final"])]
    p = np.concatenate(parts, axis=1)
    assert p.shape == (128, NPG)
    return np.ascontiguousarray(p)


def kernel(**inp):
    inp = {k: np.asarray(v) for k, v in inp.items()}
    perm, cst, biasT = _prep_common(inp)
    la = _layer_arrays(inp, [0, 1], perm)
    nc = _prog(2, True)
    in_maps = []
    for c in range(8):
        m = {"x": np.ascontiguousarray(inp["x"][c // 2, (c % 2) * NTOK:(c % 2 + 1) * NTOK, :], dtype=np.float32),
             "prmg": _prmg(inp, c // 2, c % 2), "cst": cst, "biasT": biasT}
        m.update(la)
        in_maps.append(m)
    res = run_bass_kernel_spmd(nc, in_maps, core_ids=list(range(8)))
    out = np.zeros((4, 4096, D), np.float32)
    for c in range(8):
        out[c // 2, (c % 2) * NTOK:(c % 2 + 1) * NTOK, :] = np.asarray(res.results[c]["y"], dtype=np.float32)
    return out
```
